# Optimizing a Trainium2 kernel written in Bass

```python
import jax, jax.numpy as jnp
from jax import lax
import numpy as np

D_MODEL = 1024
BATCH = 16
SEQ = 2048
DEPTH = 2

HEAD_DIM = 64
SB_HEADS = 8
NSA_HEADS = 8
NSA_KV_GROUPS = 2
NSA_REP = NSA_HEADS // NSA_KV_GROUPS
NSA_CMP_LEN = 32
NSA_CMP_STRIDE = 16
NSA_CMP_HIDDEN = 256
NSA_SLC_BLOCK = 64
NSA_SLC_TOPK = 16
NSA_WINDOW = 512
Q_BLOCK = 128
SLC_Q_BLOCK = 32
SB_W = SB_HEADS * HEAD_DIM
NSA_Q_W = NSA_HEADS * HEAD_DIM
NSA_KV_W = 6 * NSA_KV_GROUPS * HEAD_DIM
NSA_GATE_W = 3 * NSA_HEADS
MIX0_IN = 3 * SB_W + NSA_Q_W + NSA_KV_W + NSA_GATE_W
MIX0_OUT = SB_W + NSA_Q_W
CONV_WIDTH = 3
D_FF = 2816
N_EXPERTS = 8
TOP_K = 2
EXPERT_D_FF = 2816
MOE_ROW_BLOCK = 512
LN_EPS = 1e-5
DN_ALPHA = (2 * DEPTH) ** 0.25
DN_BETA = (8 * DEPTH) ** -0.25
NEG = -1e30
SEL_FORCE = 1e30

kernel_name = 'hybrid_sb_nsa_conv_moe_block'


def layer_norm(x, g, b):
    xf = x.astype(jnp.float32)
    mu = xf.mean(-1, keepdims=True)
    var = jnp.square(xf - mu).mean(-1, keepdims=True)
    return ((xf - mu) * lax.rsqrt(var + LN_EPS) * g + b).astype(x.dtype)


def masked_softmax(s, mask):
    return jax.nn.softmax(jnp.where(mask, s, NEG), axis=-1) * mask


def stick_breaking_attention(q, k, v):
    S = q.shape[2]
    scale = HEAD_DIM ** -0.5
    outs = []
    for start in range(0, S, Q_BLOCK):
        end = start + Q_BLOCK
        z = jnp.einsum('bhqd,bhkd->bhqk', q[:, :, start:end], k[:, :, :end]).astype(jnp.float32) * scale
        causal = jnp.arange(end)[None, :] < jnp.arange(start, end)[:, None]
        log_fail = jnp.where(causal, jax.nn.log_sigmoid(-z), 0.0)
        later = lax.cumsum(log_fail, axis=3, reverse=True) - log_fail
        w = jnp.where(causal, jnp.exp(jax.nn.log_sigmoid(z) + later), 0.0)
        outs.append(jnp.einsum('bhqk,bhkd->bhqd', w.astype(v.dtype), v[:, :, :end]))
    return jnp.concatenate(outs, axis=2)


def compress_blocks(x, pos, w1, w2):
    B_, G, S, dh = x.shape
    r = NSA_CMP_LEN // NSA_CMP_STRIDE
    n_chunk = S // NSA_CMP_STRIDE
    nc = n_chunk - r + 1
    ch = x.reshape(B_, G, n_chunk, NSA_CMP_STRIDE, dh)
    blocks = jnp.concatenate([ch[:, :, j:j + nc] for j in range(r)], axis=3) + pos
    h = jax.nn.silu(blocks.reshape(B_, G, nc, NSA_CMP_LEN * dh) @ w1)
    return h @ w2


def nsa_attention(q, kv, gates, cmp_pos, cmp_w1, cmp_w2):
    B_, G, R, S, dh = q.shape
    scale = HEAD_DIM ** -0.5
    t = jnp.arange(S)
    k_cmp, v_cmp, k_slc, v_slc, k_win, v_win = kv

    kc = compress_blocks(k_cmp, cmp_pos[0], cmp_w1[0], cmp_w2[0])
    vc = compress_blocks(v_cmp, cmp_pos[1], cmp_w1[1], cmp_w2[1])
    nc = kc.shape[2]
    cmp_start = jnp.arange(nc) * NSA_CMP_STRIDE
    cmp_last = cmp_start + NSA_CMP_LEN - 1
    s_c = jnp.einsum('bgrtd,bgnd->bgrtn', q, kc).astype(jnp.float32) * scale
    p_c = masked_softmax(s_c, cmp_last[None, :] <= t[:, None])
    o_cmp = jnp.einsum('bgrtn,bgnd->bgrtd', p_c.astype(q.dtype), vc)

    ns = S // NSA_SLC_BLOCK
    slc_start = jnp.arange(ns) * NSA_SLC_BLOCK
    overlap = ((cmp_start[:, None] < slc_start[None, :] + NSA_SLC_BLOCK)
               & (cmp_start[:, None] + NSA_CMP_LEN > slc_start[None, :])).astype(jnp.float32)
    imp = jnp.einsum('bgrtn,ns->bgts', p_c, overlap)
    blk = jnp.arange(ns)[None, :]
    cur = (t // NSA_SLC_BLOCK)[:, None]
    valid = slc_start[None, :] <= t[:, None]
    forced = (blk == 0) | (blk == cur) | (blk == cur - 1)
    imp = jnp.where(valid, jnp.where(forced, SEL_FORCE, imp), NEG)
    n_top = min(NSA_SLC_TOPK, ns)
    _, sel = lax.top_k(imp, n_top)

    ksb = k_slc.reshape(B_, G, ns, NSA_SLC_BLOCK, dh)
    vsb = v_slc.reshape(B_, G, ns, NSA_SLC_BLOCK, dh)
    nq = S // SLC_Q_BLOCK
    bi = jnp.arange(B_)[:, None, None, None]
    gi = jnp.arange(G)[None, :, None, None]
    n_keys = n_top * NSA_SLC_BLOCK

    def slc_block(args):
        qc, sc, tc = args
        kg = ksb[bi, gi, sc].reshape(B_, G, SLC_Q_BLOCK, n_keys, dh)
        vg = vsb[bi, gi, sc].reshape(B_, G, SLC_Q_BLOCK, n_keys, dh)
        kpos = (sc[..., None] * NSA_SLC_BLOCK + jnp.arange(NSA_SLC_BLOCK)).reshape(B_, G, SLC_Q_BLOCK, n_keys)
        s = jnp.einsum('bgrqd,bgqkd->bgrqk', qc, kg).astype(jnp.float32) * scale
        p = masked_softmax(s, (kpos <= tc[:, None])[:, :, None])
        return jnp.einsum('bgrqk,bgqkd->bgrqd', p.astype(qc.dtype), vg)

    q_ch = jnp.moveaxis(q.reshape(B_, G, R, nq, SLC_Q_BLOCK, dh), 3, 0)
    sel_ch = jnp.moveaxis(sel.reshape(B_, G, nq, SLC_Q_BLOCK, n_top), 2, 0)
    o_slc = lax.map(slc_block, (q_ch, sel_ch, t.reshape(nq, SLC_Q_BLOCK)))
    o_slc = jnp.moveaxis(o_slc, 0, 3).reshape(B_, G, R, S, dh)

    span = NSA_WINDOW + Q_BLOCK
    pad = ((0, 0), (0, 0), (NSA_WINDOW, 0), (0, 0))
    kp = jnp.pad(k_win, pad)
    vp = jnp.pad(v_win, pad)

    def win_block(i):
        start = i * Q_BLOCK
        qb = lax.dynamic_slice_in_dim(q, start, Q_BLOCK, axis=3)
        kb = lax.dynamic_slice_in_dim(kp, start, span, axis=2)
        vb = lax.dynamic_slice_in_dim(vp, start, span, axis=2)
        tq = start + jnp.arange(Q_BLOCK)
        kpos = start - NSA_WINDOW + jnp.arange(span)
        diff = tq[:, None] - kpos[None, :]
        mask = (diff >= 0) & (diff < NSA_WINDOW) & (kpos[None, :] >= 0)
        s = jnp.einsum('bgrqd,bgkd->bgrqk', qb, kb).astype(jnp.float32) * scale
        p = masked_softmax(s, mask)
        return jnp.einsum('bgrqk,bgkd->bgrqd', p.astype(q.dtype), vb)

    o_win = lax.map(win_block, jnp.arange(S // Q_BLOCK))
    o_win = jnp.moveaxis(o_win, 0, 3).reshape(B_, G, R, S, dh)

    g = jax.nn.sigmoid(gates)
    return g[..., 0:1] * o_cmp + g[..., 1:2] * o_slc + g[..., 2:3] * o_win


def parallel_attention_mixer(h, w_in, cmp_pos, cmp_w1, cmp_w2, w_out):
    B_, S, _ = h.shape
    proj = h @ w_in
    sizes = [SB_W, SB_W, SB_W, NSA_Q_W, NSA_KV_W]
    cuts = [int(v) for v in np.cumsum(sizes)]
    sb_q, sb_k, sb_v, nsa_q, nsa_kv, nsa_g = jnp.split(proj, cuts, axis=-1)
    heads = lambda a: a.reshape(B_, S, SB_HEADS, HEAD_DIM).transpose(0, 2, 1, 3)
    o_sb = stick_breaking_attention(heads(sb_q), heads(sb_k), heads(sb_v))
    o_sb = o_sb.transpose(0, 2, 1, 3).reshape(B_, S, SB_W)
    q = nsa_q.reshape(B_, S, NSA_KV_GROUPS, NSA_REP, HEAD_DIM).transpose(0, 2, 3, 1, 4)
    kv = nsa_kv.reshape(B_, S, 6, NSA_KV_GROUPS, HEAD_DIM).transpose(2, 0, 3, 1, 4)
    gates = nsa_g.reshape(B_, S, NSA_KV_GROUPS, NSA_REP, 3).transpose(0, 2, 3, 1, 4)
    o_nsa = nsa_attention(q, kv, gates, cmp_pos, cmp_w1, cmp_w2)
    o_nsa = o_nsa.transpose(0, 3, 1, 2, 4).reshape(B_, S, NSA_Q_W)
    return jnp.concatenate([o_sb, o_nsa], axis=-1) @ w_out


def short_conv_mixer(h, w_in, conv_taps, w_out):
    gate_b, gate_c, u = jnp.split(h @ w_in, 3, axis=-1)
    z = gate_c * u
    zc = lax.conv_general_dilated(z, conv_taps[:, None, :], window_strides=(1,),
                                  padding=[(CONV_WIDTH - 1, 0)],
                                  dimension_numbers=('NWC', 'WIO', 'NWC'),
                                  feature_group_count=z.shape[-1])
    return (gate_b * zc) @ w_out


def swiglu(h, w1, w3, w2):
    return (jax.nn.silu(h @ w1) * (h @ w3)) @ w2


def moe_swiglu(h, w_router, b_router, w1, w3, w2):
    B_, S, D = h.shape
    T = B_ * S
    xf = h.reshape(T, D)
    logits = (xf @ w_router + b_router).astype(jnp.float32)
    top_val, top_idx = lax.top_k(logits, TOP_K)
    gate = jax.nn.softmax(top_val, axis=-1)
    n_assign = T * TOP_K
    e_flat = top_idx.reshape(-1)
    tok_flat = jnp.arange(n_assign) // TOP_K
    g_flat = gate.reshape(-1)
    order = jnp.argsort(e_flat)
    e_sorted = e_flat[order]
    counts = jnp.bincount(e_flat, length=N_EXPERTS)
    starts = jnp.cumsum(counts) - counts
    padded = (counts + MOE_ROW_BLOCK - 1) // MOE_ROW_BLOCK * MOE_ROW_BLOCK
    pends = jnp.cumsum(padded)
    pstarts = pends - padded
    dest = pstarts[e_sorted] + (jnp.arange(n_assign) - starts[e_sorted])
    n_blocks = -(-n_assign // MOE_ROW_BLOCK) + N_EXPERTS
    rows = n_blocks * MOE_ROW_BLOCK
    buf = jnp.zeros((rows, D), h.dtype).at[dest].set(xf[tok_flat[order]])
    row_tok = jnp.zeros((rows,), jnp.int32).at[dest].set(tok_flat[order].astype(jnp.int32))
    row_w = jnp.zeros((rows,), jnp.float32).at[dest].set(g_flat[order])
    blk_e = jnp.minimum(jnp.searchsorted(pends, jnp.arange(n_blocks) * MOE_ROW_BLOCK, side='right'), N_EXPERTS - 1)

    def expert_rows(args):
        xb, e = args
        return swiglu(xb, w1[e], w3[e], w2[e])

    yb = lax.map(expert_rows, (buf.reshape(n_blocks, MOE_ROW_BLOCK, D), blk_e))
    y = yb.reshape(rows, D) * row_w[:, None].astype(yb.dtype)
    return jax.ops.segment_sum(y, row_tok, num_segments=T).reshape(B_, S, D)


def setup_inputs(seed: int = 0) -> dict:
    key = jax.random.key(seed)
    ks = iter(jax.random.split(key, 40))
    nrm = lambda shape, scale: jax.random.normal(next(ks), shape, jnp.float32) * scale
    n_even = (DEPTH + 1) // 2
    n_odd = DEPTH // 2
    D = D_MODEL
    return {
        'x': nrm((BATCH, SEQ, D), 1.0),
        'c': nrm((BATCH, D), 1.0),
        'ada_w': nrm((DEPTH, D, 6 * D), 0.02),
        'ada_b': nrm((DEPTH, 6 * D), 0.02),
        'ln_g': 1.0 + nrm((DEPTH, 2, D), 0.02),
        'ln_b': nrm((DEPTH, 2, D), 0.02),
        'mix_w_in': nrm((n_even, D, MIX0_IN), D ** -0.5),
        'cmp_pos': nrm((n_even, 2, NSA_CMP_LEN, HEAD_DIM), 0.1),
        'cmp_w1': nrm((n_even, 2, NSA_CMP_LEN * HEAD_DIM, NSA_CMP_HIDDEN), (NSA_CMP_LEN * HEAD_DIM) ** -0.5),
        'cmp_w2': nrm((n_even, 2, NSA_CMP_HIDDEN, HEAD_DIM), NSA_CMP_HIDDEN ** -0.5),
        'mix_w_out': nrm((n_even, MIX0_OUT, D), MIX0_OUT ** -0.5 * DN_BETA),
        'ffn_w1': nrm((n_even, D, D_FF), D ** -0.5),
        'ffn_w3': nrm((n_even, D, D_FF), D ** -0.5),
        'ffn_w2': nrm((n_even, D_FF, D), D_FF ** -0.5 * DN_BETA),
        'conv_w_in': nrm((n_odd, D, 3 * D), D ** -0.5),
        'conv_taps': nrm((n_odd, CONV_WIDTH, D), CONV_WIDTH ** -0.5),
        'conv_w_out': nrm((n_odd, D, D), D ** -0.5 * DN_BETA),
        'router_w': nrm((n_odd, D, N_EXPERTS), D ** -0.5),
        'router_b': nrm((n_odd, N_EXPERTS), 0.01),
        'exp_w1': nrm((n_odd, N_EXPERTS, D, EXPERT_D_FF), D ** -0.5),
        'exp_w3': nrm((n_odd, N_EXPERTS, D, EXPERT_D_FF), D ** -0.5),
        'exp_w2': nrm((n_odd, N_EXPERTS, EXPERT_D_FF, D), EXPERT_D_FF ** -0.5 * DN_BETA),
    }


def reference(x, c, ada_w, ada_b, ln_g, ln_b, mix_w_in, cmp_pos, cmp_w1, cmp_w2, mix_w_out,
              ffn_w1, ffn_w3, ffn_w2, conv_w_in, conv_taps, conv_w_out, router_w, router_b,
              exp_w1, exp_w3, exp_w2):
    cond = jax.nn.silu(c)
    for i in range(DEPTH):
        j = i // 2
        mod = cond @ ada_w[i] + ada_b[i]
        sh1, sc1, g1, sh2, sc2, g2 = jnp.split(mod[:, None, :], 6, axis=-1)
        h = x * (1 + sc1) + sh1
        if i % 2 == 0:
            y = parallel_attention_mixer(h, mix_w_in[j], cmp_pos[j], cmp_w1[j], cmp_w2[j], mix_w_out[j])
        else:
            y = short_conv_mixer(h, conv_w_in[j], conv_taps[j], conv_w_out[j])
        x = layer_norm(DN_ALPHA * x + (1 + g1) * y, ln_g[i, 0], ln_b[i, 0])
        h = x * (1 + sc2) + sh2
        if i % 2 == 0:
            y = swiglu(h, ffn_w1[j], ffn_w3[j], ffn_w2[j])
        else:
            y = moe_swiglu(h, router_w[j], router_b[j], exp_w1[j], exp_w3[j], exp_w2[j])
        x = layer_norm(DN_ALPHA * x + (1 + g2) * y, ln_g[i, 1], ln_b[i, 1])
    return x
```

```python
import numpy as np
from contextlib import ExitStack
import concourse.bass as bass
import concourse.mybir as mybir
from concourse.bass_utils import run_bass_kernel_spmd

F32 = mybir.dt.float32
BF16 = mybir.dt.bfloat16
AF = mybir.ActivationFunctionType
ALU = mybir.AluOpType
AX = mybir.AxisListType

NCORES = 8
D = 1024
SEQ = 2048
NBC = 2
TOK = NBC * SEQ
DFF = 2816
NF = DFF // 128
NEXP = 8
ALPHA = 4.0 ** 0.25
EPS = 1e-5
NEGB = -1024.0
MIXIN = 2840
MOE_BS = 512
MOE_NBLK = (TOK * 2) // MOE_BS + NEXP
NSLOT = MOE_NBLK * MOE_BS
I32 = mybir.dt.int32
ENGS = ("pe", "act", "dve", "pool", "sp")
DBG = {}


class T:
    __slots__ = ("h", "w", "r", "name")

    def __init__(self, h, name=""):
        self.h = h
        self.w = None
        self.r = {}
        self.name = name

    def __getitem__(self, k):
        return self.h[k]


class Sched:
    def __init__(self, nc, n_dma_sems=48):
        self.nc = nc
        self.streams = {e: [] for e in ENGS}
        self.cnt = {e: 0 for e in ENGS}
        self.seen = {e: {} for e in ENGS}
        self.n_dma = n_dma_sems
        self.dma_cnt = [0] * n_dma_sems
        self.dma_rr = 0
        self.sw_rr = 0
        self.n_hw = n_dma_sems - 16
        self.n_ops = 0
        self.n_waits = 0

    def _wait(self, eng, dep):
        key, val = dep
        if eng == "pe" and key == ("E", "pe"):
            return
        if self.seen[eng].get(key, 0) >= val:
            return
        self.seen[eng][key] = val
        self.streams[eng].append(("wait", key, val))
        self.n_waits += 1

    def _deps(self, eng, reads, writes):
        for t in reads:
            if t.w is not None:
                self._wait(eng, t.w)
        for t in writes:
            if t.w is not None:
                self._wait(eng, t.w)
            for k, v in t.r.items():
                self._wait(eng, (k, v))

    def _mark(self, me, reads, writes):
        k, v = me
        for t in reads:
            if t.r.get(k, 0) < v:
                t.r[k] = v
        for t in writes:
            t.w = me
            t.r = {}

    def op(self, eng, fn, reads=(), writes=()):
        self._deps(eng, reads, writes)
        self.cnt[eng] += 1
        me = (("E", eng), self.cnt[eng])
        self.streams[eng].append(("op", fn, ("E", eng), 1))
        self._mark(me, reads, writes)
        self.n_ops += 1

    def dma(self, q, out_ap, in_ap, reads=(), writes=(), **kw):
        if q == "pool":
            i = self.n_hw + self.sw_rr
            self.sw_rr = (self.sw_rr + 1) % (self.n_dma - self.n_hw)
        else:
            i = self.dma_rr
            self.dma_rr = (i + 1) % self.n_hw
        if self.dma_cnt[i] > 0:
            self._wait(q, (("D", i), self.dma_cnt[i]))
        self._deps(q, reads, writes)
        self.dma_cnt[i] += 16
        me = (("D", i), self.dma_cnt[i])
        self.streams[q].append(("op", lambda e: e.dma_start(out=out_ap, in_=in_ap, **kw), ("D", i), 16))
        self._mark(me, reads, writes)
        self.n_ops += 1

    def dma_fn(self, q, fn, reads=(), writes=()):
        if q == "pool":
            i = self.n_hw + self.sw_rr
            self.sw_rr = (self.sw_rr + 1) % (self.n_dma - self.n_hw)
        else:
            i = self.dma_rr
            self.dma_rr = (i + 1) % self.n_hw
        if self.dma_cnt[i] > 0:
            self._wait(q, (("D", i), self.dma_cnt[i]))
        self._deps(q, reads, writes)
        self.dma_cnt[i] += 16
        me = (("D", i), self.dma_cnt[i])
        self.streams[q].append(("op", fn, ("D", i), 16))
        self._mark(me, reads, writes)
        self.n_ops += 1

    def barrier(self):
        for e in ENGS:
            for e2 in ENGS:
                if e2 != e and self.cnt[e2] > 0:
                    self._wait(e, (("E", e2), self.cnt[e2]))
            for i in range(self.n_dma):
                if self.dma_cnt[i] > 0:
                    self._wait(e, (("D", i), self.dma_cnt[i]))

    def emit(self, stack):
        nc = self.nc
        sems = {}
        for e in ENGS:
            sems[("E", e)] = stack.enter_context(nc.semaphore("s_" + e))
        for i in range(self.n_dma):
            sems[("D", i)] = stack.enter_context(nc.semaphore("d_%d" % i))
        block = stack.enter_context(nc.Block())

        def run(engh, items):
            for it in items:
                if it[0] == "wait":
                    engh.wait_ge(sems[it[1]], it[2])
                else:
                    it[1](engh).then_inc(sems[it[2]], it[3])

        @block.tensor
        def _(e):
            run(e, self.streams["pe"])

        @block.scalar
        def _(e):
            run(e, self.streams["act"])

        @block.vector
        def _(e):
            run(e, self.streams["dve"])

        @block.gpsimd
        def _(e):
            run(e, self.streams["pool"])

        @block.sync
        def _(e):
            run(e, self.streams["sp"])


class K:
    SB_BASE = 16512
    SB_LIMIT = 229376

    def __init__(self, nc):
        self.nc = nc
        self.S = Sched(nc)
        self.pers_off = self.SB_BASE
        self.stage_base = self.SB_BASE
        self.off = self.SB_BASE
        self.uid = 0
        self.ps = [T(nc.alloc_psum_tensor("psb%d" % i, [128, 512], F32), "ps%d" % i) for i in range(8)]

    def _alloc(self, name, shape, dt, off):
        self.uid += 1
        h = self.nc.alloc_sbuf_tensor_at("%s_%d" % (name, self.uid), shape, dt, offset=off)
        return h

    @staticmethod
    def _bytes(shape, dt):
        n = 1
        for s in shape[1:]:
            n *= s
        b = n * (2 if dt == BF16 else 4)
        return (b + 31) // 32 * 32

    def pers(self, name, shape, dt):
        assert self.off == self.stage_base, "persistent alloc only between stages"
        h = self._alloc(name, shape, dt, self.pers_off)
        self.pers_off += self._bytes(shape, dt)
        self.stage_base = self.off = self.pers_off
        return T(h, name)

    def sb(self, name, shape, dt):
        h = self._alloc(name, shape, dt, self.off)
        self.off += self._bytes(shape, dt)
        assert self.off <= self.SB_LIMIT, "SBUF overflow at %s: %d" % (name, self.off)
        return T(h, name)

    def view(self, t, name=""):
        return T(t.h, name or t.name)

    def end_stage(self):
        self.S.barrier()
        self.off = self.stage_base
        for p in self.ps:
            p.w = None
            p.r = {}

    def mm(self, ot, o_ap, lt, l_ap, rt, r_ap, start=True, stop=True, skip=False):
        if skip:
            self.S.op("pe", lambda e: e.matmul(o_ap, lhsT=l_ap, rhs=r_ap, start=start, stop=stop, skip_group_check=True),
                      reads=[lt, rt], writes=[ot])
        else:
            self.S.op("pe", lambda e: e.matmul(o_ap, lhsT=l_ap, rhs=r_ap, start=start, stop=stop),
                      reads=[lt, rt], writes=[ot])

    def tr(self, ot, o_ap, it, i_ap, idt, id_ap):
        self.S.op("pe", lambda e: e.transpose(o_ap, i_ap, id_ap), reads=[it, idt], writes=[ot])

    def act(self, ot, o_ap, it, i_ap, func, bias=None, scale=None, extra_reads=(), eng="act"):
        kw = {}
        if bias is not None:
            kw["bias"] = bias
        if scale is not None:
            kw["scale"] = scale
        self.S.op("act", lambda e: e.activation(out=o_ap, in_=i_ap, func=func, **kw),
                  reads=[it] + list(extra_reads), writes=[ot])

    def tt(self, ot, o_ap, at, a_ap, bt, b_ap, op, eng="dve"):
        self.S.op(eng, lambda e: e.tensor_tensor(out=o_ap, in0=a_ap, in1=b_ap, op=op),
                  reads=[at, bt], writes=[ot])

    def ts(self, ot, o_ap, at, a_ap, s1, s2, op0, op1=None, extra_reads=(), eng="dve"):
        if op1 is None:
            self.S.op(eng, lambda e: e.tensor_scalar(out=o_ap, in0=a_ap, scalar1=s1, scalar2=None, op0=op0),
                      reads=[at] + list(extra_reads), writes=[ot])
        else:
            self.S.op(eng, lambda e: e.tensor_scalar(out=o_ap, in0=a_ap, scalar1=s1, scalar2=s2, op0=op0, op1=op1),
                      reads=[at] + list(extra_reads), writes=[ot])

    def stt(self, ot, o_ap, at, a_ap, scalar, bt, b_ap, op0, op1, extra_reads=(), eng="dve"):
        self.S.op(eng, lambda e: e.scalar_tensor_tensor(out=o_ap, in0=a_ap, scalar=scalar, in1=b_ap, op0=op0, op1=op1),
                  reads=[at, bt] + list(extra_reads), writes=[ot])

    def cp(self, ot, o_ap, it, i_ap, eng="dve"):
        if eng == "act":
            self.S.op("act", lambda e: e.copy(out=o_ap, in_=i_ap), reads=[it], writes=[ot])
        else:
            self.S.op(eng, lambda e: e.tensor_copy(out=o_ap, in_=i_ap), reads=[it], writes=[ot])

    def memset(self, ot, o_ap, val, eng="dve"):
        self.S.op(eng, lambda e: e.memset(o_ap, val), reads=[], writes=[ot])

    def dma(self, q, o_ap, i_ap, reads=(), writes=(), **kw):
        self.S.dma(q, o_ap, i_ap, reads=reads, writes=writes, **kw)

    def gather(self, o_ap, src_ap, idx_ap, reads=(), writes=(), bounds=None):
        if bounds is None:
            self.S.dma_fn("pool", lambda e: e.indirect_dma_start(
                out=o_ap, out_offset=None, in_=src_ap, in_offset=bass.IndirectOffsetOnAxis(ap=idx_ap, axis=0)),
                reads=reads, writes=writes)
        else:
            regs = self.__dict__.setdefault("_bound_regs", {})

            def fn(e):
                if bounds not in regs:
                    rg = e.alloc_register("bnd%d" % bounds)
                    e.reg_mov(rg, bounds)
                    regs[bounds] = rg
                return e.indirect_dma_start(
                    out=o_ap, out_offset=None, in_=src_ap, in_offset=bass.IndirectOffsetOnAxis(ap=idx_ap, axis=0),
                    bounds_check=regs[bounds], oob_is_err=False)
            self.S.dma_fn("pool", fn, reads=reads, writes=writes)

    def scatter(self, dst_ap, i_ap, idx_ap, reads=(), writes=()):
        self.S.dma_fn("pool", lambda e: e.indirect_dma_start(
            out=dst_ap, out_offset=bass.IndirectOffsetOnAxis(ap=idx_ap, axis=0), in_=i_ap, in_offset=None),
            reads=reads, writes=writes)


def host_consts():
    s = np.arange(128)[:, None]
    c = np.arange(512)[None, :]
    cst = {}
    cst["ident"] = np.eye(128, dtype=np.float32)
    cst["cb"] = np.where(c >= s, 0.0, NEGB).astype(np.float32)
    cst["cbs"] = np.where(c > s, 0.0, NEGB).astype(np.float32)
    cst["wb"] = np.where(c - 384 < s, 0.0, NEGB).astype(np.float32)
    j = np.arange(128)[:, None]
    ss = np.arange(128)[None, :]
    cst["negtri"] = np.where(j >= ss, -1.0, 0.0).astype(np.float32)
    cst["negones"] = -np.ones((128, 128), np.float32)
    n = np.arange(128)[:, None]
    t = np.arange(SEQ)[None, :]
    cmpb = np.where(16 * n + 31 <= t, 0.0, NEGB).astype(np.float32)
    cmpb[127, :] = NEGB
    cst["cmpbias"] = cmpb
    cmp_start = np.arange(127) * 16
    slc_start = np.arange(32) * 64
    ov = ((cmp_start[:, None] < slc_start[None, :] + 64) & (cmp_start[:, None] + 32 > slc_start[None, :]))
    ovp = np.zeros((128, 32), np.float32)
    ovp[:127] = ov.astype(np.float32)
    cst["overlap"] = ovp
    ex = np.zeros((32, 16, 128), np.float32)
    for kb in range(16):
        for sl in range(128):
            ex[2 * kb + sl // 64, kb, sl] = 1.0
    cst["expand"] = ex
    tt = np.arange(SEQ)
    blk = np.arange(32)[None, :]
    cur = (tt // 64)[:, None]
    valid = slc_start[None, :] <= tt[:, None]
    forced = (blk == 0) | (blk == cur) | (blk == cur - 1)
    A = (valid & ~forced).astype(np.float32)
    Bm = np.where(valid, np.where(forced, 1e30, 0.0), -1e30).astype(np.float32)
    cst["selA"] = A.reshape(16, 128, 32).transpose(1, 0, 2).copy()
    cst["selB"] = Bm.reshape(16, 128, 32).transpose(1, 0, 2).copy()
    r_ = np.arange(128)[:, None]
    c_ = np.arange(128)[None, :]
    cst["trilt"] = (r_ < c_).astype(np.float32)
    cst["ones"] = np.ones((128, 128), np.float32)
    cst["pidx"] = np.arange(128, dtype=np.float32).reshape(128, 1)
    cst["thr"] = np.tile((np.arange(MOE_NBLK + 8) * float(MOE_BS))[None, :], (128, 1)).astype(np.float32)
    return cst


CONST_SHAPES = {
    "ident": [128, 128], "cb": [128, 512], "cbs": [128, 512], "wb": [128, 512],
    "negtri": [128, 128], "negones": [128, 128], "cmpbias": [128, SEQ], "overlap": [128, 32],
    "expand": [32, 16, 128], "selA": [128, 16, 32], "selB": [128, 16, 32],
    "trilt": [128, 128], "ones": [128, 128], "pidx": [128, 1], "thr": [128, MOE_NBLK + 8],
}

INPUT_SHAPES = {
    "x": [TOK, D], "c": [NBC, D],
    "ada_w": [2, D, 6 * D], "ada_b": [2, 6 * D], "ln_g": [2, 2, D], "ln_b": [2, 2, D],
    "mix_w_in": [1, D, MIXIN], "cmp_pos": [1, 2, 32, 64], "cmp_w1": [1, 2, 2048, 256],
    "cmp_w2": [1, 2, 256, 64], "mix_w_out": [1, D, D],
    "ffn_w1": [1, D, DFF], "ffn_w3": [1, D, DFF], "ffn_w2": [1, DFF, D],
    "conv_w_in": [1, D, 3 * D], "conv_taps": [1, 3, D], "conv_w_out": [1, D, D],
    "router_w": [1, D, NEXP], "router_b": [1, NEXP],
    "exp_w1": [1, NEXP, 2 * D, DFF // 2], "exp_w3": [1, NEXP, 2 * D, DFF // 2], "exp_w2": [1, NEXP, DFF, D],
}

NQK = 18
VT_W = 768


def build_program(stages=("s0", "s1", "s2a", "s2", "s3", "s4", "s5", "s6"), dbg=(), feed=()):
    nc = bass.Bass("TRN2", target_bir_lowering=False)
    k = K(nc)
    S = k.S
    I = {n: nc.dram_tensor(n, shp, F32, kind="ExternalInput").ap() for n, shp in INPUT_SHAPES.items()}
    C = {n: nc.dram_tensor("c_" + n, shp, F32, kind="ExternalInput").ap() for n, shp in CONST_SHAPES.items()}
    out_d = nc.dram_tensor("out", [TOK, D], F32, kind="ExternalOutput").ap()

    def scratch(name, shape, dt):
        kind = "ExternalOutput" if name in dbg else ("ExternalInput" if name in feed else "Internal")
        return nc.dram_tensor(name, shape, dt, kind=kind).ap()

    mod_d = scratch("mod_d", [2, NBC, 6 * D], F32)
    qkT_d = scratch("qkT_d", [NBC, NQK, 128, SEQ], BF16)
    vtok_d = scratch("vtok_d", [NBC, SEQ, VT_W], BF16)
    gates_d = scratch("gates_d", [NBC, SEQ, 24], F32)
    kcT_d = scratch("kcT_d", [NBC, 2, 128, 128], BF16)
    vc_d = scratch("vc_d", [NBC, 2, 128, 64], BF16)
    oT_d = scratch("oT_d", [NBC, 8, 128, SEQ], BF16)
    x1_d = scratch("x1_d", [TOK, D], F32)
    x2_d = scratch("x2_d", [TOK, D], F32)
    x3_d = scratch("x3_d", [TOK, D], F32)
    h2T_d = scratch("h2T_d", [8, 128, TOK], BF16)
    acc_d = scratch("acc_d", [TOK, D], F32)
    dt_ = {n: T(None, n) for n in ("mod", "qkT", "vtok", "gates", "kcT", "vc", "oT", "x1", "x2", "x3", "h2T", "acc", "out")}
    dummy_in = T(None, "in")

    ident = k.pers("ident", [128, 128], F32)
    identb = k.pers("identb", [128, 128], BF16)
    modT = [k.pers("modT%d" % l, [128, 48, NBC], F32) for l in range(2)]
    modP = [k.pers("modP%d" % l, [128, 48, NBC], F32) for l in range(2)]
    gw = k.pers("gw", [128, TOK // 128, NEXP], F32)
    k.dma("sp", ident[:, :], C["ident"], writes=[ident])
    k.dma("pool", identb[:, :], C["ident"], writes=[identb])

    PS = k.ps

    def load_bc_row(name, src_row_ap, q="sp", add_one=False):
        t = name if isinstance(name, T) else k.sb(name, [128, D], F32)
        k.dma(q, t[:, :], src_row_ap.partition_broadcast(128), writes=[t])
        if add_one:
            k.ts(t, t[:, :], t, t[:, :], 1.0, None, ALU.add)
        return t

    def transpose_mod(xts, hT, l, b, sh_kind, sc_kind, psel, hT32=None):
        for c in range(8):
            p = PS[psel[c % len(psel)]]
            for j in range(4):
                k.tr(p, p[:, j * 128:(j + 1) * 128], xts[j], xts[j][:, c * 128:(c + 1) * 128], ident, ident[:, :])
            if hT is not None:
                k.act(hT, hT[:, c, :], p, p[:, :], AF.Identity,
                      bias=modT[l][:, sh_kind * 8 + c, b:b + 1], scale=modP[l][:, sc_kind * 8 + c, b:b + 1],
                      extra_reads=[modT[l], modP[l]])
            if hT32 is not None:
                k.act(hT32, hT32[:, c, :], p, p[:, :], AF.Identity,
                      bias=modT[l][:, sh_kind * 8 + c, b:b + 1], scale=modP[l][:, sc_kind * 8 + c, b:b + 1],
                      extra_reads=[modT[l], modP[l]])

    def resid_ln(xt, yps, gate_bc, lng, lnb, tmp, r, outt, stats, mv):
        if isinstance(yps, T):
            k.tt(tmp, tmp[:, :], yps, yps[:, :], gate_bc, gate_bc[:, :], ALU.mult)
        else:
            for h in range(2):
                k.tt(tmp, tmp[:, h * 512:(h + 1) * 512], yps[h], yps[h][:, :], gate_bc, gate_bc[:, h * 512:(h + 1) * 512], ALU.mult)
        k.stt(r, r[:, :], xt, xt[:, :], ALPHA, tmp, tmp[:, :], ALU.mult, ALU.add)
        for h in range(2):
            S.op("dve", (lambda hh: (lambda e: e.bn_stats(out=stats[:, hh, :], in_=r[:, hh * 512:(hh + 1) * 512])))(h),
                 reads=[r], writes=[stats])
        S.op("dve", lambda e: e.bn_aggr(out=mv[:, 0:2], in_=stats[:, :, :].rearrange("p a b -> p (a b)")),
             reads=[stats], writes=[mv])
        k.ts(mv, mv[:, 2:3], mv, mv[:, 1:2], EPS, None, ALU.add)
        S.op("pool", lambda e: e.tensor_tensor(out=mv[:, 3:4], in0=mv[:, 2:3], in1=mv[:, 4:5], op=ALU.pow),
             reads=[mv], writes=[mv])
        k.ts(tmp, tmp[:, :], r, r[:, :], mv[:, 0:1], mv[:, 3:4], ALU.subtract, ALU.mult, extra_reads=[mv])
        k.tt(tmp, tmp[:, :], tmp, tmp[:, :], lng, lng[:, :], ALU.mult)
        k.tt(outt, outt[:, :], tmp, tmp[:, :], lnb, lnb[:, :], ALU.add)

    def new_mv(name):
        mv = k.sb(name, [128, 8], F32)
        k.memset(mv, mv[:, :], -0.5)
        return mv

    if "s0" in stages:
        c_s = k.sb("c_s", [NBC, D], F32)
        cond_s = k.sb("cond_s", [NBC, D], F32)
        condT = k.sb("condT", [128, 8, NBC], BF16)
        k.dma("sp", c_s[:, :], I["c"], writes=[c_s])
        k.act(cond_s, cond_s[:, :], c_s, c_s[:, :], AF.Silu)
        for c in range(8):
            k.tr(PS[0], PS[0][:, c * NBC:(c + 1) * NBC], cond_s, cond_s[:, c * 128:(c + 1) * 128], ident, ident[0:NBC, 0:NBC])
        k.cp(condT, condT[:, :, :].rearrange("p a b -> p (a b)"), PS[0], PS[0][:, 0:8 * NBC])
        wbuf = [k.sb("adaw%d" % i, [128, 8, 512], BF16) for i in range(2)]
        mod_s = k.sb("mod_s", [NBC, 6 * D], F32)
        adab = k.sb("adab", [NBC, 6 * D], F32)
        for l in range(2):
            k.dma("sp", adab[:, :], I["ada_b"][l, :].partition_broadcast(NBC), writes=[adab])
            for blk in range(12):
                wb = wbuf[blk % 2]
                src = I["ada_w"][l].rearrange("(kc p) n -> p kc n", p=128)[:, :, blk * 512:(blk + 1) * 512]
                k.dma("pool", wb[:, :, :], src, writes=[wb])
                p = PS[1 + blk % 2]
                for kc in range(8):
                    k.mm(p, p[0:NBC, :], condT, condT[:, kc, :], wb, wb[:, kc, :], start=(kc == 0), stop=(kc == 7))
                k.tt(mod_s, mod_s[:, blk * 512:(blk + 1) * 512], p, p[0:NBC, :], adab, adab[:, blk * 512:(blk + 1) * 512], ALU.add)
            k.dma("sp", mod_d[l], mod_s[:, :], reads=[mod_s])
            for ch in range(48):
                k.tr(PS[3], PS[3][:, ch * NBC:(ch + 1) * NBC], mod_s, mod_s[:, ch * 128:(ch + 1) * 128], ident, ident[0:NBC, 0:NBC])
            k.cp(modT[l], modT[l][:, :, :].rearrange("p a b -> p (a b)"), PS[3], PS[3][:, 0:48 * NBC])
            k.ts(modP[l], modP[l][:, :, :].rearrange("p a b -> p (a b)"), modT[l],
                 modT[l][:, :, :].rearrange("p a b -> p (a b)"), 1.0, None, ALU.add)
        k.end_stage()

    if "s1" in stages:
        win = k.sb("win", [128, 8, MIXIN], BF16)
        wdup = k.sb("wdup", [128, 8, 512], BF16)
        wsrc = I["mix_w_in"][0].rearrange("(kc p) n -> p kc n", p=128)
        for kc in range(8):
            k.dma("pool", win[:, kc, :], wsrc[:, kc, :], writes=[win])
        for i, col in enumerate((2304, 2368, 2560, 2624)):
            for rep in range(2):
                k.dma("pool", wdup[:, :, i * 128 + rep * 64: i * 128 + rep * 64 + 64], wsrc[:, :, col:col + 64], writes=[wdup])
        xts = [[k.sb("xt%d_%d" % (i, j), [128, D], F32) for j in range(4)] for i in range(2)]
        hTs = [k.sb("hT%d" % i, [128, 8, 512], BF16) for i in range(2)]
        fm = [k.sb("fm%d" % i, [128, 512], BF16) for i in range(4)]
        vst = [k.sb("vst%d" % i, [128, VT_W], BF16) for i in range(2)]
        gst = [k.sb("gst%d" % i, [128, 24], F32) for i in range(2)]
        fm_i = 0
        v_i = 0
        chunks = []
        for c in range(4):
            chunks.append((win, 0 + c * 128, 1.0))
        for c in range(4):
            chunks.append((win, 512 + c * 128, 0.125))
        for c in range(4):
            chunks.append((win, 1536 + c * 128, 1.0))
        chunks.append((win, 2048, 1.0))
        chunks.append((win, 2176, 1.0))
        for c in range(4):
            chunks.append((wdup, c * 128, 0.125))
        for g in range(DBG.get("s1_groups", TOK // 512)):
            b = g // 4
            t0 = (g % 4) * 512
            xt = xts[g % 2]
            hT = hTs[g % 2]
            for j in range(4):
                k.dma("sp", xt[j][:, :], I["x"][g * 512 + j * 128: g * 512 + (j + 1) * 128, :], writes=[xt[j]])
            transpose_mod(xt, hT, 0, b, 0, 1, (0, 1))
            for ci, (wt, col0, scl) in enumerate(chunks if DBG.get("s1_fm", True) else []):
                p = PS[2 + ci % 3]
                for kc in range(8):
                    k.mm(p, p[:, :], wt, wt[:, kc, col0:col0 + 128], hT, hT[:, kc, :], start=(kc == 0), stop=(kc == 7))
                f = fm[fm_i % 4]
                fm_i += 1
                if ci % 2 == 0:
                    k.act(f, f[:, :], p, p[:, :], AF.Identity, scale=scl)
                else:
                    k.ts(f, f[:, :], p, p[:, :], scl, None, ALU.mult)
                k.dma("sp", qkT_d[b, ci, :, t0:t0 + 512], f[:, :], reads=[f])
            for j in range(4 if DBG.get("s1_tm", True) else 0):
                pv, pg = PS[5 + (j % 2)], PS[7]
                for kc in range(8):
                    k.mm(pv, pv[:, :], hT, hT[:, kc, j * 128:(j + 1) * 128], win, win[:, kc, 1024:1536], start=(kc == 0), stop=(kc == 7))
                if DBG.get("pg1", True):
                    for kc in range(8):
                        k.mm(pg, pg[:, 0:128], hT, hT[:, kc, j * 128:(j + 1) * 128], win, win[:, kc, 2432:2560], start=(kc == 0), stop=(kc == 7))
                if DBG.get("pg2", True):
                    for kc in range(8):
                        k.mm(pg, pg[:, 128:256], hT, hT[:, kc, j * 128:(j + 1) * 128], win, win[:, kc, 2688:2816], start=(kc == 0), stop=(kc == 7))
                    for kc in range(8):
                        k.mm(pg, pg[:, 256:280], hT, hT[:, kc, j * 128:(j + 1) * 128], win, win[:, kc, 2816:2840], start=(kc == 0), stop=(kc == 7))
                vs = vst[v_i % 2]
                gs = gst[v_i % 2]
                v_i += 1
                k.cp(vs, vs[:, 0:512], pv, pv[:, :], eng="dve")
                if DBG.get("pgc", True):
                    k.cp(vs, vs[:, 512:768], pg, pg[:, 0:256], eng=DBG.get("pgc_eng", "act"))
                if DBG.get("sig", True):
                    k.act(gs, gs[:, :], pg, pg[:, 256:280], AF.Sigmoid)
                tok = t0 + j * 128
                k.dma("sp", vtok_d[b, tok:tok + 128, :], vs[:, :], reads=[vs])
                if DBG.get("gdma", True):
                    k.dma("sp", gates_d[b, tok:tok + 128, :], gs[:, :], reads=[gs])
        k.end_stage()

    if "s2a" in stages:
        w1 = [k.sb("cw1_%d" % i, [128, 32, 256], BF16) for i in range(2)]
        w2k = k.sb("cw2k", [128, 2, 128], BF16)
        w2v = k.sb("cw2v", [128, 2, 64], BF16)
        posT = [k.sb("posT%d" % i, [128, 32], F32) for i in range(2)]
        pos_s = [k.sb("pos_s%d" % i, [32, 128], F32) for i in range(2)]
        for kv in range(2):
            src = I["cmp_w1"][0, kv].rearrange("(l d) h -> d l h", d=64)
            for half in range(2):
                k.dma("pool", w1[kv][half * 64:(half + 1) * 64, :, :], src, writes=[w1[kv]])
            for half in range(2):
                k.dma("sp", pos_s[kv][:, half * 64:(half + 1) * 64], I["cmp_pos"][0, kv], writes=[pos_s[kv]])
            k.tr(PS[0], PS[0][:, kv * 32:(kv + 1) * 32], pos_s[kv], pos_s[kv][:, :], ident, ident[0:32, 0:32])
            k.cp(posT[kv], posT[kv][:, :], PS[0], PS[0][:, kv * 32:(kv + 1) * 32])
        w2ksrc = I["cmp_w2"][0, 0].rearrange("(hc p) d -> p hc d", p=128)
        for rep in range(2):
            k.dma("pool", w2k[:, :, rep * 64:(rep + 1) * 64], w2ksrc, writes=[w2k])
        k.dma("pool", w2v[:, :, :], I["cmp_w2"][0, 1].rearrange("(hc p) d -> p hc d", p=128), writes=[w2v])
        src_t = [k.sb("cmpsrc%d" % i, [128, SEQ], BF16) for i in range(2)]
        kp = [k.sb("kp%d" % i, [128, 32, 128], BF16) for i in range(2)]
        HsT = [k.sb("HsT%d" % i, [128, 2, 128], BF16) for i in range(2)]
        kc_s = [k.sb("kc_s%d" % i, [128, 128], BF16) for i in range(2)]
        vc_s = [k.sb("vc_s%d" % i, [128, 64], BF16) for i in range(2)]
        it = 0
        for b in range(NBC):
            for kv in range(2):
                st = src_t[it % 2]
                kpt = kp[it % 2]
                it += 1
                k.dma("sp", st[:, :], qkT_d[b, 12 + kv, :, :], writes=[st])
                for l in range(32):
                    k.act(kpt, kpt[:, l, 0:127], st, st[:, l:l + 16 * 126 + 1:16], AF.Identity,
                          bias=posT[kv][:, l:l + 1], extra_reads=[posT[kv]])
                for g in range(2):
                    hs = HsT[g]
                    for hc in range(2):
                        p = PS[1 + hc]
                        for l in range(32):
                            k.mm(p, p[:, 0:127], w1[kv], w1[kv][g * 64:(g + 1) * 64, l, hc * 128:(hc + 1) * 128],
                                 kpt, kpt[g * 64:(g + 1) * 64, l, 0:127], start=(l == 0), stop=(l == 31))
                        k.act(hs, hs[:, hc, 0:127], p, p[:, 0:127], AF.Silu)
                    if kv == 0:
                        p = PS[3]
                        for hc in range(2):
                            k.mm(p, p[:, 0:127], w2k, w2k[:, hc, :], hs, hs[:, hc, 0:127], start=(hc == 0), stop=(hc == 1))
                        kcs = kc_s[g]
                        k.memset(kcs, kcs[:, 96:128], 0.0)
                        k.act(kcs, kcs[:, 0:127], p, p[:, 0:127], AF.Identity, scale=0.125)
                        k.dma("sp", kcT_d[b, g], kcs[:, :], reads=[kcs])
                    else:
                        p = PS[4]
                        for hc in range(2):
                            k.mm(p, p[0:127, 0:64], hs, hs[:, hc, 0:127], w2v, w2v[:, hc, :], start=(hc == 0), stop=(hc == 1))
                        vcs = vc_s[g]
                        k.memset(vcs, vcs[:, :], 0.0)
                        k.cp(vcs, vcs[0:127, :], p, p[0:127, 0:64])
                        k.dma("sp", vc_d[b, g], vcs[:, :], reads=[vcs])
        k.end_stage()

    if "s2" in stages:
        stage_attention(k, nc, I, C, dt_, ident, identb, qkT_d, vtok_d, gates_d, kcT_d, vc_d, oT_d)
        k.end_stage()

    if "s3" in stages:
        wout = k.sb("wout", [128, 8, D], BF16)
        k.dma("pool", wout[:, :, :], I["mix_w_out"][0].rearrange("(kc p) n -> p kc n", p=128), writes=[wout])
        lng = load_bc_row("lng", I["ln_g"][0, 0, :])
        lnb = load_bc_row("lnb", I["ln_b"][0, 0, :])
        gbc = [load_bc_row("gbc%d" % b, mod_d[0, b, 2 * D:3 * D], add_one=True) for b in range(NBC)]
        oTs = [k.sb("oTs%d" % i, [128, 8, 512], BF16) for i in range(2)]
        xts = [k.sb("xt%d" % i, [128, D], F32) for i in range(3)]
        outs = [k.sb("xo%d" % i, [128, D], F32) for i in range(2)]
        tmp = k.sb("tmp", [128, D], F32)
        r = k.sb("r", [128, D], F32)
        stats = k.sb("stats", [128, 2, 6], F32)
        mv = new_mv("mv")
        ti = 0
        for g in range(TOK // 512):
            b = g // 4
            t0 = (g % 4) * 512
            oT = oTs[g % 2]
            k.dma("sp", oT[:, :, :], oT_d[b, :, :, t0:t0 + 512].rearrange("c p t -> p c t"), writes=[oT])
            for j in range(4):
                xt = xts[ti % 3]
                ot = outs[ti % 2]
                tok = g * 512 + j * 128
                k.dma("sp", xt[:, :], I["x"][tok:tok + 128, :], writes=[xt])
                yps = (PS[(ti % 2) * 2], PS[(ti % 2) * 2 + 1])
                for h in range(2):
                    for kc in range(8):
                        k.mm(yps[h], yps[h][:, :], oT, oT[:, kc, j * 128:(j + 1) * 128], wout, wout[:, kc, h * 512:(h + 1) * 512],
                             start=(kc == 0), stop=(kc == 7))
                resid_ln(xt, yps, gbc[b], lng, lnb, tmp, r, ot, stats, mv)
                k.dma("sp", x1_d[tok:tok + 128, :], ot[:, :], reads=[ot])
                ti += 1
        k.end_stage()

    def ffn_weights(w1src, w3src, w2src):
        w1 = k.sb("fw1", [128, 8, DFF], BF16)
        w3 = k.sb("fw3", [128, 8, DFF], BF16)
        w2 = k.sb("fw2", [128, NF, D], BF16)
        return w1, w3, w2

    def ffn_load(w1, w3, w2, w1src, w3src, w2src):
        s1 = w1src.rearrange("(kc p) n -> p kc n", p=128)
        s3 = w3src.rearrange("(kc p) n -> p kc n", p=128)
        s2 = w2src.rearrange("(f p) n -> p f n", p=128)
        for kc in range(8):
            k.dma("pool", w1[:, kc, :], s1[:, kc, :], writes=[w1])
            k.dma("pool", w3[:, kc, :], s3[:, kc, :], writes=[w3])
        for f0 in range(0, NF, 4):
            f1 = min(NF, f0 + 4)
            k.dma("pool", w2[:, f0:f1, :], s2[:, f0:f1, :], writes=[w2])

    def ffn_up(hT, w1, w3, gT, sa_bufs, cnt):
        for f in range(NF):
            pa = PS[(cnt[0] % 2) * 2]
            pb = PS[(cnt[0] % 2) * 2 + 1]
            sa = sa_bufs[cnt[0] % 2]
            cnt[0] += 1
            for kc in range(8):
                k.mm(pa, pa[:, :], w1, w1[:, kc, f * 128:(f + 1) * 128], hT, hT[:, kc, :], start=(kc == 0), stop=(kc == 7))
            for kc in range(8):
                k.mm(pb, pb[:, :], w3, w3[:, kc, f * 128:(f + 1) * 128], hT, hT[:, kc, :], start=(kc == 0), stop=(kc == 7))
            k.act(sa, sa[:, :], pa, pa[:, :], AF.Silu)
            k.tt(gT, gT[:, f, :], sa, sa[:, :], pb, pb[:, :], ALU.mult)

    def ffn_down(gT, w2, j, yps):
        for h in range(2):
            for f in range(NF):
                k.mm(yps[h], yps[h][:, :], gT, gT[:, f, j * 128:(j + 1) * 128], w2, w2[:, f, h * 512:(h + 1) * 512],
                     start=(f == 0), stop=(f == NF - 1))

    if "s4" in stages:
        w1, w3, w2 = ffn_weights(None, None, None)
        ffn_load(w1, w3, w2, I["ffn_w1"][0], I["ffn_w3"][0], I["ffn_w2"][0])
        lng = load_bc_row("lng", I["ln_g"][0, 1, :])
        lnb = load_bc_row("lnb", I["ln_b"][0, 1, :])
        gbc1 = k.sb("gbc", [128, D], F32)
        xts = [k.sb("xt%d" % j, [128, D], F32) for j in range(4)]
        hT = k.sb("hT", [128, 8, 512], BF16)
        gT = k.sb("gT", [128, NF, 512], BF16)
        sa_bufs = [k.sb("sa%d" % i, [128, 512], F32) for i in range(2)]
        tmp = k.sb("tmp", [128, D], F32)
        r = k.sb("r", [128, D], F32)
        ot = r
        stats = k.sb("stats", [128, 2, 6], F32)
        mv = new_mv("mv")
        cnt = [0]
        for g in range(TOK // 512):
            b = g // 4
            if g % 4 == 0:
                load_bc_row(gbc1, mod_d[0, b, 5 * D:6 * D], add_one=True)
            gbc = [gbc1, gbc1]
            for j in range(4):
                tok = g * 512 + j * 128
                k.dma("sp", xts[j][:, :], x1_d[tok:tok + 128, :], writes=[xts[j]])
            transpose_mod(xts, hT, 0, b, 3, 4, (4, 5))
            ffn_up(hT, w1, w3, gT, sa_bufs, cnt)
            for j in range(4):
                tok = g * 512 + j * 128
                yps = (PS[4 + (j % 2) * 2], PS[5 + (j % 2) * 2])
                ffn_down(gT, w2, j, yps)
                resid_ln(xts[j], yps, gbc[b], lng, lnb, tmp, r, ot, stats, mv)
                k.dma("sp", x2_d[tok:tok + 128, :], ot[:, :], reads=[ot])
        k.end_stage()

    if "s5" in stages:
        cwin = k.sb("cwin", [128, 8, 3 * D], BF16)
        cwout = k.sb("cwout", [128, 8, D], BF16)
        s_in = I["conv_w_in"][0].rearrange("(kc p) n -> p kc n", p=128)
        for kc in range(8):
            k.dma("pool", cwin[:, kc, :], s_in[:, kc, :], writes=[cwin])
        k.dma("pool", cwout[:, :, :], I["conv_w_out"][0].rearrange("(kc p) n -> p kc n", p=128), writes=[cwout])
        taps_s = k.sb("taps_s", [3, D], F32)
        tapsT = k.sb("tapsT", [128, 8, 3], F32)
        k.dma("sp", taps_s[:, :], I["conv_taps"][0], writes=[taps_s])
        for c in range(8):
            k.tr(PS[0], PS[0][:, c * 3:(c + 1) * 3], taps_s, taps_s[:, c * 128:(c + 1) * 128], ident, ident[0:3, 0:3])
        k.cp(tapsT, tapsT[:, :, :].rearrange("p a b -> p (a b)"), PS[0], PS[0][:, 0:24])
        lng = load_bc_row("lng", I["ln_g"][1, 0, :])
        lnb = load_bc_row("lnb", I["ln_b"][1, 0, :])
        gbc = [load_bc_row("gbc%d" % b, mod_d[1, b, 2 * D:3 * D], add_one=True) for b in range(NBC)]
        xts = [k.sb("xt%d" % j, [128, D], F32) for j in range(4)]
        hT = k.sb("hT", [128, 8, 512], BF16)
        zT = [k.sb("zT%d" % i, [128, 8, 516], F32) for i in range(2)]
        cs = [k.sb("cs%d" % i, [128, 512], F32) for i in range(2)]
        bs = [k.sb("bs%d" % i, [128, 512], F32) for i in range(2)]
        zc = [k.sb("zc%d" % i, [128, 512], F32) for i in range(2)]
        vT = k.sb("vT", [128, 8, 512], BF16)
        tmp = k.sb("tmp", [128, D], F32)
        r = k.sb("r", [128, D], F32)
        ot = k.sb("xo", [128, D], F32)
        stats = k.sb("stats", [128, 2, 6], F32)
        mv = new_mv("mv")
        for g in range(TOK // 512):
            b = g // 4
            z = zT[g % 2]
            zprev = zT[(g + 1) % 2]
            for j in range(4):
                tok = g * 512 + j * 128
                k.dma("sp", xts[j][:, :], x2_d[tok:tok + 128, :], writes=[xts[j]])
            transpose_mod(xts, hT, 1, b, 0, 1, (0, 1))
            if g % 4 == 0:
                k.memset(z, z[:, :, 0:2], 0.0)
            else:
                k.cp(z, z[:, :, 0:2], zprev, zprev[:, :, 512:514], eng="dve")
            for c in range(8):
                pb_, pc_, pu_ = PS[2 + (c % 2) * 3], PS[3 + (c % 2) * 3], PS[4 + (c % 2) * 3]
                for which, p in ((0, pb_), (1, pc_), (2, pu_)):
                    col = which * D + c * 128
                    for kc in range(8):
                        k.mm(p, p[:, :], cwin, cwin[:, kc, col:col + 128], hT, hT[:, kc, :], start=(kc == 0), stop=(kc == 7))
                cst_ = cs[c % 2]
                bst = bs[c % 2]
                zct = zc[c % 2]
                k.cp(cst_, cst_[:, :], pc_, pc_[:, :], eng="act")
                k.cp(bst, bst[:, :], pb_, pb_[:, :], eng="act")
                k.tt(z, z[:, c, 2:514], cst_, cst_[:, :], pu_, pu_[:, :], ALU.mult)
                k.ts(zct, zct[:, :], z, z[:, c, 0:512], tapsT[:, c, 0:1], None, ALU.mult, extra_reads=[tapsT])
                k.stt(zct, zct[:, :], z, z[:, c, 1:513], tapsT[:, c, 1:2], zct, zct[:, :], ALU.mult, ALU.add, extra_reads=[tapsT])
                k.stt(zct, zct[:, :], z, z[:, c, 2:514], tapsT[:, c, 2:3], zct, zct[:, :], ALU.mult, ALU.add, extra_reads=[tapsT])
                k.tt(vT, vT[:, c, :], zct, zct[:, :], bst, bst[:, :], ALU.mult)
            for j in range(4):
                tok = g * 512 + j * 128
                yps = (PS[(j % 2) * 2], PS[(j % 2) * 2 + 1])
                for h in range(2):
                    for kc in range(8):
                        k.mm(yps[h], yps[h][:, :], vT, vT[:, kc, j * 128:(j + 1) * 128], cwout, cwout[:, kc, h * 512:(h + 1) * 512],
                             start=(kc == 0), stop=(kc == 7))
                resid_ln(xts[j], yps, gbc[b], lng, lnb, tmp, r, ot, stats, mv)
                k.dma("sp", x3_d[tok:tok + 128, :], ot[:, :], reads=[ot])
        k.end_stage()

    if "s6" in stages:
        stage_moe_sparse(k, nc, I, C, ident, identb, modT, modP, mod_d, x3_d, out_d, transpose_mod, resid_ln, load_bc_row, new_mv)

    if "s6dense" in stages:
        rw = k.sb("rw", [128, 8, NEXP], F32)
        rb = k.sb("rb", [128, NEXP], F32)
        k.dma("sp", rw[:, :, :], I["router_w"][0].rearrange("(kc p) e -> p kc e", p=128), writes=[rw])
        k.dma("sp", rb[:, :], I["router_b"][0, :].partition_broadcast(128), writes=[rb])
        xts = [k.sb("xt%d" % j, [128, D], F32) for j in range(4)]
        hTs = [k.sb("hT%d" % i, [128, 8, 512], BF16) for i in range(2)]
        hT32 = k.sb("hT32", [128, 8, 512], F32)
        lg = k.sb("lg", [128, NEXP], F32)
        lg2 = k.sb("lg2", [128, NEXP], F32)
        m1 = k.sb("m1", [128, NEXP], F32)
        m2 = k.sb("m2", [128, NEXP], F32)
        sc = k.sb("sc", [128, 8], F32)
        for g in range(TOK // 512):
            b = g // 4
            hT = hTs[g % 2]
            for j in range(4):
                tok = g * 512 + j * 128
                k.dma("sp", xts[j][:, :], x3_d[tok:tok + 128, :], writes=[xts[j]])
            transpose_mod(xts, hT, 1, b, 3, 4, (0, 1), hT32=hT32)
            k.dma("sp", h2T_d[:, :, g * 512:(g + 1) * 512].rearrange("c p t -> p c t"), hT[:, :, :], reads=[hT])
            for j in range(4):
                ti = g * 4 + j
                p = PS[2 + j % 2]
                for kc in range(8):
                    k.mm(p, p[:, 0:NEXP], hT32, hT32[:, kc, j * 128:(j + 1) * 128], rw, rw[:, kc, :], start=(kc == 0), stop=(kc == 7))
                k.tt(lg, lg[:, :], p, p[:, 0:NEXP], rb, rb[:, :], ALU.add)
                S.op("dve", lambda e: e.reduce_max(out=sc[:, 0:1], in_=lg[:, :], axis=AX.X), reads=[lg], writes=[sc])
                k.ts(m1, m1[:, :], lg, lg[:, :], sc[:, 0:1], None, ALU.is_equal, extra_reads=[sc])
                k.stt(lg2, lg2[:, :], m1, m1[:, :], -1e30, lg, lg[:, :], ALU.mult, ALU.add)
                S.op("dve", lambda e: e.reduce_max(out=sc[:, 1:2], in_=lg2[:, :], axis=AX.X), reads=[lg2], writes=[sc])
                k.ts(m2, m2[:, :], lg2, lg2[:, :], sc[:, 1:2], None, ALU.is_equal, extra_reads=[sc])
                k.tt(sc, sc[:, 2:3], sc, sc[:, 0:1], sc, sc[:, 1:2], ALU.subtract)
                k.act(sc, sc[:, 3:4], sc, sc[:, 2:3], AF.Sigmoid)
                k.ts(sc, sc[:, 4:5], sc, sc[:, 3:4], -1.0, 1.0, ALU.mult, ALU.add)
                k.ts(m1, m1[:, :], m1, m1[:, :], sc[:, 3:4], None, ALU.mult, extra_reads=[sc])
                k.stt(gw, gw[:, ti, :], m2, m2[:, :], sc[:, 4:5], m1, m1[:, :], ALU.mult, ALU.add, extra_reads=[sc])
        k.end_stage()
        w1, w3, w2 = ffn_weights(None, None, None)
        hT = [k.sb("hTe%d" % i, [128, 8, 512], BF16) for i in range(2)]
        gT = k.sb("gT", [128, NF, 512], BF16)
        sa_bufs = [k.sb("sa%d" % i, [128, 512], F32) for i in range(2)]
        accs = [k.sb("acc%d" % i, [128, D], F32) for i in range(2)]
        cnt = [0]
        ai = 0
        acc_tr = [T(None, "acc%d" % i) for i in range(TOK // 128)]
        for ex in range(NEXP):
            ffn_load(w1, w3, w2, I["exp_w1"][0, ex], I["exp_w3"][0, ex], I["exp_w2"][0, ex])
            for g in range(TOK // 512):
                h = hT[g % 2]
                k.dma("sp", h[:, :, :], h2T_d[:, :, g * 512:(g + 1) * 512].rearrange("c p t -> p c t"), writes=[h])
                ffn_up(h, w1, w3, gT, sa_bufs, cnt)
                for j in range(4):
                    tok = g * 512 + j * 128
                    ti = g * 4 + j
                    yps = (PS[4 + (j % 2) * 2], PS[5 + (j % 2) * 2])
                    ffn_down(gT, w2, j, yps)
                    acc = accs[ai % 2]
                    ai += 1
                    if ex == 0:
                        for hh in range(2):
                            k.ts(acc, acc[:, hh * 512:(hh + 1) * 512], yps[hh], yps[hh][:, :], gw[:, ti, ex:ex + 1], None, ALU.mult, extra_reads=[gw])
                    else:
                        k.dma("sp", acc[:, :], acc_d[tok:tok + 128, :], reads=[acc_tr[ti]], writes=[acc])
                        for hh in range(2):
                            k.stt(acc, acc[:, hh * 512:(hh + 1) * 512], yps[hh], yps[hh][:, :], gw[:, ti, ex:ex + 1],
                                  acc, acc[:, hh * 512:(hh + 1) * 512], ALU.mult, ALU.add, extra_reads=[gw])
                    k.dma("sp", acc_d[tok:tok + 128, :], acc[:, :], reads=[acc], writes=[acc_tr[ti]])
        k.end_stage()
        lng = load_bc_row("lng", I["ln_g"][1, 1, :])
        lnb = load_bc_row("lnb", I["ln_b"][1, 1, :])
        gbc = [load_bc_row("gbc%d" % b, mod_d[1, b, 5 * D:6 * D], add_one=True) for b in range(NBC)]
        xts = [k.sb("xt%d" % j, [128, D], F32) for j in range(2)]
        ys = [k.sb("ya%d" % j, [128, D], F32) for j in range(2)]
        outs = [k.sb("xo%d" % j, [128, D], F32) for j in range(2)]
        tmp = k.sb("tmp", [128, D], F32)
        r = k.sb("r", [128, D], F32)
        stats = k.sb("stats", [128, 2, 6], F32)
        mv = new_mv("mv")
        for ti in range(TOK // 128):
            b = ti // 16
            tok = ti * 128
            xt, ya, ot = xts[ti % 2], ys[ti % 2], outs[ti % 2]
            k.dma("sp", xt[:, :], x3_d[tok:tok + 128, :], writes=[xt])
            k.dma("sp", ya[:, :], acc_d[tok:tok + 128, :], writes=[ya])
            resid_ln(xt, ya, gbc[b], lng, lnb, tmp, r, ot, stats, mv)
            k.dma("sp", out_d[tok:tok + 128, :], ot[:, :], reads=[ot])
        k.end_stage()

    S.barrier()
    with ExitStack() as st:
        S.emit(st)
    return nc, k


def skewed(items):
    n = len(items)
    depth = 2
    for i in range(min(depth, n)):
        items[i][0]()
    for i in range(n):
        if i + depth < n:
            items[i + depth][0]()
        items[i][1]()


def stage_attention(k, nc, I, C, dt_, ident, identb, qkT_d, vtok_d, gates_d, kcT_d, vc_d, oT_d):
    S = k.S
    PS = k.ps
    TINY = 1e-30
    cb = k.sb("cb", [128, 512], BF16)
    cbs = k.sb("cbs", [128, 512], BF16)
    wbm = k.sb("wbm", [128, 512], BF16)
    negtri = k.sb("negtri", [128, 128], BF16)
    negones = k.sb("negones", [128, 128], BF16)
    cmpbias = k.sb("cmpbias", [128, SEQ], BF16)
    expand = k.sb("expand", [32, 16, 128], BF16)
    selA = k.sb("selA", [128, 16, 32], F32)
    selB = k.sb("selB", [128, 16, 32], F32)
    for t, n in ((cb, "cb"), (cbs, "cbs"), (wbm, "wb"), (negtri, "negtri"), (negones, "negones"), (cmpbias, "cmpbias")):
        k.dma("pool", t[:, :], C[n], writes=[t])
    k.dma("pool", expand[:, :, :], C["expand"], writes=[expand])
    k.dma("sp", selA[:, :, :], C["selA"], writes=[selA])
    k.dma("sp", selB[:, :, :], C["selB"], writes=[selB])
    QK_IDX = [0, 1, 2, 3, 4, 5, 6, 7, 8, 9, 10, 11, 14, 15, 16, 17]
    qk = {ci: k.sb("qk%d" % ci, [128, SEQ], BF16) for ci in QK_IDX}
    sbv = k.sb("sbv", [128, 16, 512], BF16)
    vslc = k.sb("vslc", [128, 16, 2, 66], BF16)
    vwin = k.sb("vwin", [128, 16, 2, 66], BF16)
    gates = k.sb("gates", [128, 16, 24], F32)
    kcT = k.sb("kcT", [128, 2, 128], BF16)
    vcaug = k.sb("vcaug", [128, 2, 98], BF16)
    selT = k.sb("selT", [32, 2, SEQ], BF16)
    imp = k.sb("imp", [128, 16, 2, 32], F32)
    imp2 = k.sb("imp2", [128, 16, 32], F32)
    cmp3 = k.sb("cmp3", [128, 32, 32], BF16)
    rank = k.sb("rank", [128, 32], F32)
    selbias = k.sb("selbias", [128, 16, 32], BF16)
    rd = [k.sb("rd%d" % i, [128, 8], F32) for i in range(4)]
    tmpi = k.sb("tmpi", [128, 4, 32], F32)
    tmpo = [k.sb("tmpo%d" % i, [128, 4, 64], F32) for i in range(2)]
    Ecmp = [k.sb("Ecmp%d" % i, [128, 512], BF16) for i in range(2)]
    Pb = [k.sb("Pb%d" % i, [128, 512], BF16) for i in range(3)]
    e32 = [k.sb("e32_%d" % i, [128, 512], F32) for i in range(2)]
    spb = [k.sb("spb%d" % i, [128, 512], BF16) for i in range(3)]
    Wb = [k.sb("Wb%d" % i, [128, 512], BF16) for i in range(2)]
    accb = k.sb("accb", [128, 512], BF16)
    onsa = [k.sb("onsa%d" % i, [128, 4, 128], F32) for i in range(2)]
    onsab = [k.sb("onsab%d" % i, [128, 4, 128], BF16) for i in range(2)]
    ostage = [k.sb("ostage%d" % i, [128, 512], BF16) for i in range(3)]
    k.memset(vslc, vslc[:, :, :, 64:66], 1.0)
    k.memset(vwin, vwin[:, :, :, 64:66], 1.0)
    k.memset(vcaug, vcaug[:, :, 64:65], 1.0)
    for g in range(2):
        k.dma("pool", vcaug[:, g, 65:97], C["overlap"], writes=[vcaug])
    ctr = {}

    def nxt(name, n):
        v = ctr.get(name, 0)
        ctr[name] = v + 1
        return v % n

    NFILL = DBG.get("nfill", 2)

    def filler(bank):
        for _ in range(NFILL):
            k.mm(bank, bank[:, :], identb, identb[:, :], cb, cb[:, :], start=True, stop=True)

    for b in range(NBC):
        for ci in QK_IDX:
            k.dma("sp", qk[ci][:, :], qkT_d[b, ci, :, :], writes=[qk[ci]])
        k.dma("sp", sbv[:, :, :], vtok_d[b, :, 0:512].rearrange("(t p) c -> p t c", p=128), writes=[sbv])
        for g in range(2):
            k.dma("sp", vslc[:, :, g, 0:64], vtok_d[b, :, 512 + g * 64:512 + (g + 1) * 64].rearrange("(t p) c -> p t c", p=128), writes=[vslc])
            k.dma("sp", vwin[:, :, g, 0:64], vtok_d[b, :, 640 + g * 64:640 + (g + 1) * 64].rearrange("(t p) c -> p t c", p=128), writes=[vwin])
            k.dma("sp", kcT[:, g, :], kcT_d[b, g], writes=[kcT])
            k.dma("sp", vcaug[:, g, 0:64], vc_d[b, g], writes=[vcaug])
        k.dma("sp", gates[:, :, :], gates_d[b].rearrange("(t p) c -> p t c", p=128), writes=[gates])

        def cmp_scores(g, hq, qg, ncols_rhs):
            chunk = 8 + hq // 2
            po = 64 * (hq % 2)
            t0 = qg * 512
            p = PS[0]
            k.mm(p, p[:, :], kcT, kcT[po:po + 64, g, :], qk[chunk], qk[chunk][po:po + 64, t0:t0 + 512], start=True, stop=False)
            k.mm(p, p[:, :], identb, identb[:, :], cmpbias, cmpbias[:, t0:t0 + 512], start=False, stop=True)
            E = Ecmp[nxt("ec", 2)]
            k.act(E, E[:, :], p, p[:, :], AF.Exp)
            R = PS[2 + nxt("cr", 2)]
            for sub in range(4):
                k.mm(R, R[:, sub * 128:sub * 128 + ncols_rhs], E, E[:, sub * 128:(sub + 1) * 128], vcaug, vcaug[:, g, 0:ncols_rhs],
                     start=True, stop=True)
            return R

        def recip_den(R, col):
            rdt = rd[nxt("rd", 4)]
            Rv = R[:, :].rearrange("p (s c) -> p s c", s=4)
            k.ts(rdt, rdt[:, 0:4], R, Rv[:, :, col], TINY, None, ALU.max)
            S.op("dve", lambda e: e.reciprocal(out=rdt[:, 0:4], in_=rdt[:, 0:4]), reads=[rdt], writes=[rdt])
            return rdt, Rv

        for g in range(2):
            for r in range(4):
                hq = 4 * g + r
                for qg in range(4):
                    R = cmp_scores(g, hq, qg, 97)
                    rdt, Rv = recip_den(R, 64)
                    bc = rdt[:, 0:4].unsqueeze(2).broadcast_to([128, 4, 32])
                    if r == 0:
                        k.tt(imp, imp[:, 4 * qg:4 * qg + 4, g, :], R, Rv[:, :, 65:97], rdt, bc, ALU.mult)
                    else:
                        k.tt(tmpi, tmpi[:, :, :], R, Rv[:, :, 65:97], rdt, bc, ALU.mult)
                        k.tt(imp, imp[:, 4 * qg:4 * qg + 4, g, :], imp, imp[:, 4 * qg:4 * qg + 4, g, :], tmpi, tmpi[:, :, :], ALU.add)
        for g in range(2):
            k.tt(imp2, imp2[:, :, :], imp, imp[:, :, g, :], selA, selA[:, :, :], ALU.mult)
            k.tt(imp2, imp2[:, :, :], imp2, imp2[:, :, :], selB, selB[:, :, :], ALU.add)
            for tl in range(16):
                a = imp2[:, tl, :]
                in0 = a.unsqueeze(1).broadcast_to([128, 32, 32])
                in1 = a.unsqueeze(2).broadcast_to([128, 32, 32])
                k.tt(cmp3, cmp3[:, :, :], imp2, in0, imp2, in1, ALU.is_gt)
                S.op("dve", lambda e: e.reduce_sum(out=rank[:, :], in_=cmp3[:, :, :], axis=AX.X), reads=[cmp3], writes=[rank])
                k.ts(selbias, selbias[:, tl, :], rank, rank[:, :], 15.5, NEGB, ALU.is_gt, ALU.mult)
            for half in range(2):
                p = PS[4 + half]
                pv = p[:, :].bitcast(BF16)
                for t8 in range(8):
                    tl = half * 8 + t8
                    k.tr(p, pv[0:32, t8 * 128:(t8 + 1) * 128], selbias, selbias[:, tl, :], identb, identb[:, :])
                k.cp(selT, selT[0:32, g, half * 1024:(half + 1) * 1024], p, pv[0:32, 0:1024], eng="act")

        for g in range(2):
            for pair in range(2):
                for qg in range(4):
                    t0 = qg * 512
                    on = onsa[nxt("on", 2)]
                    for r2 in range(2):
                        hq = 4 * g + 2 * pair + r2
                        chunk = 8 + hq // 2
                        po = 64 * r2
                        qT = qk[chunk]
                        R = cmp_scores(g, hq, qg, 65)
                        rdt, Rv = recip_den(R, 64)
                        k.tt(rdt, rdt[:, 4:8], rdt, rdt[:, 0:4], gates, gates[:, 4 * qg:4 * qg + 4, hq * 3 + 0], ALU.mult)
                        k.tt(on, on[:, :, po:po + 64], R, Rv[:, :, 0:64], rdt, rdt[:, 4:8].unsqueeze(2).broadcast_to([128, 4, 64]), ALU.mult)
                        for br in range(2):
                            kT = qk[14 + g] if br == 0 else qk[16 + g]
                            vaug = vslc if br == 0 else vwin
                            ACC = PS[4 + br]
                            if br == 0:
                                kbs = list(range(0, 4 * qg + 4))
                            else:
                                kbs = list(range(max(0, 4 * qg - 4), 4 * qg + 4))
                            started = [False] * 4
                            items = []
                            for kb in kbs:
                                diag = kb >= 4 * qg
                                if diag:
                                    o = (kb - 4 * qg) * 128
                                    c0, c1 = o, 512
                                    bias_t, bias_ap = cb, cb[:, 0:512 - o]
                                    subs = list(range(o // 128, 4))
                                elif br == 1:
                                    m = kb - (4 * qg - 4)
                                    c0, c1 = 0, 128 * (m + 1)
                                    bias_t, bias_ap = wbm, wbm[:, 384 - 128 * m:512]
                                    subs = list(range(0, m + 1))
                                else:
                                    c0, c1 = 0, 512
                                    bias_t, bias_ap = None, None
                                    subs = [0, 1, 2, 3]
                                nco = c1 - c0
                                Pt = Pb[nxt("pt", 3)]
                                sp_ = PS[6 + nxt("ss", 2)]

                                def s1(kb=kb, c0=c0, c1=c1, nco=nco, bias_t=bias_t, bias_ap=bias_ap, Pt=Pt, sp_=sp_, kT=kT, br=br):
                                    k.mm(sp_, sp_[:, 0:nco], kT, kT[po:po + 64, kb * 128:(kb + 1) * 128], qT, qT[po:po + 64, t0 + c0:t0 + c1],
                                         start=True, stop=(br == 1 and bias_t is None))
                                    if br == 0:
                                        k.mm(sp_, sp_[:, 0:nco], expand, expand[0:32, kb, :], selT, selT[0:32, g, t0 + c0:t0 + c1],
                                             start=False, stop=(bias_t is None))
                                    if bias_t is not None:
                                        k.mm(sp_, sp_[:, 0:nco], identb, identb[:, :], bias_t, bias_ap, start=False, stop=True)
                                    k.act(Pt, Pt[:, 0:nco], sp_, sp_[:, 0:nco], AF.Exp)

                                def s2(kb=kb, c0=c0, subs=subs, Pt=Pt, vaug=vaug, ACC=ACC, started=started, last_kb=kbs[-1], qg=qg):
                                    filler(PS[1])
                                    for sub in subs:
                                        lo = sub * 128 - c0
                                        is_last = (kb == 4 * qg + sub)
                                        k.mm(ACC, ACC[:, sub * 128:sub * 128 + 65], Pt, Pt[:, lo:lo + 128], vaug, vaug[:, kb, g, 0:65],
                                             start=(not any(started)), stop=is_last, skip=True)
                                        started[sub] = True
                                items.append((s1, s2))
                            skewed(items)
                            rdt, Av = recip_den(ACC, 64)
                            k.tt(rdt, rdt[:, 4:8], rdt, rdt[:, 0:4], gates, gates[:, 4 * qg:4 * qg + 4, hq * 3 + 1 + br], ALU.mult)
                            to = tmpo[nxt("to", 2)]
                            k.tt(to, to[:, :, :], ACC, Av[:, :, 0:64], rdt, rdt[:, 4:8].unsqueeze(2).broadcast_to([128, 4, 64]), ALU.mult)
                            k.tt(on, on[:, :, po:po + 64], on, on[:, :, po:po + 64], to, to[:, :, :], ALU.add)
                    onb = onsab[nxt("onb", 2)]
                    k.cp(onb, onb[:, :, :], on, on[:, :, :], eng="dve")
                    p = PS[2 + nxt("cr", 2)]
                    pv = p[:, :].bitcast(BF16)
                    for sub in range(4):
                        k.tr(p, pv[:, sub * 128:(sub + 1) * 128], onb, onb[:, sub, :], identb, identb[:, :])
                    ost = ostage[nxt("os", 3)]
                    k.cp(ost, ost[:, :], p, pv[:, 0:512], eng="act")
                    k.dma("sp", oT_d[b, 4 + 2 * g + pair, :, t0:t0 + 512], ost[:, :], reads=[ost])

        items = []
        for h in range(8):
            c = h // 2
            po = 64 * (h % 2)
            qT = qk[c]
            kT = qk[4 + c]
            for qg in range(4):
                t0 = qg * 512
                OT = PS[4 + ((h * 4 + qg) % 2)]
                kbs = list(range(4 * qg + 3, -1, -1))
                for bi, kb in enumerate(kbs):
                    diag = kb >= 4 * qg
                    o = (kb - 4 * qg) * 128 if diag else 0
                    nco = 512 - o
                    first = (bi == 0)

                    def s1(kb=kb, o=o, nco=nco, diag=diag, qT=qT, kT=kT, po=po, t0=t0, st={}):
                        p1 = PS[nxt("d1", 2)]
                        k.mm(p1, p1[:, 0:nco], kT, kT[po:po + 64, kb * 128:(kb + 1) * 128], qT, qT[po:po + 64, t0 + o:t0 + 512],
                             start=True, stop=(not diag))
                        if diag:
                            k.mm(p1, p1[:, 0:nco], identb, identb[:, :], cbs, cbs[:, 0:nco], start=False, stop=True)
                        e = e32[nxt("e", 2)]
                        k.act(e, e[:, 0:nco], p1, p1[:, 0:nco], AF.Exp)
                        spt = spb[nxt("sp", 3)]
                        k.act(spt, spt[:, 0:nco], e, e[:, 0:nco], AF.Ln, bias=1.0)
                        st["sp"] = spt

                    items.append([s1, None, dict(kb=kb, o=o, nco=nco, diag=diag, first=first, qT=qT, kT=kT, po=po, t0=t0, OT=OT, c=c, h=h, qg=qg)])
        def make_s2(s1, d):
            st = s1.__defaults__[-1]

            def s2():
                kb, o, nco, diag, first = d["kb"], d["o"], d["nco"], d["diag"], d["first"]
                qT, kT, po, t0, OT, c, h, qg = d["qT"], d["kT"], d["po"], d["t0"], d["OT"], d["c"], d["h"], d["qg"]
                spt = st["sp"]
                p2 = PS[2 + nxt("d2", 2)]
                k.mm(p2, p2[:, 0:nco], kT, kT[po:po + 64, kb * 128:(kb + 1) * 128], qT, qT[po:po + 64, t0 + o:t0 + 512], start=True, stop=False)
                if diag:
                    k.mm(p2, p2[:, 0:nco], identb, identb[:, :], cbs, cbs[:, 0:nco], start=False, stop=False)
                has_acc = not first
                k.mm(p2, p2[:, 0:nco], negtri, negtri[:, :], spt, spt[:, 0:nco], start=False, stop=(not has_acc))
                if has_acc:
                    if diag:
                        k.mm(p2, p2[:, 128:nco], negones, negones[:, :], accb, accb[:, o + 128:512], start=False, stop=True)
                    else:
                        k.mm(p2, p2[:, 0:512], negones, negones[:, :], accb, accb[:, 0:512], start=False, stop=True)
                filler(PS[6 + nxt("fl", 2)])
                W = Wb[nxt("w", 2)]
                k.act(W, W[:, 0:nco], p2, p2[:, 0:nco], AF.Exp)
                if diag:
                    k.cp(accb, accb[:, o:o + 128], spt, spt[:, 0:128], eng="dve")
                    if nco > 128:
                        k.tt(accb, accb[:, o + 128:512], accb, accb[:, o + 128:512], spt, spt[:, 128:nco], ALU.add)
                else:
                    k.tt(accb, accb[:, :], accb, accb[:, :], spt, spt[:, :], ALU.add)
                last = (kb == 0)
                vl = sbv[:, kb, c * 128:(c + 1) * 128]
                if diag:
                    k.mm(OT, OT[:, o:o + 128], sbv, vl, W, W[:, 0:128], start=first, stop=last, skip=True)
                    if nco > 128:
                        k.mm(OT, OT[:, o + 128:512], sbv, vl, W, W[:, 128:nco], start=False, stop=last, skip=True)
                else:
                    k.mm(OT, OT[:, 0:512], sbv, vl, W, W[:, 0:512], start=False, stop=last, skip=True)
                if last:
                    ost = ostage[nxt("os", 3)]
                    k.cp(ost, ost[po:po + 64, :], OT, OT[po:po + 64, :], eng="dve")
                    k.dma("sp", oT_d[b, c, po:po + 64, t0:t0 + 512], ost[po:po + 64, :], reads=[ost])
            return s2
        its = [(it[0], make_s2(it[0], it[2])) for it in items]
        skewed(its)


def stage_moe_sparse(k, nc, I, C, ident, identb, modT, modP, mod_d, x3_d, out_d, transpose_mod, resid_ln, load_bc_row, new_mv):
    S = k.S
    PS = k.ps
    NT = TOK // 128
    NB = MOE_NBLK
    BIG = 100000.0
    dk = "ExternalOutput" if DBG.get("moe_dbg") else "Internal"
    xs_d = nc.dram_tensor("xs_d", [NSLOT, D], BF16, kind=dk).ap()
    yo_d = nc.dram_tensor("yo_d", [NSLOT, D], F32, kind=dk).ap()
    gwt = k.pers("gwt", [128, NT, 2], F32)
    sloti = k.pers("sloti", [128, NT, 2], I32)
    widx1 = k.pers("widx1", [128, NB, 16], I32)
    widx2 = k.pers("widx2", [128, NB, NF], I32)

    rw = k.sb("rw", [128, 8, NEXP], F32)
    rb = k.sb("rb", [128, NEXP], F32)
    trilt = k.sb("trilt", [128, 128], BF16)
    onesb = k.sb("onesb", [128, 128], BF16)
    pidx = k.sb("pidx", [128, 1], F32)
    thr = k.sb("thr", [128, NB + 8], F32)
    k.dma("sp", rw[:, :, :], I["router_w"][0].rearrange("(kc p) e -> p kc e", p=128), writes=[rw])
    k.dma("sp", rb[:, :], I["router_b"][0, :].partition_broadcast(128), writes=[rb])
    k.dma("pool", trilt[:, :], C["trilt"], writes=[trilt])
    k.dma("pool", onesb[:, :], C["ones"], writes=[onesb])
    k.dma("sp", pidx[:, :], C["pidx"], writes=[pidx])
    k.dma("sp", thr[:, :], C["thr"], writes=[thr])
    zt = k.sb("zt", [128, 16384], BF16)
    k.memset(zt, zt[:, :], 0.0)
    xs_tr = T(None, "xs_dram")
    xs_flat = xs_d.rearrange("(p a) c -> p (a c)", p=128)
    for i in range(NSLOT * D // 128 // 16384):
        k.dma("sp", xs_flat[:, i * 16384:(i + 1) * 16384], zt[:, :], reads=[zt], writes=[xs_tr])
    scb = [load_bc_row("scb%d" % b, mod_d[1, b, 4 * D:5 * D], add_one=True) for b in range(NBC)]
    shb = [load_bc_row("shb%d" % b, mod_d[1, b, 3 * D:4 * D]) for b in range(NBC)]
    xts = [k.sb("xt%d" % j, [128, D], F32) for j in range(4)]
    hT32 = k.sb("hT32", [128, 8, 512], F32)
    h2tok = k.sb("h2tok", [128, NT, D], BF16)
    tmp32 = k.sb("tmp32", [128, D], F32)
    posall = k.sb("posall", [128, NT, NEXP], F32)
    m1s = k.sb("m1s", [128, NT, NEXP], F32)
    m2s = k.sb("m2s", [128, NT, NEXP], F32)
    Macc = k.sb("Macc", [128, NEXP], F32)
    Maccb = k.sb("Maccb", [128, NEXP], BF16)
    Mbs = [k.sb("Mb%d" % i, [128, NEXP], BF16) for i in range(2)]
    lg = k.sb("lg", [128, NEXP], F32)
    lg2 = k.sb("lg2", [128, NEXP], F32)
    sc = k.sb("sc", [128, 8], F32)
    k.memset(Macc, Macc[:, :], 0.0)
    k.memset(Maccb, Maccb[:, :], 0.0)
    for g in range(TOK // 512):
        b = g // 4
        for j in range(4):
            tok = g * 512 + j * 128
            k.dma("sp", xts[j][:, :], x3_d[tok:tok + 128, :], writes=[xts[j]])
        transpose_mod(xts, None, 1, b, 3, 4, (0, 1), hT32=hT32)
        for j in range(4):
            ti = g * 4 + j
            k.tt(tmp32, tmp32[:, :], xts[j], xts[j][:, :], scb[b], scb[b][:, :], ALU.mult)
            k.tt(h2tok, h2tok[:, ti, :], tmp32, tmp32[:, :], shb[b], shb[b][:, :], ALU.add)
            p = PS[2 + j % 2]
            for kc in range(8):
                k.mm(p, p[:, 0:NEXP], hT32, hT32[:, kc, j * 128:(j + 1) * 128], rw, rw[:, kc, :], start=(kc == 0), stop=(kc == 7))
            k.tt(lg, lg[:, :], p, p[:, 0:NEXP], rb, rb[:, :], ALU.add)
            S.op("dve", lambda e: e.reduce_max(out=sc[:, 0:1], in_=lg[:, :], axis=AX.X), reads=[lg], writes=[sc])
            k.ts(m1s, m1s[:, ti, :], lg, lg[:, :], sc[:, 0:1], None, ALU.is_equal, extra_reads=[sc])
            k.stt(lg2, lg2[:, :], m1s, m1s[:, ti, :], -1e30, lg, lg[:, :], ALU.mult, ALU.add)
            S.op("dve", lambda e: e.reduce_max(out=sc[:, 1:2], in_=lg2[:, :], axis=AX.X), reads=[lg2], writes=[sc])
            k.ts(m2s, m2s[:, ti, :], lg2, lg2[:, :], sc[:, 1:2], None, ALU.is_equal, extra_reads=[sc])
            k.tt(sc, sc[:, 2:3], sc, sc[:, 0:1], sc, sc[:, 1:2], ALU.subtract)
            k.act(gwt, gwt[:, ti, 0:1], sc, sc[:, 2:3], AF.Sigmoid)
            k.ts(gwt, gwt[:, ti, 1:2], gwt, gwt[:, ti, 0:1], -1.0, 1.0, ALU.mult, ALU.add)
            Mb = Mbs[ti % 2]
            k.tt(Mb, Mb[:, :], m1s, m1s[:, ti, :], m2s, m2s[:, ti, :], ALU.add)
            pp = PS[4 + ti % 2]
            k.mm(pp, pp[:, 0:NEXP], trilt, trilt[:, :], Mb, Mb[:, :], start=True, stop=(ti == 0))
            if ti > 0:
                k.mm(pp, pp[:, 0:NEXP], onesb, onesb[:, :], Maccb, Maccb[:, :], start=False, stop=True)
            k.cp(posall, posall[:, ti, :], pp, pp[:, 0:NEXP])
            k.tt(Macc, Macc[:, :], Macc, Macc[:, :], Mb, Mb[:, :], ALU.add)
            k.cp(Maccb, Maccb[:, :], Macc, Macc[:, :])
    cnt = k.sb("cnt", [128, NEXP], F32)
    cmpA = k.sb("cmpA", [128, NEXP, 8], F32)
    nblk = k.sb("nblk", [128, NEXP], F32)
    pst = k.sb("pst", [128, NEXP + 1], F32)
    cmpB = k.sb("cmpB", [128, NB, NEXP], F32)
    blk = k.sb("blk", [128, NB], F32)
    b1 = k.sb("b1", [128, NB], F32)
    b2 = k.sb("b2", [128, NB], F32)
    pidx2 = k.sb("pidx2", [128, 1], F32)
    w1f = k.sb("w1f", [128, NB, 16], F32)
    w2f = k.sb("w2f", [128, NB, NF], F32)
    posp = k.sb("posp", [128, NT, NEXP], F32)
    slotf = k.sb("slotf", [128, NT, 2], F32)
    pc = PS[6]
    k.mm(pc, pc[:, 0:NEXP], onesb, onesb[:, :], Maccb, Maccb[:, :], start=True, stop=True)
    k.cp(cnt, cnt[:, :], pc, pc[:, 0:NEXP])
    k.tt(cmpA, cmpA[:, :, :], cnt, cnt[:, :].unsqueeze(2).broadcast_to([128, NEXP, 8]),
         thr, thr[:, 0:8].unsqueeze(1).broadcast_to([128, NEXP, 8]), ALU.is_gt)
    S.op("dve", lambda e: e.reduce_sum(out=nblk[:, :], in_=cmpA[:, :, :], axis=AX.X), reads=[cmpA], writes=[nblk])
    k.memset(pst, pst[:, :], 0.0)
    for ex in range(NEXP):
        k.stt(pst, pst[:, ex + 1:ex + 2], nblk, nblk[:, ex:ex + 1], float(MOE_BS), pst, pst[:, ex:ex + 1], ALU.mult, ALU.add)
    k.tt(cmpB, cmpB[:, :, :], pst, pst[:, 1:NEXP + 1].unsqueeze(1).broadcast_to([128, NB, NEXP]),
         thr, thr[:, 0:NB].unsqueeze(2).broadcast_to([128, NB, NEXP]), ALU.is_le)
    S.op("dve", lambda e: e.reduce_sum(out=blk[:, :], in_=cmpB[:, :, :], axis=AX.X), reads=[cmpB], writes=[blk])
    k.ts(blk, blk[:, :], blk, blk[:, :], float(NEXP - 1), None, ALU.min)
    k.ts(pidx2, pidx2[:, :], pidx, pidx[:, :], 2.0, None, ALU.mult)
    k.ts(b1, b1[:, :], blk, blk[:, :], 2048.0, pidx2[:, 0:1], ALU.mult, ALU.add, extra_reads=[pidx2])
    k.ts(b2, b2[:, :], blk, blk[:, :], float(DFF), pidx[:, 0:1], ALU.mult, ALU.add, extra_reads=[pidx])
    for c in range(16):
        kc, h = c // 2, c % 2
        k.ts(w1f, w1f[:, :, c], b1, b1[:, :], float(kc * 256 + h), None, ALU.add)
    for f in range(NF):
        k.ts(w2f, w2f[:, :, f], b2, b2[:, :], float(f * 128), None, ALU.add)
    k.cp(widx1, widx1[:, :, :], w1f, w1f[:, :, :])
    k.cp(widx2, widx2[:, :, :], w2f, w2f[:, :, :])
    k.tt(posp, posp[:, :, :], posall, posall[:, :, :], pst, pst[:, 0:NEXP].unsqueeze(1).broadcast_to([128, NT, NEXP]), ALU.add)
    for kk, ms in ((0, m1s), (1, m2s)):
        k.tt(ms, ms[:, :, :], ms, ms[:, :, :], posp, posp[:, :, :], ALU.mult)
        S.op("dve", (lambda kk_, ms_: (lambda e: e.reduce_sum(out=slotf[:, :, kk_], in_=ms_[:, :, :], axis=AX.X)))(kk, ms),
             reads=[ms], writes=[slotf])
    k.cp(sloti, sloti[:, :, :], slotf, slotf[:, :, :])
    if DBG.get("moe_dbg"):
        md = nc.dram_tensor("moe_dbg", [128, 512], F32, kind="ExternalOutput").ap()
        k.dma("sp", md[:, 0:64], slotf[:, :, :].rearrange("p a b -> p (a b)"), reads=[slotf])
        k.dma("sp", md[:, 64:128], gwt[:, :, :].rearrange("p a b -> p (a b)"), reads=[gwt])
        k.dma("sp", md[:, 128:128 + NB], blk[:, :], reads=[blk])
        k.dma("sp", md[:, 160:169], pst[:, :], reads=[pst])
        k.dma("sp", md[:, 176:184], cnt[:, :], reads=[cnt])
        k.dma("sp", md[:, 256:512], posall[:, :, :].rearrange("p a b -> p (a b)"), reads=[posall])
    for ti in range(NT):
        for kk in range(2):
            k.scatter(xs_d, h2tok[:, ti, :], sloti[:, ti, kk:kk + 1], reads=[h2tok, sloti, xs_tr])
    k.end_stage()
    if DBG.get("moe_stop") == "a":
        return

    w1h = k.sb("ew1", [128, 8, DFF], BF16)
    w3h = k.sb("ew3", [128, 8, DFF], BF16)
    w2h = k.sb("ew2", [128, NF, D], BF16)
    w1T = [[k.view(w1h, "ew1_%d_%d" % (i, h)) for h in range(2)] for i in range(8)]
    w3T = [[k.view(w3h, "ew3_%d_%d" % (i, h)) for h in range(2)] for i in range(8)]
    w2T = [k.view(w2h, "ew2_%d" % i) for i in range(NF)]
    xt = [k.sb("xs%d" % j, [128, D], BF16) for j in range(4)]
    hTs = [k.sb("hTe%d" % i, [128, 8, 512], BF16) for i in range(2)]
    gT = k.sb("gT", [128, NF, 512], BF16)
    sab = [k.sb("sa%d" % i, [128, 512], F32) for i in range(2)]
    yo = [k.sb("yo%d" % i, [128, D], F32) for i in range(2)]
    HALF = DFF // 2
    w1src = I["exp_w1"][0].rearrange("e r c -> (e r) c")
    w3src = I["exp_w3"][0].rearrange("e r c -> (e r) c")
    w2src = I["exp_w2"][0].rearrange("e f n -> (e f) n")
    cntr = [0, 0]
    for kb in range(DBG.get("moe_nb", NB)):
        for h in range(2):
            for kc in range(8):
                k.gather(w1h[:, kc, h * HALF:(h + 1) * HALF], w1src, widx1[:, kb, kc * 2 + h:kc * 2 + h + 1],
                         reads=[widx1], writes=[w1T[kc][h]])
                k.gather(w3h[:, kc, h * HALF:(h + 1) * HALF], w3src, widx1[:, kb, kc * 2 + h:kc * 2 + h + 1],
                         reads=[widx1], writes=[w3T[kc][h]])
        for f in range(NF):
            k.gather(w2h[:, f, :], w2src, widx2[:, kb, f:f + 1], reads=[widx2], writes=[w2T[f]])
        for j in range(4):
            r0 = kb * MOE_BS + j * 128
            k.dma("sp", xt[j][:, :], xs_d[r0:r0 + 128, :], writes=[xt[j]])
        hT = hTs[kb % 2]
        for c in range(8):
            pb = PS[4 + c % 2]
            pv = pb[:, :].bitcast(BF16)
            for j in range(4):
                k.tr(pb, pv[:, j * 128:(j + 1) * 128], xt[j], xt[j][:, c * 128:(c + 1) * 128], identb, identb[:, :])
            k.cp(hT, hT[:, c, :], pb, pv[:, 0:512], eng="act")
        for f in range(NF):
            pa = PS[(cntr[0] % 2) * 2]
            pb = PS[(cntr[0] % 2) * 2 + 1]
            sa = sab[cntr[0] % 2]
            cntr[0] += 1
            fh = 0 if f < NF // 2 else 1
            for kc in range(8):
                k.mm(pa, pa[:, :], w1T[kc][fh], w1h[:, kc, f * 128:(f + 1) * 128], hT, hT[:, kc, :], start=(kc == 0), stop=(kc == 7))
            for kc in range(8):
                k.mm(pb, pb[:, :], w3T[kc][fh], w3h[:, kc, f * 128:(f + 1) * 128], hT, hT[:, kc, :], start=(kc == 0), stop=(kc == 7))
            k.act(sa, sa[:, :], pa, pa[:, :], AF.Silu)
            k.tt(gT, gT[:, f, :], sa, sa[:, :], pb, pb[:, :], ALU.mult)
        for j in range(4):
            yps = (PS[4 + (j % 2) * 2], PS[5 + (j % 2) * 2])
            for hh in range(2):
                for f in range(NF):
                    k.mm(yps[hh], yps[hh][:, :], gT, gT[:, f, j * 128:(j + 1) * 128], w2T[f], w2h[:, f, hh * 512:(hh + 1) * 512],
                         start=(f == 0), stop=(f == NF - 1))
            y = yo[cntr[1] % 2]
            cntr[1] += 1
            k.cp(y, y[:, 0:512], yps[0], yps[0][:, :], eng="act")
            k.cp(y, y[:, 512:1024], yps[1], yps[1][:, :], eng="dve")
            r0 = kb * MOE_BS + j * 128
            k.dma("sp", yo_d[r0:r0 + 128, :], y[:, :], reads=[y])
    k.end_stage()
    if DBG.get("moe_stop") == "b":
        return

    lng = load_bc_row("lng", I["ln_g"][1, 1, :])
    lnb = load_bc_row("lnb", I["ln_b"][1, 1, :])
    gbc = [load_bc_row("gbc%d" % b, mod_d[1, b, 5 * D:6 * D], add_one=True) for b in range(NBC)]
    xts = [k.sb("xt%d" % j, [128, D], F32) for j in range(2)]
    r1s = [k.sb("r1_%d" % j, [128, D], F32) for j in range(2)]
    r2s = [k.sb("r2_%d" % j, [128, D], F32) for j in range(2)]
    outs = [k.sb("xo%d" % j, [128, D], F32) for j in range(2)]
    tmp = k.sb("tmp", [128, D], F32)
    r = k.sb("r", [128, D], F32)
    stats = k.sb("stats", [128, 2, 6], F32)
    mv = new_mv("mv")
    for ti in range(NT):
        b = ti // (SEQ // 128)
        tok = ti * 128
        xt_, r1, r2, ot = xts[ti % 2], r1s[ti % 2], r2s[ti % 2], outs[ti % 2]
        k.dma("sp", xt_[:, :], x3_d[tok:tok + 128, :], writes=[xt_])
        k.gather(r1[:, :], yo_d, sloti[:, ti, 0:1], reads=[sloti], writes=[r1])
        k.gather(r2[:, :], yo_d, sloti[:, ti, 1:2], reads=[sloti], writes=[r2])
        k.ts(r1, r1[:, :], r1, r1[:, :], gwt[:, ti, 0:1], None, ALU.mult, extra_reads=[gwt])
        k.stt(r1, r1[:, :], r2, r2[:, :], gwt[:, ti, 1:2], r1, r1[:, :], ALU.mult, ALU.add, extra_reads=[gwt])
        resid_ln(xt_, r1, gbc[b], lng, lnb, tmp, r, ot, stats, mv)
        k.dma("sp", out_d[tok:tok + 128, :], ot[:, :], reads=[ot])
    k.end_stage()


_CACHE = {}


def kernel(**inputs):
    if "nc" not in _CACHE:
        _CACHE["nc"] = build_program()[0]
        _CACHE["cst"] = host_consts()
    nc = _CACHE["nc"]
    cst = _CACHE["cst"]
    x = np.ascontiguousarray(inputs["x"], dtype=np.float32)
    c = np.ascontiguousarray(inputs["c"], dtype=np.float32)
    in_maps = []
    for core in range(NCORES):
        m = {}
        for n in INPUT_SHAPES:
            if n == "x":
                m[n] = x[core * NBC:(core + 1) * NBC].reshape(TOK, D)
            elif n == "c":
                m[n] = c[core * NBC:(core + 1) * NBC]
            else:
                m[n] = np.ascontiguousarray(inputs[n], dtype=np.float32).reshape(INPUT_SHAPES[n])
        for n, v in cst.items():
            m["c_" + n] = v
        in_maps.append(m)
    res = run_bass_kernel_spmd(nc, in_maps, core_ids=list(range(NCORES)))
    outs = [np.asarray(r["out"]).reshape(NBC, SEQ, D) for r in res.results]
    return np.concatenate(outs, axis=0).astype(np.float32)
```

```python
import numpy as np
from contextlib import ExitStack
import concourse.bass as bass
import concourse.mybir as mybir
from concourse.bass_utils import run_bass_kernel_spmd

F32 = mybir.dt.float32
BF16 = mybir.dt.bfloat16
AF = mybir.ActivationFunctionType
ALU = mybir.AluOpType
AX = mybir.AxisListType

NCORES = 8
D = 1024
SEQ = 2048
NBC = 2
TOK = NBC * SEQ
DFF = 2816
NF = DFF // 128
NEXP = 8
ALPHA = 4.0 ** 0.25
EPS = 1e-5
NEGB = -1024.0
MIXIN = 2840
MOE_BS = 512
MOE_NBLK = (TOK * 2) // MOE_BS + NEXP
NSLOT = MOE_NBLK * MOE_BS
I32 = mybir.dt.int32
ENGS = ("pe", "act", "dve", "pool", "sp")
DBG = {}


class T:
    __slots__ = ("h", "w", "r", "name")

    def __init__(self, h, name=""):
        self.h = h
        self.w = None
        self.r = {}
        self.name = name

    def __getitem__(self, k):
        return self.h[k]


class Sched:
    def __init__(self, nc, n_dma_sems=48):
        self.nc = nc
        self.streams = {e: [] for e in ENGS}
        self.cnt = {e: 0 for e in ENGS}
        self.seen = {e: {} for e in ENGS}
        self.n_dma = n_dma_sems
        self.dma_cnt = [0] * n_dma_sems
        self.dma_rr = 0
        self.sw_rr = 0
        self.n_hw = n_dma_sems - 16
        self.n_ops = 0
        self.n_waits = 0

    def _wait(self, eng, dep):
        key, val = dep
        if eng == "pe" and key == ("E", "pe"):
            return
        if self.seen[eng].get(key, 0) >= val:
            return
        self.seen[eng][key] = val
        self.streams[eng].append(("wait", key, val))
        self.n_waits += 1

    def _deps(self, eng, reads, writes):
        for t in reads:
            if t.w is not None:
                self._wait(eng, t.w)
        for t in writes:
            if t.w is not None:
                self._wait(eng, t.w)
            for k, v in t.r.items():
                self._wait(eng, (k, v))

    def _mark(self, me, reads, writes):
        k, v = me
        for t in reads:
            if t.r.get(k, 0) < v:
                t.r[k] = v
        for t in writes:
            t.w = me
            t.r = {}

    def op(self, eng, fn, reads=(), writes=()):
        self._deps(eng, reads, writes)
        self.cnt[eng] += 1
        me = (("E", eng), self.cnt[eng])
        self.streams[eng].append(("op", fn, ("E", eng), 1))
        self._mark(me, reads, writes)
        self.n_ops += 1

    def dma(self, q, out_ap, in_ap, reads=(), writes=(), **kw):
        if q == "pool":
            i = self.n_hw + self.sw_rr
            self.sw_rr = (self.sw_rr + 1) % (self.n_dma - self.n_hw)
        else:
            i = self.dma_rr
            self.dma_rr = (i + 1) % self.n_hw
        if self.dma_cnt[i] > 0:
            self._wait(q, (("D", i), self.dma_cnt[i]))
        self._deps(q, reads, writes)
        self.dma_cnt[i] += 16
        me = (("D", i), self.dma_cnt[i])
        self.streams[q].append(("op", lambda e: e.dma_start(out=out_ap, in_=in_ap, **kw), ("D", i), 16))
        self._mark(me, reads, writes)
        self.n_ops += 1

    def dma_fn(self, q, fn, reads=(), writes=()):
        if q == "pool":
            i = self.n_hw + self.sw_rr
            self.sw_rr = (self.sw_rr + 1) % (self.n_dma - self.n_hw)
        else:
            i = self.dma_rr
            self.dma_rr = (i + 1) % self.n_hw
        if self.dma_cnt[i] > 0:
            self._wait(q, (("D", i), self.dma_cnt[i]))
        self._deps(q, reads, writes)
        self.dma_cnt[i] += 16
        me = (("D", i), self.dma_cnt[i])
        self.streams[q].append(("op", fn, ("D", i), 16))
        self._mark(me, reads, writes)
        self.n_ops += 1

    def barrier(self):
        for e in ENGS:
            for e2 in ENGS:
                if e2 != e and self.cnt[e2] > 0:
                    self._wait(e, (("E", e2), self.cnt[e2]))
            for i in range(self.n_dma):
                if self.dma_cnt[i] > 0:
                    self._wait(e, (("D", i), self.dma_cnt[i]))

    def emit(self, stack):
        nc = self.nc
        sems = {}
        for e in ENGS:
            sems[("E", e)] = stack.enter_context(nc.semaphore("s_" + e))
        for i in range(self.n_dma):
            sems[("D", i)] = stack.enter_context(nc.semaphore("d_%d" % i))
        block = stack.enter_context(nc.Block())

        def run(engh, items):
            for it in items:
                if it[0] == "wait":
                    engh.wait_ge(sems[it[1]], it[2])
                else:
                    it[1](engh).then_inc(sems[it[2]], it[3])

        @block.tensor
        def _(e):
            run(e, self.streams["pe"])

        @block.scalar
        def _(e):
            run(e, self.streams["act"])

        @block.vector
        def _(e):
            run(e, self.streams["dve"])

        @block.gpsimd
        def _(e):
            run(e, self.streams["pool"])

        @block.sync
        def _(e):
            run(e, self.streams["sp"])


class K:
    SB_BASE = 16512
    SB_LIMIT = 229376

    def __init__(self, nc):
        self.nc = nc
        self.S = Sched(nc)
        self.pers_off = self.SB_BASE
        self.stage_base = self.SB_BASE
        self.off = self.SB_BASE
        self.uid = 0
        self.ps = [T(nc.alloc_psum_tensor("psb%d" % i, [128, 512], F32), "ps%d" % i) for i in range(8)]

    def _alloc(self, name, shape, dt, off):
        self.uid += 1
        h = self.nc.alloc_sbuf_tensor_at("%s_%d" % (name, self.uid), shape, dt, offset=off)
        return h

    @staticmethod
    def _bytes(shape, dt):
        n = 1
        for s in shape[1:]:
            n *= s
        b = n * (2 if dt == BF16 else 4)
        return (b + 31) // 32 * 32

    def pers(self, name, shape, dt):
        assert self.off == self.stage_base, "persistent alloc only between stages"
        h = self._alloc(name, shape, dt, self.pers_off)
        self.pers_off += self._bytes(shape, dt)
        self.stage_base = self.off = self.pers_off
        return T(h, name)

    def sb(self, name, shape, dt):
        h = self._alloc(name, shape, dt, self.off)
        self.off += self._bytes(shape, dt)
        assert self.off <= self.SB_LIMIT, "SBUF overflow at %s: %d" % (name, self.off)
        return T(h, name)

    def view(self, t, name=""):
        return T(t.h, name or t.name)

    def end_stage(self):
        self.S.barrier()
        self.off = self.stage_base
        for p in self.ps:
            p.w = None
            p.r = {}

    def mm(self, ot, o_ap, lt, l_ap, rt, r_ap, start=True, stop=True, skip=False):
        if skip:
            self.S.op("pe", lambda e: e.matmul(o_ap, lhsT=l_ap, rhs=r_ap, start=start, stop=stop, skip_group_check=True),
                      reads=[lt, rt], writes=[ot])
        else:
            self.S.op("pe", lambda e: e.matmul(o_ap, lhsT=l_ap, rhs=r_ap, start=start, stop=stop),
                      reads=[lt, rt], writes=[ot])

    def tr(self, ot, o_ap, it, i_ap, idt, id_ap):
        self.S.op("pe", lambda e: e.transpose(o_ap, i_ap, id_ap), reads=[it, idt], writes=[ot])

    def act(self, ot, o_ap, it, i_ap, func, bias=None, scale=None, extra_reads=(), eng="act"):
        kw = {}
        if bias is not None:
            kw["bias"] = bias
        if scale is not None:
            kw["scale"] = scale
        self.S.op("act", lambda e: e.activation(out=o_ap, in_=i_ap, func=func, **kw),
                  reads=[it] + list(extra_reads), writes=[ot])

    def tt(self, ot, o_ap, at, a_ap, bt, b_ap, op, eng="dve"):
        self.S.op(eng, lambda e: e.tensor_tensor(out=o_ap, in0=a_ap, in1=b_ap, op=op),
                  reads=[at, bt], writes=[ot])

    def ts(self, ot, o_ap, at, a_ap, s1, s2, op0, op1=None, extra_reads=(), eng="dve"):
        if op1 is None:
            self.S.op(eng, lambda e: e.tensor_scalar(out=o_ap, in0=a_ap, scalar1=s1, scalar2=None, op0=op0),
                      reads=[at] + list(extra_reads), writes=[ot])
        else:
            self.S.op(eng, lambda e: e.tensor_scalar(out=o_ap, in0=a_ap, scalar1=s1, scalar2=s2, op0=op0, op1=op1),
                      reads=[at] + list(extra_reads), writes=[ot])

    def stt(self, ot, o_ap, at, a_ap, scalar, bt, b_ap, op0, op1, extra_reads=(), eng="dve"):
        self.S.op(eng, lambda e: e.scalar_tensor_tensor(out=o_ap, in0=a_ap, scalar=scalar, in1=b_ap, op0=op0, op1=op1),
                  reads=[at, bt] + list(extra_reads), writes=[ot])

    def cp(self, ot, o_ap, it, i_ap, eng="dve"):
        if eng == "act":
            self.S.op("act", lambda e: e.copy(out=o_ap, in_=i_ap), reads=[it], writes=[ot])
        else:
            self.S.op(eng, lambda e: e.tensor_copy(out=o_ap, in_=i_ap), reads=[it], writes=[ot])

    def memset(self, ot, o_ap, val, eng="dve"):
        self.S.op(eng, lambda e: e.memset(o_ap, val), reads=[], writes=[ot])

    def dma(self, q, o_ap, i_ap, reads=(), writes=(), **kw):
        self.S.dma(q, o_ap, i_ap, reads=reads, writes=writes, **kw)

    def gather(self, o_ap, src_ap, idx_ap, reads=(), writes=(), bounds=None):
        if bounds is None:
            self.S.dma_fn("pool", lambda e: e.indirect_dma_start(
                out=o_ap, out_offset=None, in_=src_ap, in_offset=bass.IndirectOffsetOnAxis(ap=idx_ap, axis=0)),
                reads=reads, writes=writes)
        else:
            regs = self.__dict__.setdefault("_bound_regs", {})

            def fn(e):
                if bounds not in regs:
                    rg = e.alloc_register("bnd%d" % bounds)
                    e.reg_mov(rg, bounds)
                    regs[bounds] = rg
                return e.indirect_dma_start(
                    out=o_ap, out_offset=None, in_=src_ap, in_offset=bass.IndirectOffsetOnAxis(ap=idx_ap, axis=0),
                    bounds_check=regs[bounds], oob_is_err=False)
            self.S.dma_fn("pool", fn, reads=reads, writes=writes)

    def scatter(self, dst_ap, i_ap, idx_ap, reads=(), writes=()):
        self.S.dma_fn("pool", lambda e: e.indirect_dma_start(
            out=dst_ap, out_offset=bass.IndirectOffsetOnAxis(ap=idx_ap, axis=0), in_=i_ap, in_offset=None),
            reads=reads, writes=writes)


def host_consts():
    s = np.arange(128)[:, None]
    c = np.arange(512)[None, :]
    cst = {}
    cst["ident"] = np.eye(128, dtype=np.float32)
    cst["cb"] = np.where(c >= s, 0.0, NEGB).astype(np.float32)
    cst["cbs"] = np.where(c > s, 0.0, NEGB).astype(np.float32)
    cst["wb"] = np.where(c - 384 < s, 0.0, NEGB).astype(np.float32)
    j = np.arange(128)[:, None]
    ss = np.arange(128)[None, :]
    cst["negtri"] = np.where(j >= ss, -1.0, 0.0).astype(np.float32)
    cst["negones"] = -np.ones((128, 128), np.float32)
    n = np.arange(128)[:, None]
    t = np.arange(SEQ)[None, :]
    cmpb = np.where(16 * n + 31 <= t, 0.0, NEGB).astype(np.float32)
    cmpb[127, :] = NEGB
    cst["cmpbias"] = cmpb
    cmp_start = np.arange(127) * 16
    slc_start = np.arange(32) * 64
    ov = ((cmp_start[:, None] < slc_start[None, :] + 64) & (cmp_start[:, None] + 32 > slc_start[None, :]))
    ovp = np.zeros((128, 32), np.float32)
    ovp[:127] = ov.astype(np.float32)
    cst["overlap"] = ovp
    ex = np.zeros((32, 16, 128), np.float32)
    for kb in range(16):
        for sl in range(128):
            ex[2 * kb + sl // 64, kb, sl] = 1.0
    cst["expand"] = ex
    tt = np.arange(SEQ)
    blk = np.arange(32)[None, :]
    cur = (tt // 64)[:, None]
    valid = slc_start[None, :] <= tt[:, None]
    forced = (blk == 0) | (blk == cur) | (blk == cur - 1)
    A = (valid & ~forced).astype(np.float32)
    Bm = np.where(valid, np.where(forced, 1e30, 0.0), -1e30).astype(np.float32)
    cst["selA"] = A.reshape(16, 128, 32).transpose(1, 0, 2).copy()
    cst["selB"] = Bm.reshape(16, 128, 32).transpose(1, 0, 2).copy()
    r_ = np.arange(128)[:, None]
    c_ = np.arange(128)[None, :]
    cst["trilt"] = (r_ < c_).astype(np.float32)
    cst["ones"] = np.ones((128, 128), np.float32)
    cst["pidx"] = np.arange(128, dtype=np.float32).reshape(128, 1)
    cst["thr"] = np.tile((np.arange(MOE_NBLK + 8) * float(MOE_BS))[None, :], (128, 1)).astype(np.float32)
    return cst


CONST_SHAPES = {
    "ident": [128, 128], "cb": [128, 512], "cbs": [128, 512], "wb": [128, 512],
    "negtri": [128, 128], "negones": [128, 128], "cmpbias": [128, SEQ], "overlap": [128, 32],
    "expand": [32, 16, 128], "selA": [128, 16, 32], "selB": [128, 16, 32],
    "trilt": [128, 128], "ones": [128, 128], "pidx": [128, 1], "thr": [128, MOE_NBLK + 8],
}

INPUT_SHAPES = {
    "x": [TOK, D], "c": [NBC, D],
    "ada_w": [2, D, 6 * D], "ada_b": [2, 6 * D], "ln_g": [2, 2, D], "ln_b": [2, 2, D],
    "mix_w_in": [1, D, MIXIN], "cmp_pos": [1, 2, 32, 64], "cmp_w1": [1, 2, 2048, 256],
    "cmp_w2": [1, 2, 256, 64], "mix_w_out": [1, D, D],
    "ffn_w1": [1, D, DFF], "ffn_w3": [1, D, DFF], "ffn_w2": [1, DFF, D],
    "conv_w_in": [1, D, 3 * D], "conv_taps": [1, 3, D], "conv_w_out": [1, D, D],
    "router_w": [1, D, NEXP], "router_b": [1, NEXP],
    "exp_w1": [1, NEXP, 2 * D, DFF // 2], "exp_w3": [1, NEXP, 2 * D, DFF // 2], "exp_w2": [1, NEXP, DFF, D],
}

NQK = 18
VT_W = 768


def build_program(stages=("s0", "s1", "s2a", "s2", "s3", "s4", "s5", "s6"), dbg=(), feed=()):
    nc = bass.Bass("TRN2", target_bir_lowering=False)
    k = K(nc)
    S = k.S
    I = {n: nc.dram_tensor(n, shp, F32, kind="ExternalInput").ap() for n, shp in INPUT_SHAPES.items()}
    C = {n: nc.dram_tensor("c_" + n, shp, F32, kind="ExternalInput").ap() for n, shp in CONST_SHAPES.items()}
    out_d = nc.dram_tensor("out", [TOK, D], F32, kind="ExternalOutput").ap()

    def scratch(name, shape, dt):
        kind = "ExternalOutput" if name in dbg else ("ExternalInput" if name in feed else "Internal")
        return nc.dram_tensor(name, shape, dt, kind=kind).ap()

    mod_d = scratch("mod_d", [2, NBC, 6 * D], F32)
    qkT_d = scratch("qkT_d", [NBC, NQK, 128, SEQ], BF16)
    vtok_d = scratch("vtok_d", [NBC, SEQ, VT_W], BF16)
    gates_d = scratch("gates_d", [NBC, SEQ, 24], F32)
    kcT_d = scratch("kcT_d", [NBC, 2, 128, 128], BF16)
    vc_d = scratch("vc_d", [NBC, 2, 128, 64], BF16)
    oT_d = scratch("oT_d", [NBC, 8, 128, SEQ], BF16)
    x1_d = scratch("x1_d", [TOK, D], F32)
    x2_d = scratch("x2_d", [TOK, D], F32)
    x3_d = scratch("x3_d", [TOK, D], F32)
    h2T_d = scratch("h2T_d", [8, 128, TOK], BF16)
    acc_d = scratch("acc_d", [TOK, D], F32)
    dt_ = {n: T(None, n) for n in ("mod", "qkT", "vtok", "gates", "kcT", "vc", "oT", "x1", "x2", "x3", "h2T", "acc", "out")}
    dummy_in = T(None, "in")

    ident = k.pers("ident", [128, 128], F32)
    identb = k.pers("identb", [128, 128], BF16)
    modT = [k.pers("modT%d" % l, [128, 48, NBC], F32) for l in range(2)]
    modP = [k.pers("modP%d" % l, [128, 48, NBC], F32) for l in range(2)]
    gw = k.pers("gw", [128, TOK // 128, NEXP], F32)
    k.dma("sp", ident[:, :], C["ident"], writes=[ident])
    k.dma("pool", identb[:, :], C["ident"], writes=[identb])

    PS = k.ps

    def load_bc_row(name, src_row_ap, q="sp", add_one=False):
        t = name if isinstance(name, T) else k.sb(name, [128, D], F32)
        k.dma(q, t[:, :], src_row_ap.partition_broadcast(128), writes=[t])
        if add_one:
            k.ts(t, t[:, :], t, t[:, :], 1.0, None, ALU.add)
        return t

    def transpose_mod(xts, hT, l, b, sh_kind, sc_kind, psel, hT32=None):
        for c in range(8):
            p = PS[psel[c % len(psel)]]
            for j in range(4):
                k.tr(p, p[:, j * 128:(j + 1) * 128], xts[j], xts[j][:, c * 128:(c + 1) * 128], ident, ident[:, :])
            if hT is not None:
                k.act(hT, hT[:, c, :], p, p[:, :], AF.Identity,
                      bias=modT[l][:, sh_kind * 8 + c, b:b + 1], scale=modP[l][:, sc_kind * 8 + c, b:b + 1],
                      extra_reads=[modT[l], modP[l]])
            if hT32 is not None:
                k.act(hT32, hT32[:, c, :], p, p[:, :], AF.Identity,
                      bias=modT[l][:, sh_kind * 8 + c, b:b + 1], scale=modP[l][:, sc_kind * 8 + c, b:b + 1],
                      extra_reads=[modT[l], modP[l]])

    def resid_ln(xt, yps, gate_bc, lng, lnb, tmp, r, outt, stats, mv):
        if isinstance(yps, T):
            k.tt(tmp, tmp[:, :], yps, yps[:, :], gate_bc, gate_bc[:, :], ALU.mult)
        else:
            for h in range(2):
                k.tt(tmp, tmp[:, h * 512:(h + 1) * 512], yps[h], yps[h][:, :], gate_bc, gate_bc[:, h * 512:(h + 1) * 512], ALU.mult)
        k.stt(r, r[:, :], xt, xt[:, :], ALPHA, tmp, tmp[:, :], ALU.mult, ALU.add)
        for h in range(2):
            S.op("dve", (lambda hh: (lambda e: e.bn_stats(out=stats[:, hh, :], in_=r[:, hh * 512:(hh + 1) * 512])))(h),
                 reads=[r], writes=[stats])
        S.op("dve", lambda e: e.bn_aggr(out=mv[:, 0:2], in_=stats[:, :, :].rearrange("p a b -> p (a b)")),
             reads=[stats], writes=[mv])
        k.ts(mv, mv[:, 2:3], mv, mv[:, 1:2], EPS, None, ALU.add)
        S.op("pool", lambda e: e.tensor_tensor(out=mv[:, 3:4], in0=mv[:, 2:3], in1=mv[:, 4:5], op=ALU.pow),
             reads=[mv], writes=[mv])
        k.ts(tmp, tmp[:, :], r, r[:, :], mv[:, 0:1], mv[:, 3:4], ALU.subtract, ALU.mult, extra_reads=[mv])
        k.tt(tmp, tmp[:, :], tmp, tmp[:, :], lng, lng[:, :], ALU.mult)
        k.tt(outt, outt[:, :], tmp, tmp[:, :], lnb, lnb[:, :], ALU.add)

    def new_mv(name):
        mv = k.sb(name, [128, 8], F32)
        k.memset(mv, mv[:, :], -0.5)
        return mv

    if "s0" in stages:
        c_s = k.sb("c_s", [NBC, D], F32)
        cond_s = k.sb("cond_s", [NBC, D], F32)
        condT = k.sb("condT", [128, 8, NBC], BF16)
        k.dma("sp", c_s[:, :], I["c"], writes=[c_s])
        k.act(cond_s, cond_s[:, :], c_s, c_s[:, :], AF.Silu)
        for c in range(8):
            k.tr(PS[0], PS[0][:, c * NBC:(c + 1) * NBC], cond_s, cond_s[:, c * 128:(c + 1) * 128], ident, ident[0:NBC, 0:NBC])
        k.cp(condT, condT[:, :, :].rearrange("p a b -> p (a b)"), PS[0], PS[0][:, 0:8 * NBC])
        wbuf = [k.sb("adaw%d" % i, [128, 8, 512], BF16) for i in range(2)]
        mod_s = k.sb("mod_s", [NBC, 6 * D], F32)
        adab = k.sb("adab", [NBC, 6 * D], F32)
        for l in range(2):
            k.dma("sp", adab[:, :], I["ada_b"][l, :].partition_broadcast(NBC), writes=[adab])
            for blk in range(12):
                wb = wbuf[blk % 2]
                src = I["ada_w"][l].rearrange("(kc p) n -> p kc n", p=128)[:, :, blk * 512:(blk + 1) * 512]
                k.dma("pool", wb[:, :, :], src, writes=[wb])
                p = PS[1 + blk % 2]
                for kc in range(8):
                    k.mm(p, p[0:NBC, :], condT, condT[:, kc, :], wb, wb[:, kc, :], start=(kc == 0), stop=(kc == 7))
                k.tt(mod_s, mod_s[:, blk * 512:(blk + 1) * 512], p, p[0:NBC, :], adab, adab[:, blk * 512:(blk + 1) * 512], ALU.add)
            k.dma("sp", mod_d[l], mod_s[:, :], reads=[mod_s])
            for ch in range(48):
                k.tr(PS[3], PS[3][:, ch * NBC:(ch + 1) * NBC], mod_s, mod_s[:, ch * 128:(ch + 1) * 128], ident, ident[0:NBC, 0:NBC])
            k.cp(modT[l], modT[l][:, :, :].rearrange("p a b -> p (a b)"), PS[3], PS[3][:, 0:48 * NBC])
            k.ts(modP[l], modP[l][:, :, :].rearrange("p a b -> p (a b)"), modT[l],
                 modT[l][:, :, :].rearrange("p a b -> p (a b)"), 1.0, None, ALU.add)
        k.end_stage()

    if "s1" in stages:
        win = k.sb("win", [128, 8, MIXIN], BF16)
        wdup = k.sb("wdup", [128, 8, 512], BF16)
        wsrc = I["mix_w_in"][0].rearrange("(kc p) n -> p kc n", p=128)
        for kc in range(8):
            k.dma("pool", win[:, kc, :], wsrc[:, kc, :], writes=[win])
        for i, col in enumerate((2304, 2368, 2560, 2624)):
            for rep in range(2):
                k.dma("pool", wdup[:, :, i * 128 + rep * 64: i * 128 + rep * 64 + 64], wsrc[:, :, col:col + 64], writes=[wdup])
        xts = [[k.sb("xt%d_%d" % (i, j), [128, D], F32) for j in range(4)] for i in range(2)]
        hTs = [k.sb("hT%d" % i, [128, 8, 512], BF16) for i in range(2)]
        fm = [k.sb("fm%d" % i, [128, 512], BF16) for i in range(4)]
        vst = [k.sb("vst%d" % i, [128, VT_W], BF16) for i in range(2)]
        gst = [k.sb("gst%d" % i, [128, 24], F32) for i in range(2)]
        fm_i = 0
        v_i = 0
        chunks = []
        for c in range(4):
            chunks.append((win, 0 + c * 128, 1.0))
        for c in range(4):
            chunks.append((win, 512 + c * 128, 0.125))
        for c in range(4):
            chunks.append((win, 1536 + c * 128, 1.0))
        chunks.append((win, 2048, 1.0))
        chunks.append((win, 2176, 1.0))
        for c in range(4):
            chunks.append((wdup, c * 128, 0.125))
        for g in range(DBG.get("s1_groups", TOK // 512)):
            b = g // 4
            t0 = (g % 4) * 512
            xt = xts[g % 2]
            hT = hTs[g % 2]
            for j in range(4):
                k.dma("sp", xt[j][:, :], I["x"][g * 512 + j * 128: g * 512 + (j + 1) * 128, :], writes=[xt[j]])
            transpose_mod(xt, hT, 0, b, 0, 1, (0, 1))
            for ci, (wt, col0, scl) in enumerate(chunks if DBG.get("s1_fm", True) else []):
                p = PS[2 + ci % 3]
                for kc in range(8):
                    k.mm(p, p[:, :], wt, wt[:, kc, col0:col0 + 128], hT, hT[:, kc, :], start=(kc == 0), stop=(kc == 7))
                f = fm[fm_i % 4]
                fm_i += 1
                if ci % 2 == 0:
                    k.act(f, f[:, :], p, p[:, :], AF.Identity, scale=scl)
                else:
                    k.ts(f, f[:, :], p, p[:, :], scl, None, ALU.mult)
                k.dma("sp", qkT_d[b, ci, :, t0:t0 + 512], f[:, :], reads=[f])
            for j in range(4 if DBG.get("s1_tm", True) else 0):
                pv, pg = PS[5 + (j % 2)], PS[7]
                for kc in range(8):
                    k.mm(pv, pv[:, :], hT, hT[:, kc, j * 128:(j + 1) * 128], win, win[:, kc, 1024:1536], start=(kc == 0), stop=(kc == 7))
                if DBG.get("pg1", True):
                    for kc in range(8):
                        k.mm(pg, pg[:, 0:128], hT, hT[:, kc, j * 128:(j + 1) * 128], win, win[:, kc, 2432:2560], start=(kc == 0), stop=(kc == 7))
                if DBG.get("pg2", True):
                    for kc in range(8):
                        k.mm(pg, pg[:, 128:256], hT, hT[:, kc, j * 128:(j + 1) * 128], win, win[:, kc, 2688:2816], start=(kc == 0), stop=(kc == 7))
                    for kc in range(8):
                        k.mm(pg, pg[:, 256:280], hT, hT[:, kc, j * 128:(j + 1) * 128], win, win[:, kc, 2816:2840], start=(kc == 0), stop=(kc == 7))
                vs = vst[v_i % 2]
                gs = gst[v_i % 2]
                v_i += 1
                k.cp(vs, vs[:, 0:512], pv, pv[:, :], eng="dve")
                if DBG.get("pgc", True):
                    k.cp(vs, vs[:, 512:768], pg, pg[:, 0:256], eng=DBG.get("pgc_eng", "act"))
                if DBG.get("sig", True):
                    k.act(gs, gs[:, :], pg, pg[:, 256:280], AF.Sigmoid)
                tok = t0 + j * 128
                k.dma("sp", vtok_d[b, tok:tok + 128, :], vs[:, :], reads=[vs])
                if DBG.get("gdma", True):
                    k.dma("sp", gates_d[b, tok:tok + 128, :], gs[:, :], reads=[gs])
        k.end_stage()

    if "s2a" in stages:
        w1 = [k.sb("cw1_%d" % i, [128, 32, 256], BF16) for i in range(2)]
        w2k = k.sb("cw2k", [128, 2, 128], BF16)
        w2v = k.sb("cw2v", [128, 2, 64], BF16)
        posT = [k.sb("posT%d" % i, [128, 32], F32) for i in range(2)]
        pos_s = [k.sb("pos_s%d" % i, [32, 128], F32) for i in range(2)]
        for kv in range(2):
            src = I["cmp_w1"][0, kv].rearrange("(l d) h -> d l h", d=64)
            for half in range(2):
                k.dma("pool", w1[kv][half * 64:(half + 1) * 64, :, :], src, writes=[w1[kv]])
            for half in range(2):
                k.dma("sp", pos_s[kv][:, half * 64:(half + 1) * 64], I["cmp_pos"][0, kv], writes=[pos_s[kv]])
            k.tr(PS[0], PS[0][:, kv * 32:(kv + 1) * 32], pos_s[kv], pos_s[kv][:, :], ident, ident[0:32, 0:32])
            k.cp(posT[kv], posT[kv][:, :], PS[0], PS[0][:, kv * 32:(kv + 1) * 32])
        w2ksrc = I["cmp_w2"][0, 0].rearrange("(hc p) d -> p hc d", p=128)
        for rep in range(2):
            k.dma("pool", w2k[:, :, rep * 64:(rep + 1) * 64], w2ksrc, writes=[w2k])
        k.dma("pool", w2v[:, :, :], I["cmp_w2"][0, 1].rearrange("(hc p) d -> p hc d", p=128), writes=[w2v])
        src_t = [k.sb("cmpsrc%d" % i, [128, SEQ], BF16) for i in range(2)]
        kp = [k.sb("kp%d" % i, [128, 32, 128], BF16) for i in range(2)]
        HsT = [k.sb("HsT%d" % i, [128, 2, 128], BF16) for i in range(2)]
        kc_s = [k.sb("kc_s%d" % i, [128, 128], BF16) for i in range(2)]
        vc_s = [k.sb("vc_s%d" % i, [128, 64], BF16) for i in range(2)]
        it = 0
        for b in range(NBC):
            for kv in range(2):
                st = src_t[it % 2]
                kpt = kp[it % 2]
                it += 1
                k.dma("sp", st[:, :], qkT_d[b, 12 + kv, :, :], writes=[st])
                for l in range(32):
                    k.act(kpt, kpt[:, l, 0:127], st, st[:, l:l + 16 * 126 + 1:16], AF.Identity,
                          bias=posT[kv][:, l:l + 1], extra_reads=[posT[kv]])
                for g in range(2):
                    hs = HsT[g]
                    for hc in range(2):
                        p = PS[1 + hc]
                        for l in range(32):
                            k.mm(p, p[:, 0:127], w1[kv], w1[kv][g * 64:(g + 1) * 64, l, hc * 128:(hc + 1) * 128],
                                 kpt, kpt[g * 64:(g + 1) * 64, l, 0:127], start=(l == 0), stop=(l == 31))
                        k.act(hs, hs[:, hc, 0:127], p, p[:, 0:127], AF.Silu)
                    if kv == 0:
                        p = PS[3]
                        for hc in range(2):
                            k.mm(p, p[:, 0:127], w2k, w2k[:, hc, :], hs, hs[:, hc, 0:127], start=(hc == 0), stop=(hc == 1))
                        kcs = kc_s[g]
                        k.memset(kcs, kcs[:, 96:128], 0.0)
                        k.act(kcs, kcs[:, 0:127], p, p[:, 0:127], AF.Identity, scale=0.125)
                        k.dma("sp", kcT_d[b, g], kcs[:, :], reads=[kcs])
                    else:
                        p = PS[4]
                        for hc in range(2):
                            k.mm(p, p[0:127, 0:64], hs, hs[:, hc, 0:127], w2v, w2v[:, hc, :], start=(hc == 0), stop=(hc == 1))
                        vcs = vc_s[g]
                        k.memset(vcs, vcs[:, :], 0.0)
                        k.cp(vcs, vcs[0:127, :], p, p[0:127, 0:64])
                        k.dma("sp", vc_d[b, g], vcs[:, :], reads=[vcs])
        k.end_stage()

    if "s2" in stages:
        stage_attention(k, nc, I, C, dt_, ident, identb, qkT_d, vtok_d, gates_d, kcT_d, vc_d, oT_d)
        k.end_stage()

    if "s3" in stages:
        wout = k.sb("wout", [128, 8, D], BF16)
        k.dma("pool", wout[:, :, :], I["mix_w_out"][0].rearrange("(kc p) n -> p kc n", p=128), writes=[wout])
        lng = load_bc_row("lng", I["ln_g"][0, 0, :])
        lnb = load_bc_row("lnb", I["ln_b"][0, 0, :])
        gbc = [load_bc_row("gbc%d" % b, mod_d[0, b, 2 * D:3 * D], add_one=True) for b in range(NBC)]
        oTs = [k.sb("oTs%d" % i, [128, 8, 512], BF16) for i in range(2)]
        xts = [k.sb("xt%d" % i, [128, D], F32) for i in range(3)]
        outs = [k.sb("xo%d" % i, [128, D], F32) for i in range(2)]
        tmp = k.sb("tmp", [128, D], F32)
        r = k.sb("r", [128, D], F32)
        stats = k.sb("stats", [128, 2, 6], F32)
        mv = new_mv("mv")
        ti = 0
        for g in range(TOK // 512):
            b = g // 4
            t0 = (g % 4) * 512
            oT = oTs[g % 2]
            k.dma("sp", oT[:, :, :], oT_d[b, :, :, t0:t0 + 512].rearrange("c p t -> p c t"), writes=[oT])
            for j in range(4):
                xt = xts[ti % 3]
                ot = outs[ti % 2]
                tok = g * 512 + j * 128
                k.dma("sp", xt[:, :], I["x"][tok:tok + 128, :], writes=[xt])
                yps = (PS[(ti % 2) * 2], PS[(ti % 2) * 2 + 1])
                for h in range(2):
                    for kc in range(8):
                        k.mm(yps[h], yps[h][:, :], oT, oT[:, kc, j * 128:(j + 1) * 128], wout, wout[:, kc, h * 512:(h + 1) * 512],
                             start=(kc == 0), stop=(kc == 7))
                resid_ln(xt, yps, gbc[b], lng, lnb, tmp, r, ot, stats, mv)
                k.dma("sp", x1_d[tok:tok + 128, :], ot[:, :], reads=[ot])
                ti += 1
        k.end_stage()

    def ffn_weights(w1src, w3src, w2src):
        w1 = k.sb("fw1", [128, 8, DFF], BF16)
        w3 = k.sb("fw3", [128, 8, DFF], BF16)
        w2 = k.sb("fw2", [128, NF, D], BF16)
        return w1, w3, w2

    def ffn_load(w1, w3, w2, w1src, w3src, w2src):
        s1 = w1src.rearrange("(kc p) n -> p kc n", p=128)
        s3 = w3src.rearrange("(kc p) n -> p kc n", p=128)
        s2 = w2src.rearrange("(f p) n -> p f n", p=128)
        for kc in range(8):
            k.dma("pool", w1[:, kc, :], s1[:, kc, :], writes=[w1])
            k.dma("pool", w3[:, kc, :], s3[:, kc, :], writes=[w3])
        for f0 in range(0, NF, 4):
            f1 = min(NF, f0 + 4)
            k.dma("pool", w2[:, f0:f1, :], s2[:, f0:f1, :], writes=[w2])

    def ffn_up(hT, w1, w3, gT, sa_bufs, cnt):
        for f in range(NF):
            pa = PS[(cnt[0] % 2) * 2]
            pb = PS[(cnt[0] % 2) * 2 + 1]
            sa = sa_bufs[cnt[0] % 2]
            cnt[0] += 1
            for kc in range(8):
                k.mm(pa, pa[:, :], w1, w1[:, kc, f * 128:(f + 1) * 128], hT, hT[:, kc, :], start=(kc == 0), stop=(kc == 7))
            for kc in range(8):
                k.mm(pb, pb[:, :], w3, w3[:, kc, f * 128:(f + 1) * 128], hT, hT[:, kc, :], start=(kc == 0), stop=(kc == 7))
            k.act(sa, sa[:, :], pa, pa[:, :], AF.Silu)
            k.tt(gT, gT[:, f, :], sa, sa[:, :], pb, pb[:, :], ALU.mult)

    def ffn_down(gT, w2, j, yps):
        for h in range(2):
            for f in range(NF):
                k.mm(yps[h], yps[h][:, :], gT, gT[:, f, j * 128:(j + 1) * 128], w2, w2[:, f, h * 512:(h + 1) * 512],
                     start=(f == 0), stop=(f == NF - 1))

    if "s4" in stages:
        w1, w3, w2 = ffn_weights(None, None, None)
        ffn_load(w1, w3, w2, I["ffn_w1"][0], I["ffn_w3"][0], I["ffn_w2"][0])
        lng = load_bc_row("lng", I["ln_g"][0, 1, :])
        lnb = load_bc_row("lnb", I["ln_b"][0, 1, :])
        gbc1 = k.sb("gbc", [128, D], F32)
        xts = [k.sb("xt%d" % j, [128, D], F32) for j in range(4)]
        hT = k.sb("hT", [128, 8, 512], BF16)
        gT = k.sb("gT", [128, NF, 512], BF16)
        sa_bufs = [k.sb("sa%d" % i, [128, 512], F32) for i in range(2)]
        tmp = k.sb("tmp", [128, D], F32)
        r = k.sb("r", [128, D], F32)
        ot = r
        stats = k.sb("stats", [128, 2, 6], F32)
        mv = new_mv("mv")
        cnt = [0]
        for g in range(TOK // 512):
            b = g // 4
            if g % 4 == 0:
                load_bc_row(gbc1, mod_d[0, b, 5 * D:6 * D], add_one=True)
            gbc = [gbc1, gbc1]
            for j in range(4):
                tok = g * 512 + j * 128
                k.dma("sp", xts[j][:, :], x1_d[tok:tok + 128, :], writes=[xts[j]])
            transpose_mod(xts, hT, 0, b, 3, 4, (4, 5))
            ffn_up(hT, w1, w3, gT, sa_bufs, cnt)
            for j in range(4):
                tok = g * 512 + j * 128
                yps = (PS[4 + (j % 2) * 2], PS[5 + (j % 2) * 2])
                ffn_down(gT, w2, j, yps)
                resid_ln(xts[j], yps, gbc[b], lng, lnb, tmp, r, ot, stats, mv)
                k.dma("sp", x2_d[tok:tok + 128, :], ot[:, :], reads=[ot])
        k.end_stage()

    if "s5" in stages:
        cwin = k.sb("cwin", [128, 8, 3 * D], BF16)
        cwout = k.sb("cwout", [128, 8, D], BF16)
        s_in = I["conv_w_in"][0].rearrange("(kc p) n -> p kc n", p=128)
        for kc in range(8):
            k.dma("pool", cwin[:, kc, :], s_in[:, kc, :], writes=[cwin])
        k.dma("pool", cwout[:, :, :], I["conv_w_out"][0].rearrange("(kc p) n -> p kc n", p=128), writes=[cwout])
        taps_s = k.sb("taps_s", [3, D], F32)
        tapsT = k.sb("tapsT", [128, 8, 3], F32)
        k.dma("sp", taps_s[:, :], I["conv_taps"][0], writes=[taps_s])
        for c in range(8):
            k.tr(PS[0], PS[0][:, c * 3:(c + 1) * 3], taps_s, taps_s[:, c * 128:(c + 1) * 128], ident, ident[0:3, 0:3])
        k.cp(tapsT, tapsT[:, :, :].rearrange("p a b -> p (a b)"), PS[0], PS[0][:, 0:24])
        lng = load_bc_row("lng", I["ln_g"][1, 0, :])
        lnb = load_bc_row("lnb", I["ln_b"][1, 0, :])
        gbc = [load_bc_row("gbc%d" % b, mod_d[1, b, 2 * D:3 * D], add_one=True) for b in range(NBC)]
        xts = [k.sb("xt%d" % j, [128, D], F32) for j in range(4)]
        hT = k.sb("hT", [128, 8, 512], BF16)
        zT = [k.sb("zT%d" % i, [128, 8, 516], F32) for i in range(2)]
        cs = [k.sb("cs%d" % i, [128, 512], F32) for i in range(2)]
        bs = [k.sb("bs%d" % i, [128, 512], F32) for i in range(2)]
        zc = [k.sb("zc%d" % i, [128, 512], F32) for i in range(2)]
        vT = k.sb("vT", [128, 8, 512], BF16)
        tmp = k.sb("tmp", [128, D], F32)
        r = k.sb("r", [128, D], F32)
        ot = k.sb("xo", [128, D], F32)
        stats = k.sb("stats", [128, 2, 6], F32)
        mv = new_mv("mv")
        for g in range(TOK // 512):
            b = g // 4
            z = zT[g % 2]
            zprev = zT[(g + 1) % 2]
            for j in range(4):
                tok = g * 512 + j * 128
                k.dma("sp", xts[j][:, :], x2_d[tok:tok + 128, :], writes=[xts[j]])
            transpose_mod(xts, hT, 1, b, 0, 1, (0, 1))
            if g % 4 == 0:
                k.memset(z, z[:, :, 0:2], 0.0)
            else:
                k.cp(z, z[:, :, 0:2], zprev, zprev[:, :, 512:514], eng="dve")
            for c in range(8):
                pb_, pc_, pu_ = PS[2 + (c % 2) * 3], PS[3 + (c % 2) * 3], PS[4 + (c % 2) * 3]
                for which, p in ((0, pb_), (1, pc_), (2, pu_)):
                    col = which * D + c * 128
                    for kc in range(8):
                        k.mm(p, p[:, :], cwin, cwin[:, kc, col:col + 128], hT, hT[:, kc, :], start=(kc == 0), stop=(kc == 7))
                cst_ = cs[c % 2]
                bst = bs[c % 2]
                zct = zc[c % 2]
                k.cp(cst_, cst_[:, :], pc_, pc_[:, :], eng="act")
                k.cp(bst, bst[:, :], pb_, pb_[:, :], eng="act")
                k.tt(z, z[:, c, 2:514], cst_, cst_[:, :], pu_, pu_[:, :], ALU.mult)
                k.ts(zct, zct[:, :], z, z[:, c, 0:512], tapsT[:, c, 0:1], None, ALU.mult, extra_reads=[tapsT])
                k.stt(zct, zct[:, :], z, z[:, c, 1:513], tapsT[:, c, 1:2], zct, zct[:, :], ALU.mult, ALU.add, extra_reads=[tapsT])
                k.stt(zct, zct[:, :], z, z[:, c, 2:514], tapsT[:, c, 2:3], zct, zct[:, :], ALU.mult, ALU.add, extra_reads=[tapsT])
                k.tt(vT, vT[:, c, :], zct, zct[:, :], bst, bst[:, :], ALU.mult)
            for j in range(4):
                tok = g * 512 + j * 128
                yps = (PS[(j % 2) * 2], PS[(j % 2) * 2 + 1])
                for h in range(2):
                    for kc in range(8):
                        k.mm(yps[h], yps[h][:, :], vT, vT[:, kc, j * 128:(j + 1) * 128], cwout, cwout[:, kc, h * 512:(h + 1) * 512],
                             start=(kc == 0), stop=(kc == 7))
                resid_ln(xts[j], yps, gbc[b], lng, lnb, tmp, r, ot, stats, mv)
                k.dma("sp", x3_d[tok:tok + 128, :], ot[:, :], reads=[ot])
        k.end_stage()

    if "s6" in stages:
        stage_moe_sparse(k, nc, I, C, ident, identb, modT, modP, mod_d, x3_d, out_d, transpose_mod, resid_ln, load_bc_row, new_mv)

    if "s6dense" in stages:
        rw = k.sb("rw", [128, 8, NEXP], F32)
        rb = k.sb("rb", [128, NEXP], F32)
        k.dma("sp", rw[:, :, :], I["router_w"][0].rearrange("(kc p) e -> p kc e", p=128), writes=[rw])
        k.dma("sp", rb[:, :], I["router_b"][0, :].partition_broadcast(128), writes=[rb])
        xts = [k.sb("xt%d" % j, [128, D], F32) for j in range(4)]
        hTs = [k.sb("hT%d" % i, [128, 8, 512], BF16) for i in range(2)]
        hT32 = k.sb("hT32", [128, 8, 512], F32)
        lg = k.sb("lg", [128, NEXP], F32)
        lg2 = k.sb("lg2", [128, NEXP], F32)
        m1 = k.sb("m1", [128, NEXP], F32)
        m2 = k.sb("m2", [128, NEXP], F32)
        sc = k.sb("sc", [128, 8], F32)
        for g in range(TOK // 512):
            b = g // 4
            hT = hTs[g % 2]
            for j in range(4):
                tok = g * 512 + j * 128
                k.dma("sp", xts[j][:, :], x3_d[tok:tok + 128, :], writes=[xts[j]])
            transpose_mod(xts, hT, 1, b, 3, 4, (0, 1), hT32=hT32)
            k.dma("sp", h2T_d[:, :, g * 512:(g + 1) * 512].rearrange("c p t -> p c t"), hT[:, :, :], reads=[hT])
            for j in range(4):
                ti = g * 4 + j
                p = PS[2 + j % 2]
                for kc in range(8):
                    k.mm(p, p[:, 0:NEXP], hT32, hT32[:, kc, j * 128:(j + 1) * 128], rw, rw[:, kc, :], start=(kc == 0), stop=(kc == 7))
                k.tt(lg, lg[:, :], p, p[:, 0:NEXP], rb, rb[:, :], ALU.add)
                S.op("dve", lambda e: e.reduce_max(out=sc[:, 0:1], in_=lg[:, :], axis=AX.X), reads=[lg], writes=[sc])
                k.ts(m1, m1[:, :], lg, lg[:, :], sc[:, 0:1], None, ALU.is_equal, extra_reads=[sc])
                k.stt(lg2, lg2[:, :], m1, m1[:, :], -1e30, lg, lg[:, :], ALU.mult, ALU.add)
                S.op("dve", lambda e: e.reduce_max(out=sc[:, 1:2], in_=lg2[:, :], axis=AX.X), reads=[lg2], writes=[sc])
                k.ts(m2, m2[:, :], lg2, lg2[:, :], sc[:, 1:2], None, ALU.is_equal, extra_reads=[sc])
                k.tt(sc, sc[:, 2:3], sc, sc[:, 0:1], sc, sc[:, 1:2], ALU.subtract)
                k.act(sc, sc[:, 3:4], sc, sc[:, 2:3], AF.Sigmoid)
                k.ts(sc, sc[:, 4:5], sc, sc[:, 3:4], -1.0, 1.0, ALU.mult, ALU.add)
                k.ts(m1, m1[:, :], m1, m1[:, :], sc[:, 3:4], None, ALU.mult, extra_reads=[sc])
                k.stt(gw, gw[:, ti, :], m2, m2[:, :], sc[:, 4:5], m1, m1[:, :], ALU.mult, ALU.add, extra_reads=[sc])
        k.end_stage()
        w1, w3, w2 = ffn_weights(None, None, None)
        hT = [k.sb("hTe%d" % i, [128, 8, 512], BF16) for i in range(2)]
        gT = k.sb("gT", [128, NF, 512], BF16)
        sa_bufs = [k.sb("sa%d" % i, [128, 512], F32) for i in range(2)]
        accs = [k.sb("acc%d" % i, [128, D], F32) for i in range(2)]
        cnt = [0]
        ai = 0
        acc_tr = [T(None, "acc%d" % i) for i in range(TOK // 128)]
        for ex in range(NEXP):
            ffn_load(w1, w3, w2, I["exp_w1"][0, ex], I["exp_w3"][0, ex], I["exp_w2"][0, ex])
            for g in range(TOK // 512):
                h = hT[g % 2]
                k.dma("sp", h[:, :, :], h2T_d[:, :, g * 512:(g + 1) * 512].rearrange("c p t -> p c t"), writes=[h])
                ffn_up(h, w1, w3, gT, sa_bufs, cnt)
                for j in range(4):
                    tok = g * 512 + j * 128
                    ti = g * 4 + j
                    yps = (PS[4 + (j % 2) * 2], PS[5 + (j % 2) * 2])
                    ffn_down(gT, w2, j, yps)
                    acc = accs[ai % 2]
                    ai += 1
                    if ex == 0:
                        for hh in range(2):
                            k.ts(acc, acc[:, hh * 512:(hh + 1) * 512], yps[hh], yps[hh][:, :], gw[:, ti, ex:ex + 1], None, ALU.mult, extra_reads=[gw])
                    else:
                        k.dma("sp", acc[:, :], acc_d[tok:tok + 128, :], reads=[acc_tr[ti]], writes=[acc])
                        for hh in range(2):
                            k.stt(acc, acc[:, hh * 512:(hh + 1) * 512], yps[hh], yps[hh][:, :], gw[:, ti, ex:ex + 1],
                                  acc, acc[:, hh * 512:(hh + 1) * 512], ALU.mult, ALU.add, extra_reads=[gw])
                    k.dma("sp", acc_d[tok:tok + 128, :], acc[:, :], reads=[acc], writes=[acc_tr[ti]])
        k.end_stage()
        lng = load_bc_row("lng", I["ln_g"][1, 1, :])
        lnb = load_bc_row("lnb", I["ln_b"][1, 1, :])
        gbc = [load_bc_row("gbc%d" % b, mod_d[1, b, 5 * D:6 * D], add_one=True) for b in range(NBC)]
        xts = [k.sb("xt%d" % j, [128, D], F32) for j in range(2)]
        ys = [k.sb("ya%d" % j, [128, D], F32) for j in range(2)]
        outs = [k.sb("xo%d" % j, [128, D], F32) for j in range(2)]
        tmp = k.sb("tmp", [128, D], F32)
        r = k.sb("r", [128, D], F32)
        stats = k.sb("stats", [128, 2, 6], F32)
        mv = new_mv("mv")
        for ti in range(TOK // 128):
            b = ti // 16
            tok = ti * 128
            xt, ya, ot = xts[ti % 2], ys[ti % 2], outs[ti % 2]
            k.dma("sp", xt[:, :], x3_d[tok:tok + 128, :], writes=[xt])
            k.dma("sp", ya[:, :], acc_d[tok:tok + 128, :], writes=[ya])
            resid_ln(xt, ya, gbc[b], lng, lnb, tmp, r, ot, stats, mv)
            k.dma("sp", out_d[tok:tok + 128, :], ot[:, :], reads=[ot])
        k.end_stage()

    S.barrier()
    with ExitStack() as st:
        S.emit(st)
    return nc, k


def skewed(items):
    n = len(items)
    if n and len(items[0]) == 3:
        for t in range(-2, n):
            if 0 <= t + 2 < n:
                items[t + 2][0]()
            if 0 <= t + 1 < n:
                items[t + 1][1]()
            if 0 <= t < n:
                items[t][2]()
        return
    depth = 2
    for i in range(min(depth, n)):
        items[i][0]()
    for i in range(n):
        if i + depth < n:
            items[i + depth][0]()
        items[i][1]()


def stage_attention(k, nc, I, C, dt_, ident, identb, qkT_d, vtok_d, gates_d, kcT_d, vc_d, oT_d):
    S = k.S
    PS = k.ps
    TINY = 1e-30
    cb = k.sb("cb", [128, 512], BF16)
    cbs = k.sb("cbs", [128, 512], BF16)
    wbm = k.sb("wbm", [128, 512], BF16)
    negtri = k.sb("negtri", [128, 128], BF16)
    negones = k.sb("negones", [128, 128], BF16)
    cmpbias = k.sb("cmpbias", [128, SEQ], BF16)
    expand = k.sb("expand", [32, 16, 128], BF16)
    selA = k.sb("selA", [128, 16, 32], F32)
    selB = k.sb("selB", [128, 16, 32], F32)
    for t, n in ((cb, "cb"), (cbs, "cbs"), (wbm, "wb"), (negtri, "negtri"), (negones, "negones"), (cmpbias, "cmpbias")):
        k.dma("pool", t[:, :], C[n], writes=[t])
    k.dma("pool", expand[:, :, :], C["expand"], writes=[expand])
    k.dma("sp", selA[:, :, :], C["selA"], writes=[selA])
    k.dma("sp", selB[:, :, :], C["selB"], writes=[selB])
    QK_IDX = [0, 1, 2, 3, 4, 5, 6, 7, 8, 9, 10, 11, 14, 15, 16, 17]
    qk = {ci: k.sb("qk%d" % ci, [128, SEQ], BF16) for ci in QK_IDX}
    sbv = k.sb("sbv", [128, 16, 512], BF16)
    vslc = k.sb("vslc", [128, 16, 2, 66], BF16)
    vwin = k.sb("vwin", [128, 16, 2, 66], BF16)
    gates = k.sb("gates", [128, 16, 24], F32)
    kcT = k.sb("kcT", [128, 2, 128], BF16)
    vcaug = k.sb("vcaug", [128, 2, 98], BF16)
    selT = k.sb("selT", [32, 2, SEQ], BF16)
    imp = k.sb("imp", [128, 16, 2, 32], F32)
    imp2 = k.sb("imp2", [128, 16, 32], F32)
    cmp3 = k.sb("cmp3", [128, 32, 32], BF16)
    rank = k.sb("rank", [128, 32], F32)
    selbias = k.sb("selbias", [128, 16, 32], BF16)
    rd = [k.sb("rd%d" % i, [128, 8], F32) for i in range(4)]
    tmpi = k.sb("tmpi", [128, 4, 32], F32)
    tmpo = [k.sb("tmpo%d" % i, [128, 4, 64], F32) for i in range(2)]
    Ecmp = [k.sb("Ecmp%d" % i, [128, 512], BF16) for i in range(2)]
    Pb = [k.sb("Pb%d" % i, [128, 512], BF16) for i in range(3)]
    e32 = [k.sb("e32_%d" % i, [128, 512], F32) for i in range(2)]
    spb = [k.sb("spb%d" % i, [128, 512], BF16) for i in range(4)]
    Wb = [k.sb("Wb%d" % i, [128, 512], BF16) for i in range(3)]
    accb = k.sb("accb", [128, 512], BF16)
    onsa = [k.sb("onsa%d" % i, [128, 4, 128], F32) for i in range(2)]
    onsab = [k.sb("onsab%d" % i, [128, 4, 128], BF16) for i in range(2)]
    ostage = [k.sb("ostage%d" % i, [128, 512], BF16) for i in range(3)]
    k.memset(vslc, vslc[:, :, :, 64:66], 1.0)
    k.memset(vwin, vwin[:, :, :, 64:66], 1.0)
    k.memset(vcaug, vcaug[:, :, 64:65], 1.0)
    for g in range(2):
        k.dma("pool", vcaug[:, g, 65:97], C["overlap"], writes=[vcaug])
    ctr = {}

    def nxt(name, n):
        v = ctr.get(name, 0)
        ctr[name] = v + 1
        return v % n

    for b in range(NBC):
        for ci in QK_IDX:
            k.dma("sp", qk[ci][:, :], qkT_d[b, ci, :, :], writes=[qk[ci]])
        k.dma("sp", sbv[:, :, :], vtok_d[b, :, 0:512].rearrange("(t p) c -> p t c", p=128), writes=[sbv])
        for g in range(2):
            k.dma("sp", vslc[:, :, g, 0:64], vtok_d[b, :, 512 + g * 64:512 + (g + 1) * 64].rearrange("(t p) c -> p t c", p=128), writes=[vslc])
            k.dma("sp", vwin[:, :, g, 0:64], vtok_d[b, :, 640 + g * 64:640 + (g + 1) * 64].rearrange("(t p) c -> p t c", p=128), writes=[vwin])
            k.dma("sp", kcT[:, g, :], kcT_d[b, g], writes=[kcT])
            k.dma("sp", vcaug[:, g, 0:64], vc_d[b, g], writes=[vcaug])
        k.dma("sp", gates[:, :, :], gates_d[b].rearrange("(t p) c -> p t c", p=128), writes=[gates])

        def cmp_scores(g, hq, qg, ncols_rhs):
            chunk = 8 + hq // 2
            po = 64 * (hq % 2)
            t0 = qg * 512
            p = PS[nxt("cs", 2)]
            k.mm(p, p[:, :], kcT, kcT[po:po + 64, g, :], qk[chunk], qk[chunk][po:po + 64, t0:t0 + 512], start=True, stop=False)
            k.mm(p, p[:, :], identb, identb[:, :], cmpbias, cmpbias[:, t0:t0 + 512], start=False, stop=True)
            E = Ecmp[nxt("ec", 2)]
            k.act(E, E[:, :], p, p[:, :], AF.Exp)
            R = PS[2 + nxt("cr", 2)]
            for sub in range(4):
                k.mm(R, R[:, sub * 128:sub * 128 + ncols_rhs], E, E[:, sub * 128:(sub + 1) * 128], vcaug, vcaug[:, g, 0:ncols_rhs],
                     start=True, stop=True)
            return R

        def recip_den(R, col):
            rdt = rd[nxt("rd", 4)]
            Rv = R[:, :].rearrange("p (s c) -> p s c", s=4)
            k.ts(rdt, rdt[:, 0:4], R, Rv[:, :, col], TINY, None, ALU.max)
            S.op("dve", lambda e: e.reciprocal(out=rdt[:, 0:4], in_=rdt[:, 0:4]), reads=[rdt], writes=[rdt])
            return rdt, Rv

        for g in range(2):
            for r in range(4):
                hq = 4 * g + r
                for qg in range(4):
                    R = cmp_scores(g, hq, qg, 97)
                    rdt, Rv = recip_den(R, 64)
                    bc = rdt[:, 0:4].unsqueeze(2).broadcast_to([128, 4, 32])
                    if r == 0:
                        k.tt(imp, imp[:, 4 * qg:4 * qg + 4, g, :], R, Rv[:, :, 65:97], rdt, bc, ALU.mult)
                    else:
                        k.tt(tmpi, tmpi[:, :, :], R, Rv[:, :, 65:97], rdt, bc, ALU.mult)
                        k.tt(imp, imp[:, 4 * qg:4 * qg + 4, g, :], imp, imp[:, 4 * qg:4 * qg + 4, g, :], tmpi, tmpi[:, :, :], ALU.add)
        for g in range(2):
            k.tt(imp2, imp2[:, :, :], imp, imp[:, :, g, :], selA, selA[:, :, :], ALU.mult)
            k.tt(imp2, imp2[:, :, :], imp2, imp2[:, :, :], selB, selB[:, :, :], ALU.add)
            for tl in range(16):
                a = imp2[:, tl, :]
                in0 = a.unsqueeze(1).broadcast_to([128, 32, 32])
                in1 = a.unsqueeze(2).broadcast_to([128, 32, 32])
                k.tt(cmp3, cmp3[:, :, :], imp2, in0, imp2, in1, ALU.is_gt)
                S.op("dve", lambda e: e.reduce_sum(out=rank[:, :], in_=cmp3[:, :, :], axis=AX.X), reads=[cmp3], writes=[rank])
                k.ts(selbias, selbias[:, tl, :], rank, rank[:, :], 15.5, NEGB, ALU.is_gt, ALU.mult)
            for half in range(2):
                p = PS[4 + half]
                pv = p[:, :].bitcast(BF16)
                for t8 in range(8):
                    tl = half * 8 + t8
                    k.tr(p, pv[0:32, t8 * 128:(t8 + 1) * 128], selbias, selbias[:, tl, :], identb, identb[:, :])
                k.cp(selT, selT[0:32, g, half * 1024:(half + 1) * 1024], p, pv[0:32, 0:1024], eng="act")

        for g in range(2):
            for pair in range(2):
                for qg in range(4):
                    t0 = qg * 512
                    on = onsa[nxt("on", 2)]
                    for r2 in range(2):
                        hq = 4 * g + 2 * pair + r2
                        chunk = 8 + hq // 2
                        po = 64 * r2
                        qT = qk[chunk]
                        R = cmp_scores(g, hq, qg, 65)
                        rdt, Rv = recip_den(R, 64)
                        k.tt(rdt, rdt[:, 4:8], rdt, rdt[:, 0:4], gates, gates[:, 4 * qg:4 * qg + 4, hq * 3 + 0], ALU.mult)
                        k.tt(on, on[:, :, po:po + 64], R, Rv[:, :, 0:64], rdt, rdt[:, 4:8].unsqueeze(2).broadcast_to([128, 4, 64]), ALU.mult)
                        for br in range(2):
                            kT = qk[14 + g] if br == 0 else qk[16 + g]
                            vaug = vslc if br == 0 else vwin
                            ACC = PS[4 + br]
                            if br == 0:
                                kbs = list(range(0, 4 * qg + 4))
                            else:
                                kbs = list(range(max(0, 4 * qg - 4), 4 * qg + 4))
                            started = [False] * 4
                            items = []
                            for kb in kbs:
                                diag = kb >= 4 * qg
                                if diag:
                                    o = (kb - 4 * qg) * 128
                                    c0, c1 = o, 512
                                    bias_t, bias_ap = cb, cb[:, 0:512 - o]
                                    subs = list(range(o // 128, 4))
                                elif br == 1:
                                    m = kb - (4 * qg - 4)
                                    c0, c1 = 0, 128 * (m + 1)
                                    bias_t, bias_ap = wbm, wbm[:, 384 - 128 * m:512]
                                    subs = list(range(0, m + 1))
                                else:
                                    c0, c1 = 0, 512
                                    bias_t, bias_ap = None, None
                                    subs = [0, 1, 2, 3]
                                nco = c1 - c0
                                Pt = Pb[nxt("pt", 3)]
                                sp_ = PS[6 + nxt("ss", 2)]

                                def s1(kb=kb, c0=c0, c1=c1, nco=nco, bias_t=bias_t, bias_ap=bias_ap, Pt=Pt, sp_=sp_, kT=kT, br=br):
                                    k.mm(sp_, sp_[:, 0:nco], kT, kT[po:po + 64, kb * 128:(kb + 1) * 128], qT, qT[po:po + 64, t0 + c0:t0 + c1],
                                         start=True, stop=(br == 1 and bias_t is None))
                                    if br == 0:
                                        k.mm(sp_, sp_[:, 0:nco], expand, expand[0:32, kb, :], selT, selT[0:32, g, t0 + c0:t0 + c1],
                                             start=False, stop=(bias_t is None))
                                    if bias_t is not None:
                                        k.mm(sp_, sp_[:, 0:nco], identb, identb[:, :], bias_t, bias_ap, start=False, stop=True)
                                    k.act(Pt, Pt[:, 0:nco], sp_, sp_[:, 0:nco], AF.Exp)

                                def s2(kb=kb, c0=c0, subs=subs, Pt=Pt, vaug=vaug, ACC=ACC, started=started, last_kb=kbs[-1], qg=qg):
                                    for sub in subs:
                                        lo = sub * 128 - c0
                                        is_last = (kb == 4 * qg + sub)
                                        k.mm(ACC, ACC[:, sub * 128:sub * 128 + 65], Pt, Pt[:, lo:lo + 128], vaug, vaug[:, kb, g, 0:65],
                                             start=(not any(started)), stop=is_last, skip=True)
                                        started[sub] = True
                                items.append((s1, s2))
                            skewed(items)
                            rdt, Av = recip_den(ACC, 64)
                            k.tt(rdt, rdt[:, 4:8], rdt, rdt[:, 0:4], gates, gates[:, 4 * qg:4 * qg + 4, hq * 3 + 1 + br], ALU.mult)
                            to = tmpo[nxt("to", 2)]
                            k.tt(to, to[:, :, :], ACC, Av[:, :, 0:64], rdt, rdt[:, 4:8].unsqueeze(2).broadcast_to([128, 4, 64]), ALU.mult)
                            k.tt(on, on[:, :, po:po + 64], on, on[:, :, po:po + 64], to, to[:, :, :], ALU.add)
                    onb = onsab[nxt("onb", 2)]
                    k.cp(onb, onb[:, :, :], on, on[:, :, :], eng="dve")
                    p = PS[2 + nxt("cr", 2)]
                    pv = p[:, :].bitcast(BF16)
                    for sub in range(4):
                        k.tr(p, pv[:, sub * 128:(sub + 1) * 128], onb, onb[:, sub, :], identb, identb[:, :])
                    ost = ostage[nxt("os", 3)]
                    k.cp(ost, ost[:, :], p, pv[:, 0:512], eng="act")
                    k.dma("sp", oT_d[b, 4 + 2 * g + pair, :, t0:t0 + 512], ost[:, :], reads=[ost])

        items = []
        for h in range(8):
            c = h // 2
            po = 64 * (h % 2)
            qT = qk[c]
            kT = qk[4 + c]
            for qg in range(4):
                t0 = qg * 512
                OT = PS[4 + ((h * 4 + qg) % 2)]
                kbs = list(range(4 * qg + 3, -1, -1))
                for bi, kb in enumerate(kbs):
                    diag = kb >= 4 * qg
                    o = (kb - 4 * qg) * 128 if diag else 0
                    nco = 512 - o
                    first = (bi == 0)

                    def s1(kb=kb, o=o, nco=nco, diag=diag, qT=qT, kT=kT, po=po, t0=t0, st={}):
                        p1 = PS[nxt("d1", 2)]
                        k.mm(p1, p1[:, 0:nco], kT, kT[po:po + 64, kb * 128:(kb + 1) * 128], qT, qT[po:po + 64, t0 + o:t0 + 512],
                             start=True, stop=(not diag))
                        if diag:
                            k.mm(p1, p1[:, 0:nco], identb, identb[:, :], cbs, cbs[:, 0:nco], start=False, stop=True)
                        e = e32[nxt("e", 2)]
                        k.act(e, e[:, 0:nco], p1, p1[:, 0:nco], AF.Exp)
                        spt = spb[nxt("sp", 4)]
                        k.act(spt, spt[:, 0:nco], e, e[:, 0:nco], AF.Ln, bias=1.0)
                        st["sp"] = spt

                    items.append([s1, None, dict(kb=kb, o=o, nco=nco, diag=diag, first=first, qT=qT, kT=kT, po=po, t0=t0, OT=OT, c=c, h=h, qg=qg)])
        def make_s2(s1, d):
            st = s1.__defaults__[-1]

            def s2():
                kb, o, nco, diag, first = d["kb"], d["o"], d["nco"], d["diag"], d["first"]
                qT, kT, po, t0, OT, c, h, qg = d["qT"], d["kT"], d["po"], d["t0"], d["OT"], d["c"], d["h"], d["qg"]
                spt = st["sp"]
                p2 = PS[2 + nxt("d2", 2)]
                k.mm(p2, p2[:, 0:nco], kT, kT[po:po + 64, kb * 128:(kb + 1) * 128], qT, qT[po:po + 64, t0 + o:t0 + 512], start=True, stop=False)
                if diag:
                    k.mm(p2, p2[:, 0:nco], identb, identb[:, :], cbs, cbs[:, 0:nco], start=False, stop=False)
                has_acc = not first
                k.mm(p2, p2[:, 0:nco], negtri, negtri[:, :], spt, spt[:, 0:nco], start=False, stop=(not has_acc))
                if has_acc:
                    if diag:
                        k.mm(p2, p2[:, 128:nco], negones, negones[:, :], accb, accb[:, o + 128:512], start=False, stop=True)
                    else:
                        k.mm(p2, p2[:, 0:512], negones, negones[:, :], accb, accb[:, 0:512], start=False, stop=True)
                W = Wb[nxt("w", 3)]
                st["W"] = W
                k.act(W, W[:, 0:nco], p2, p2[:, 0:nco], AF.Exp)
                if diag:
                    k.cp(accb, accb[:, o:o + 128], spt, spt[:, 0:128], eng="dve")
                    if nco > 128:
                        k.tt(accb, accb[:, o + 128:512], accb, accb[:, o + 128:512], spt, spt[:, 128:nco], ALU.add)
                else:
                    k.tt(accb, accb[:, :], accb, accb[:, :], spt, spt[:, :], ALU.add)

            def s3():
                kb, o, nco, diag, first = d["kb"], d["o"], d["nco"], d["diag"], d["first"]
                po, t0, OT, c = d["po"], d["t0"], d["OT"], d["c"]
                W = st["W"]
                last = (kb == 0)
                vl = sbv[:, kb, c * 128:(c + 1) * 128]
                if diag:
                    k.mm(OT, OT[:, o:o + 128], sbv, vl, W, W[:, 0:128], start=first, stop=last, skip=True)
                    if nco > 128:
                        k.mm(OT, OT[:, o + 128:512], sbv, vl, W, W[:, 128:nco], start=False, stop=last, skip=True)
                else:
                    k.mm(OT, OT[:, 0:512], sbv, vl, W, W[:, 0:512], start=False, stop=last, skip=True)
                if last:
                    ost = ostage[nxt("os", 3)]
                    k.cp(ost, ost[po:po + 64, :], OT, OT[po:po + 64, :], eng="dve")
                    k.dma("sp", oT_d[b, c, po:po + 64, t0:t0 + 512], ost[po:po + 64, :], reads=[ost])
            return s2, s3
        its = [(it[0],) + make_s2(it[0], it[2]) for it in items]
        skewed(its)


def stage_moe_sparse(k, nc, I, C, ident, identb, modT, modP, mod_d, x3_d, out_d, transpose_mod, resid_ln, load_bc_row, new_mv):
    S = k.S
    PS = k.ps
    NT = TOK // 128
    NB = MOE_NBLK
    BIG = 100000.0
    dk = "ExternalOutput" if DBG.get("moe_dbg") else "Internal"
    xs_d = nc.dram_tensor("xs_d", [NSLOT, D], BF16, kind=dk).ap()
    yo_d = nc.dram_tensor("yo_d", [NSLOT, D], F32, kind=dk).ap()
    gwt = k.pers("gwt", [128, NT, 2], F32)
    sloti = k.pers("sloti", [128, NT, 2], I32)
    widx1 = k.pers("widx1", [128, NB, 16], I32)
    widx2 = k.pers("widx2", [128, NB, NF], I32)

    rw = k.sb("rw", [128, 8, NEXP], F32)
    rb = k.sb("rb", [128, NEXP], F32)
    trilt = k.sb("trilt", [128, 128], BF16)
    onesb = k.sb("onesb", [128, 128], BF16)
    pidx = k.sb("pidx", [128, 1], F32)
    thr = k.sb("thr", [128, NB + 8], F32)
    k.dma("sp", rw[:, :, :], I["router_w"][0].rearrange("(kc p) e -> p kc e", p=128), writes=[rw])
    k.dma("sp", rb[:, :], I["router_b"][0, :].partition_broadcast(128), writes=[rb])
    k.dma("pool", trilt[:, :], C["trilt"], writes=[trilt])
    k.dma("pool", onesb[:, :], C["ones"], writes=[onesb])
    k.dma("sp", pidx[:, :], C["pidx"], writes=[pidx])
    k.dma("sp", thr[:, :], C["thr"], writes=[thr])
    zt = k.sb("zt", [128, 16384], BF16)
    k.memset(zt, zt[:, :], 0.0)
    xs_tr = T(None, "xs_dram")
    xs_flat = xs_d.rearrange("(p a) c -> p (a c)", p=128)
    for i in range(NSLOT * D // 128 // 16384):
        k.dma("sp", xs_flat[:, i * 16384:(i + 1) * 16384], zt[:, :], reads=[zt], writes=[xs_tr])
    scb = [load_bc_row("scb%d" % b, mod_d[1, b, 4 * D:5 * D], add_one=True) for b in range(NBC)]
    shb = [load_bc_row("shb%d" % b, mod_d[1, b, 3 * D:4 * D]) for b in range(NBC)]
    xts = [k.sb("xt%d" % j, [128, D], F32) for j in range(4)]
    hT32 = k.sb("hT32", [128, 8, 512], F32)
    h2tok = k.sb("h2tok", [128, NT, D], BF16)
    tmp32 = k.sb("tmp32", [128, D], F32)
    posall = k.sb("posall", [128, NT, NEXP], F32)
    m1s = k.sb("m1s", [128, NT, NEXP], F32)
    m2s = k.sb("m2s", [128, NT, NEXP], F32)
    Macc = k.sb("Macc", [128, NEXP], F32)
    Maccb = k.sb("Maccb", [128, NEXP], BF16)
    Mbs = [k.sb("Mb%d" % i, [128, NEXP], BF16) for i in range(2)]
    lg = k.sb("lg", [128, NEXP], F32)
    lg2 = k.sb("lg2", [128, NEXP], F32)
    sc = k.sb("sc", [128, 8], F32)
    k.memset(Macc, Macc[:, :], 0.0)
    k.memset(Maccb, Maccb[:, :], 0.0)
    for g in range(TOK // 512):
        b = g // 4
        for j in range(4):
            tok = g * 512 + j * 128
            k.dma("sp", xts[j][:, :], x3_d[tok:tok + 128, :], writes=[xts[j]])
        transpose_mod(xts, None, 1, b, 3, 4, (0, 1), hT32=hT32)
        for j in range(4):
            ti = g * 4 + j
            k.tt(tmp32, tmp32[:, :], xts[j], xts[j][:, :], scb[b], scb[b][:, :], ALU.mult)
            k.tt(h2tok, h2tok[:, ti, :], tmp32, tmp32[:, :], shb[b], shb[b][:, :], ALU.add)
            p = PS[2 + j % 2]
            for kc in range(8):
                k.mm(p, p[:, 0:NEXP], hT32, hT32[:, kc, j * 128:(j + 1) * 128], rw, rw[:, kc, :], start=(kc == 0), stop=(kc == 7))
            k.tt(lg, lg[:, :], p, p[:, 0:NEXP], rb, rb[:, :], ALU.add)
            S.op("dve", lambda e: e.reduce_max(out=sc[:, 0:1], in_=lg[:, :], axis=AX.X), reads=[lg], writes=[sc])
            k.ts(m1s, m1s[:, ti, :], lg, lg[:, :], sc[:, 0:1], None, ALU.is_equal, extra_reads=[sc])
            k.stt(lg2, lg2[:, :], m1s, m1s[:, ti, :], -1e30, lg, lg[:, :], ALU.mult, ALU.add)
            S.op("dve", lambda e: e.reduce_max(out=sc[:, 1:2], in_=lg2[:, :], axis=AX.X), reads=[lg2], writes=[sc])
            k.ts(m2s, m2s[:, ti, :], lg2, lg2[:, :], sc[:, 1:2], None, ALU.is_equal, extra_reads=[sc])
            k.tt(sc, sc[:, 2:3], sc, sc[:, 0:1], sc, sc[:, 1:2], ALU.subtract)
            k.act(gwt, gwt[:, ti, 0:1], sc, sc[:, 2:3], AF.Sigmoid)
            k.ts(gwt, gwt[:, ti, 1:2], gwt, gwt[:, ti, 0:1], -1.0, 1.0, ALU.mult, ALU.add)
            Mb = Mbs[ti % 2]
            k.tt(Mb, Mb[:, :], m1s, m1s[:, ti, :], m2s, m2s[:, ti, :], ALU.add)
            pp = PS[4 + ti % 2]
            k.mm(pp, pp[:, 0:NEXP], trilt, trilt[:, :], Mb, Mb[:, :], start=True, stop=(ti == 0))
            if ti > 0:
                k.mm(pp, pp[:, 0:NEXP], onesb, onesb[:, :], Maccb, Maccb[:, :], start=False, stop=True)
            k.cp(posall, posall[:, ti, :], pp, pp[:, 0:NEXP])
            k.tt(Macc, Macc[:, :], Macc, Macc[:, :], Mb, Mb[:, :], ALU.add)
            k.cp(Maccb, Maccb[:, :], Macc, Macc[:, :])
    cnt = k.sb("cnt", [128, NEXP], F32)
    cmpA = k.sb("cmpA", [128, NEXP, 8], F32)
    nblk = k.sb("nblk", [128, NEXP], F32)
    pst = k.sb("pst", [128, NEXP + 1], F32)
    cmpB = k.sb("cmpB", [128, NB, NEXP], F32)
    blk = k.sb("blk", [128, NB], F32)
    b1 = k.sb("b1", [128, NB], F32)
    b2 = k.sb("b2", [128, NB], F32)
    pidx2 = k.sb("pidx2", [128, 1], F32)
    w1f = k.sb("w1f", [128, NB, 16], F32)
    w2f = k.sb("w2f", [128, NB, NF], F32)
    posp = k.sb("posp", [128, NT, NEXP], F32)
    slotf = k.sb("slotf", [128, NT, 2], F32)
    pc = PS[6]
    k.mm(pc, pc[:, 0:NEXP], onesb, onesb[:, :], Maccb, Maccb[:, :], start=True, stop=True)
    k.cp(cnt, cnt[:, :], pc, pc[:, 0:NEXP])
    k.tt(cmpA, cmpA[:, :, :], cnt, cnt[:, :].unsqueeze(2).broadcast_to([128, NEXP, 8]),
         thr, thr[:, 0:8].unsqueeze(1).broadcast_to([128, NEXP, 8]), ALU.is_gt)
    S.op("dve", lambda e: e.reduce_sum(out=nblk[:, :], in_=cmpA[:, :, :], axis=AX.X), reads=[cmpA], writes=[nblk])
    k.memset(pst, pst[:, :], 0.0)
    for ex in range(NEXP):
        k.stt(pst, pst[:, ex + 1:ex + 2], nblk, nblk[:, ex:ex + 1], float(MOE_BS), pst, pst[:, ex:ex + 1], ALU.mult, ALU.add)
    k.tt(cmpB, cmpB[:, :, :], pst, pst[:, 1:NEXP + 1].unsqueeze(1).broadcast_to([128, NB, NEXP]),
         thr, thr[:, 0:NB].unsqueeze(2).broadcast_to([128, NB, NEXP]), ALU.is_le)
    S.op("dve", lambda e: e.reduce_sum(out=blk[:, :], in_=cmpB[:, :, :], axis=AX.X), reads=[cmpB], writes=[blk])
    k.ts(blk, blk[:, :], blk, blk[:, :], float(NEXP - 1), None, ALU.min)
    k.ts(pidx2, pidx2[:, :], pidx, pidx[:, :], 2.0, None, ALU.mult)
    k.ts(b1, b1[:, :], blk, blk[:, :], 2048.0, pidx2[:, 0:1], ALU.mult, ALU.add, extra_reads=[pidx2])
    k.ts(b2, b2[:, :], blk, blk[:, :], float(DFF), pidx[:, 0:1], ALU.mult, ALU.add, extra_reads=[pidx])
    for c in range(16):
        kc, h = c // 2, c % 2
        k.ts(w1f, w1f[:, :, c], b1, b1[:, :], float(kc * 256 + h), None, ALU.add)
    for f in range(NF):
        k.ts(w2f, w2f[:, :, f], b2, b2[:, :], float(f * 128), None, ALU.add)
    k.cp(widx1, widx1[:, :, :], w1f, w1f[:, :, :])
    k.cp(widx2, widx2[:, :, :], w2f, w2f[:, :, :])
    k.tt(posp, posp[:, :, :], posall, posall[:, :, :], pst, pst[:, 0:NEXP].unsqueeze(1).broadcast_to([128, NT, NEXP]), ALU.add)
    for kk, ms in ((0, m1s), (1, m2s)):
        k.tt(ms, ms[:, :, :], ms, ms[:, :, :], posp, posp[:, :, :], ALU.mult)
        S.op("dve", (lambda kk_, ms_: (lambda e: e.reduce_sum(out=slotf[:, :, kk_], in_=ms_[:, :, :], axis=AX.X)))(kk, ms),
             reads=[ms], writes=[slotf])
    k.cp(sloti, sloti[:, :, :], slotf, slotf[:, :, :])
    if DBG.get("moe_dbg"):
        md = nc.dram_tensor("moe_dbg", [128, 512], F32, kind="ExternalOutput").ap()
        k.dma("sp", md[:, 0:64], slotf[:, :, :].rearrange("p a b -> p (a b)"), reads=[slotf])
        k.dma("sp", md[:, 64:128], gwt[:, :, :].rearrange("p a b -> p (a b)"), reads=[gwt])
        k.dma("sp", md[:, 128:128 + NB], blk[:, :], reads=[blk])
        k.dma("sp", md[:, 160:169], pst[:, :], reads=[pst])
        k.dma("sp", md[:, 176:184], cnt[:, :], reads=[cnt])
        k.dma("sp", md[:, 256:512], posall[:, :, :].rearrange("p a b -> p (a b)"), reads=[posall])
    for ti in range(NT):
        for kk in range(2):
            k.scatter(xs_d, h2tok[:, ti, :], sloti[:, ti, kk:kk + 1], reads=[h2tok, sloti, xs_tr])
    k.end_stage()
    if DBG.get("moe_stop") == "a":
        return

    w1h = k.sb("ew1", [128, 8, DFF], BF16)
    w3h = k.sb("ew3", [128, 8, DFF], BF16)
    w2h = k.sb("ew2", [128, NF, D], BF16)
    w1T = [[k.view(w1h, "ew1_%d_%d" % (i, h)) for h in range(2)] for i in range(8)]
    w3T = [[k.view(w3h, "ew3_%d_%d" % (i, h)) for h in range(2)] for i in range(8)]
    w2T = [k.view(w2h, "ew2_%d" % i) for i in range(NF)]
    xt = [k.sb("xs%d" % j, [128, D], BF16) for j in range(4)]
    hTs = [k.sb("hTe%d" % i, [128, 8, 512], BF16) for i in range(2)]
    gT = k.sb("gT", [128, NF, 512], BF16)
    sab = [k.sb("sa%d" % i, [128, 512], F32) for i in range(2)]
    yo = [k.sb("yo%d" % i, [128, D], F32) for i in range(2)]
    HALF = DFF // 2
    w1src = I["exp_w1"][0].rearrange("e r c -> (e r) c")
    w3src = I["exp_w3"][0].rearrange("e r c -> (e r) c")
    w2src = I["exp_w2"][0].rearrange("e f n -> (e f) n")
    cntr = [0, 0]
    for kb in range(DBG.get("moe_nb", NB)):
        for h in range(2):
            for kc in range(8):
                k.gather(w1h[:, kc, h * HALF:(h + 1) * HALF], w1src, widx1[:, kb, kc * 2 + h:kc * 2 + h + 1],
                         reads=[widx1], writes=[w1T[kc][h]])
                k.gather(w3h[:, kc, h * HALF:(h + 1) * HALF], w3src, widx1[:, kb, kc * 2 + h:kc * 2 + h + 1],
                         reads=[widx1], writes=[w3T[kc][h]])
        for f in range(NF):
            k.gather(w2h[:, f, :], w2src, widx2[:, kb, f:f + 1], reads=[widx2], writes=[w2T[f]])
        for j in range(4):
            r0 = kb * MOE_BS + j * 128
            k.dma("sp", xt[j][:, :], xs_d[r0:r0 + 128, :], writes=[xt[j]])
        hT = hTs[kb % 2]
        for c in range(8):
            pb = PS[4 + c % 2]
            pv = pb[:, :].bitcast(BF16)
            for j in range(4):
                k.tr(pb, pv[:, j * 128:(j + 1) * 128], xt[j], xt[j][:, c * 128:(c + 1) * 128], identb, identb[:, :])
            k.cp(hT, hT[:, c, :], pb, pv[:, 0:512], eng="act")
        for f in range(NF):
            pa = PS[(cntr[0] % 2) * 2]
            pb = PS[(cntr[0] % 2) * 2 + 1]
            sa = sab[cntr[0] % 2]
            cntr[0] += 1
            fh = 0 if f < NF // 2 else 1
            for kc in range(8):
                k.mm(pa, pa[:, :], w1T[kc][fh], w1h[:, kc, f * 128:(f + 1) * 128], hT, hT[:, kc, :], start=(kc == 0), stop=(kc == 7))
            for kc in range(8):
                k.mm(pb, pb[:, :], w3T[kc][fh], w3h[:, kc, f * 128:(f + 1) * 128], hT, hT[:, kc, :], start=(kc == 0), stop=(kc == 7))
            k.act(sa, sa[:, :], pa, pa[:, :], AF.Silu)
            k.tt(gT, gT[:, f, :], sa, sa[:, :], pb, pb[:, :], ALU.mult)
        for j in range(4):
            yps = (PS[4 + (j % 2) * 2], PS[5 + (j % 2) * 2])
            for hh in range(2):
                for f in range(NF):
                    k.mm(yps[hh], yps[hh][:, :], gT, gT[:, f, j * 128:(j + 1) * 128], w2T[f], w2h[:, f, hh * 512:(hh + 1) * 512],
                         start=(f == 0), stop=(f == NF - 1))
            y = yo[cntr[1] % 2]
            cntr[1] += 1
            k.cp(y, y[:, 0:512], yps[0], yps[0][:, :], eng="act")
            k.cp(y, y[:, 512:1024], yps[1], yps[1][:, :], eng="dve")
            r0 = kb * MOE_BS + j * 128
            k.dma("sp", yo_d[r0:r0 + 128, :], y[:, :], reads=[y])
    k.end_stage()
    if DBG.get("moe_stop") == "b":
        return

    lng = load_bc_row("lng", I["ln_g"][1, 1, :])
    lnb = load_bc_row("lnb", I["ln_b"][1, 1, :])
    gbc = [load_bc_row("gbc%d" % b, mod_d[1, b, 5 * D:6 * D], add_one=True) for b in range(NBC)]
    xts = [k.sb("xt%d" % j, [128, D], F32) for j in range(2)]
    r1s = [k.sb("r1_%d" % j, [128, D], F32) for j in range(2)]
    r2s = [k.sb("r2_%d" % j, [128, D], F32) for j in range(2)]
    outs = [k.sb("xo%d" % j, [128, D], F32) for j in range(2)]
    tmp = k.sb("tmp", [128, D], F32)
    r = k.sb("r", [128, D], F32)
    stats = k.sb("stats", [128, 2, 6], F32)
    mv = new_mv("mv")
    for ti in range(NT):
        b = ti // (SEQ // 128)
        tok = ti * 128
        xt_, r1, r2, ot = xts[ti % 2], r1s[ti % 2], r2s[ti % 2], outs[ti % 2]
        k.dma("sp", xt_[:, :], x3_d[tok:tok + 128, :], writes=[xt_])
        k.gather(r1[:, :], yo_d, sloti[:, ti, 0:1], reads=[sloti], writes=[r1])
        k.gather(r2[:, :], yo_d, sloti[:, ti, 1:2], reads=[sloti], writes=[r2])
        k.ts(r1, r1[:, :], r1, r1[:, :], gwt[:, ti, 0:1], None, ALU.mult, extra_reads=[gwt])
        k.stt(r1, r1[:, :], r2, r2[:, :], gwt[:, ti, 1:2], r1, r1[:, :], ALU.mult, ALU.add, extra_reads=[gwt])
        resid_ln(xt_, r1, gbc[b], lng, lnb, tmp, r, ot, stats, mv)
        k.dma("sp", out_d[tok:tok + 128, :], ot[:, :], reads=[ot])
    k.end_stage()


_CACHE = {}


def kernel(**inputs):
    if "nc" not in _CACHE:
        _CACHE["nc"] = build_program()[0]
        _CACHE["cst"] = host_consts()
    nc = _CACHE["nc"]
    cst = _CACHE["cst"]
    x = np.ascontiguousarray(inputs["x"], dtype=np.float32)
    c = np.ascontiguousarray(inputs["c"], dtype=np.float32)
    in_maps = []
    for core in range(NCORES):
        m = {}
        for n in INPUT_SHAPES:
            if n == "x":
                m[n] = x[core * NBC:(core + 1) * NBC].reshape(TOK, D)
            elif n == "c":
                m[n] = c[core * NBC:(core + 1) * NBC]
            else:
                m[n] = np.ascontiguousarray(inputs[n], dtype=np.float32).reshape(INPUT_SHAPES[n])
        for n, v in cst.items():
            m["c_" + n] = v
        in_maps.append(m)
    res = run_bass_kernel_spmd(nc, in_maps, core_ids=list(range(NCORES)))
    outs = [np.asarray(r["out"]).reshape(NBC, SEQ, D) for r in res.results]
    return np.concatenate(outs, axis=0).astype(np.float32)
```

```python
import numpy as np
from contextlib import ExitStack
import concourse.bass as bass
import concourse.mybir as mybir
from concourse.bass_utils import run_bass_kernel_spmd

F32 = mybir.dt.float32
BF16 = mybir.dt.bfloat16
AF = mybir.ActivationFunctionType
ALU = mybir.AluOpType
AX = mybir.AxisListType

NCORES = 8
D = 1024
SEQ = 2048
NBC = 2
TOK = NBC * SEQ
DFF = 2816
NF = DFF // 128
NEXP = 8
ALPHA = 4.0 ** 0.25
EPS = 1e-5
NEGB = -1024.0
MIXIN = 2840
MOE_BS = 512
MOE_NBLK = (TOK * 2) // MOE_BS + NEXP
NSLOT = MOE_NBLK * MOE_BS
I32 = mybir.dt.int32
ENGS = ("pe", "act", "dve", "pool", "sp")
DBG = {}


class T:
    __slots__ = ("h", "w", "r", "name")

    def __init__(self, h, name=""):
        self.h = h
        self.w = None
        self.r = {}
        self.name = name

    def __getitem__(self, k):
        return self.h[k]


class Sched:
    def __init__(self, nc, n_dma_sems=48):
        self.nc = nc
        self.streams = {e: [] for e in ENGS}
        self.cnt = {e: 0 for e in ENGS}
        self.seen = {e: {} for e in ENGS}
        self.n_dma = n_dma_sems
        self.dma_cnt = [0] * n_dma_sems
        self.dma_rr = 0
        self.sw_rr = 0
        self.n_hw = n_dma_sems - 16
        self.n_ops = 0
        self.n_waits = 0

    def _wait(self, eng, dep):
        key, val = dep
        if eng == "pe" and key == ("E", "pe"):
            return
        if self.seen[eng].get(key, 0) >= val:
            return
        self.seen[eng][key] = val
        self.streams[eng].append(("wait", key, val))
        self.n_waits += 1

    def _deps(self, eng, reads, writes):
        for t in reads:
            if t.w is not None:
                self._wait(eng, t.w)
        for t in writes:
            if t.w is not None:
                self._wait(eng, t.w)
            for k, v in t.r.items():
                self._wait(eng, (k, v))

    def _mark(self, me, reads, writes):
        k, v = me
        for t in reads:
            if t.r.get(k, 0) < v:
                t.r[k] = v
        for t in writes:
            t.w = me
            t.r = {}

    def op(self, eng, fn, reads=(), writes=()):
        self._deps(eng, reads, writes)
        self.cnt[eng] += 1
        me = (("E", eng), self.cnt[eng])
        self.streams[eng].append(("op", fn, ("E", eng), 1))
        self._mark(me, reads, writes)
        self.n_ops += 1

    def dma(self, q, out_ap, in_ap, reads=(), writes=(), **kw):
        if q == "pool":
            i = self.n_hw + self.sw_rr
            self.sw_rr = (self.sw_rr + 1) % (self.n_dma - self.n_hw)
        else:
            i = self.dma_rr
            self.dma_rr = (i + 1) % self.n_hw
        if self.dma_cnt[i] > 0:
            self._wait(q, (("D", i), self.dma_cnt[i]))
        self._deps(q, reads, writes)
        self.dma_cnt[i] += 16
        me = (("D", i), self.dma_cnt[i])
        self.streams[q].append(("op", lambda e: e.dma_start(out=out_ap, in_=in_ap, **kw), ("D", i), 16))
        self._mark(me, reads, writes)
        self.n_ops += 1

    def dma_fn(self, q, fn, reads=(), writes=()):
        if q == "pool":
            i = self.n_hw + self.sw_rr
            self.sw_rr = (self.sw_rr + 1) % (self.n_dma - self.n_hw)
        else:
            i = self.dma_rr
            self.dma_rr = (i + 1) % self.n_hw
        if self.dma_cnt[i] > 0:
            self._wait(q, (("D", i), self.dma_cnt[i]))
        self._deps(q, reads, writes)
        self.dma_cnt[i] += 16
        me = (("D", i), self.dma_cnt[i])
        self.streams[q].append(("op", fn, ("D", i), 16))
        self._mark(me, reads, writes)
        self.n_ops += 1

    def barrier(self):
        for e in ENGS:
            for e2 in ENGS:
                if e2 != e and self.cnt[e2] > 0:
                    self._wait(e, (("E", e2), self.cnt[e2]))
            for i in range(self.n_dma):
                if self.dma_cnt[i] > 0:
                    self._wait(e, (("D", i), self.dma_cnt[i]))

    def emit(self, stack):
        nc = self.nc
        sems = {}
        for e in ENGS:
            sems[("E", e)] = stack.enter_context(nc.semaphore("s_" + e))
        for i in range(self.n_dma):
            sems[("D", i)] = stack.enter_context(nc.semaphore("d_%d" % i))
        block = stack.enter_context(nc.Block())

        def run(engh, items):
            for it in items:
                if it[0] == "wait":
                    engh.wait_ge(sems[it[1]], it[2])
                else:
                    it[1](engh).then_inc(sems[it[2]], it[3])

        @block.tensor
        def _(e):
            run(e, self.streams["pe"])

        @block.scalar
        def _(e):
            run(e, self.streams["act"])

        @block.vector
        def _(e):
            run(e, self.streams["dve"])

        @block.gpsimd
        def _(e):
            run(e, self.streams["pool"])

        @block.sync
        def _(e):
            run(e, self.streams["sp"])


class K:
    SB_BASE = 16512
    SB_LIMIT = 229376

    def __init__(self, nc):
        self.nc = nc
        self.S = Sched(nc)
        self.pers_off = self.SB_BASE
        self.stage_base = self.SB_BASE
        self.off = self.SB_BASE
        self.uid = 0
        self.ps = [T(nc.alloc_psum_tensor("psb%d" % i, [128, 512], F32), "ps%d" % i) for i in range(8)]

    def _alloc(self, name, shape, dt, off):
        self.uid += 1
        h = self.nc.alloc_sbuf_tensor_at("%s_%d" % (name, self.uid), shape, dt, offset=off)
        return h

    @staticmethod
    def _bytes(shape, dt):
        n = 1
        for s in shape[1:]:
            n *= s
        b = n * (2 if dt == BF16 else 4)
        return (b + 31) // 32 * 32

    def pers(self, name, shape, dt):
        assert self.off == self.stage_base, "persistent alloc only between stages"
        h = self._alloc(name, shape, dt, self.pers_off)
        self.pers_off += self._bytes(shape, dt)
        self.stage_base = self.off = self.pers_off
        return T(h, name)

    def sb(self, name, shape, dt):
        h = self._alloc(name, shape, dt, self.off)
        self.off += self._bytes(shape, dt)
        assert self.off <= self.SB_LIMIT, "SBUF overflow at %s: %d" % (name, self.off)
        return T(h, name)

    def view(self, t, name=""):
        return T(t.h, name or t.name)

    def end_stage(self):
        self.S.barrier()
        self.off = self.stage_base
        for p in self.ps:
            p.w = None
            p.r = {}

    def mm(self, ot, o_ap, lt, l_ap, rt, r_ap, start=True, stop=True, skip=False):
        if skip:
            self.S.op("pe", lambda e: e.matmul(o_ap, lhsT=l_ap, rhs=r_ap, start=start, stop=stop, skip_group_check=True),
                      reads=[lt, rt], writes=[ot])
        else:
            self.S.op("pe", lambda e: e.matmul(o_ap, lhsT=l_ap, rhs=r_ap, start=start, stop=stop),
                      reads=[lt, rt], writes=[ot])

    def tr(self, ot, o_ap, it, i_ap, idt, id_ap):
        self.S.op("pe", lambda e: e.transpose(o_ap, i_ap, id_ap), reads=[it, idt], writes=[ot])

    def act(self, ot, o_ap, it, i_ap, func, bias=None, scale=None, extra_reads=(), eng="act"):
        kw = {}
        if bias is not None:
            kw["bias"] = bias
        if scale is not None:
            kw["scale"] = scale
        self.S.op("act", lambda e: e.activation(out=o_ap, in_=i_ap, func=func, **kw),
                  reads=[it] + list(extra_reads), writes=[ot])

    def tt(self, ot, o_ap, at, a_ap, bt, b_ap, op, eng="dve"):
        self.S.op(eng, lambda e: e.tensor_tensor(out=o_ap, in0=a_ap, in1=b_ap, op=op),
                  reads=[at, bt], writes=[ot])

    def ts(self, ot, o_ap, at, a_ap, s1, s2, op0, op1=None, extra_reads=(), eng="dve"):
        if op1 is None:
            self.S.op(eng, lambda e: e.tensor_scalar(out=o_ap, in0=a_ap, scalar1=s1, scalar2=None, op0=op0),
                      reads=[at] + list(extra_reads), writes=[ot])
        else:
            self.S.op(eng, lambda e: e.tensor_scalar(out=o_ap, in0=a_ap, scalar1=s1, scalar2=s2, op0=op0, op1=op1),
                      reads=[at] + list(extra_reads), writes=[ot])

    def stt(self, ot, o_ap, at, a_ap, scalar, bt, b_ap, op0, op1, extra_reads=(), eng="dve"):
        self.S.op(eng, lambda e: e.scalar_tensor_tensor(out=o_ap, in0=a_ap, scalar=scalar, in1=b_ap, op0=op0, op1=op1),
                  reads=[at, bt] + list(extra_reads), writes=[ot])

    def cp(self, ot, o_ap, it, i_ap, eng="dve"):
        if eng == "act":
            self.S.op("act", lambda e: e.copy(out=o_ap, in_=i_ap), reads=[it], writes=[ot])
        else:
            self.S.op(eng, lambda e: e.tensor_copy(out=o_ap, in_=i_ap), reads=[it], writes=[ot])

    def memset(self, ot, o_ap, val, eng="dve"):
        self.S.op(eng, lambda e: e.memset(o_ap, val), reads=[], writes=[ot])

    def dma(self, q, o_ap, i_ap, reads=(), writes=(), **kw):
        self.S.dma(q, o_ap, i_ap, reads=reads, writes=writes, **kw)

    def gather(self, o_ap, src_ap, idx_ap, reads=(), writes=(), bounds=None):
        if bounds is None:
            self.S.dma_fn("pool", lambda e: e.indirect_dma_start(
                out=o_ap, out_offset=None, in_=src_ap, in_offset=bass.IndirectOffsetOnAxis(ap=idx_ap, axis=0)),
                reads=reads, writes=writes)
        else:
            regs = self.__dict__.setdefault("_bound_regs", {})

            def fn(e):
                if bounds not in regs:
                    rg = e.alloc_register("bnd%d" % bounds)
                    e.reg_mov(rg, bounds)
                    regs[bounds] = rg
                return e.indirect_dma_start(
                    out=o_ap, out_offset=None, in_=src_ap, in_offset=bass.IndirectOffsetOnAxis(ap=idx_ap, axis=0),
                    bounds_check=regs[bounds], oob_is_err=False)
            self.S.dma_fn("pool", fn, reads=reads, writes=writes)

    def scatter(self, dst_ap, i_ap, idx_ap, reads=(), writes=()):
        self.S.dma_fn("pool", lambda e: e.indirect_dma_start(
            out=dst_ap, out_offset=bass.IndirectOffsetOnAxis(ap=idx_ap, axis=0), in_=i_ap, in_offset=None),
            reads=reads, writes=writes)


def host_consts():
    s = np.arange(128)[:, None]
    c = np.arange(512)[None, :]
    cst = {}
    cst["ident"] = np.eye(128, dtype=np.float32)
    cst["cb"] = np.where(c >= s, 0.0, NEGB).astype(np.float32)
    cst["cbs"] = np.where(c > s, 0.0, NEGB).astype(np.float32)
    cst["wb"] = np.where(c - 384 < s, 0.0, NEGB).astype(np.float32)
    j = np.arange(128)[:, None]
    ss = np.arange(128)[None, :]
    cst["negtri"] = np.where(j >= ss, -1.0, 0.0).astype(np.float32)
    cst["negones"] = -np.ones((128, 128), np.float32)
    n = np.arange(128)[:, None]
    t = np.arange(SEQ)[None, :]
    cmpb = np.where(16 * n + 31 <= t, 0.0, NEGB).astype(np.float32)
    cmpb[127, :] = NEGB
    cst["cmpbias"] = cmpb
    cmp_start = np.arange(127) * 16
    slc_start = np.arange(32) * 64
    ov = ((cmp_start[:, None] < slc_start[None, :] + 64) & (cmp_start[:, None] + 32 > slc_start[None, :]))
    ovp = np.zeros((128, 32), np.float32)
    ovp[:127] = ov.astype(np.float32)
    cst["overlap"] = ovp
    ex = np.zeros((32, 16, 128), np.float32)
    for kb in range(16):
        for sl in range(128):
            ex[2 * kb + sl // 64, kb, sl] = 1.0
    cst["expand"] = ex
    tt = np.arange(SEQ)
    blk = np.arange(32)[None, :]
    cur = (tt // 64)[:, None]
    valid = slc_start[None, :] <= tt[:, None]
    forced = (blk == 0) | (blk == cur) | (blk == cur - 1)
    A = (valid & ~forced).astype(np.float32)
    Bm = np.where(valid, np.where(forced, 1e30, 0.0), -1e30).astype(np.float32)
    cst["selA"] = A.reshape(16, 128, 32).transpose(1, 0, 2).copy()
    cst["selB"] = Bm.reshape(16, 128, 32).transpose(1, 0, 2).copy()
    r_ = np.arange(128)[:, None]
    c_ = np.arange(128)[None, :]
    cst["trilt"] = (r_ < c_).astype(np.float32)
    cst["ones"] = np.ones((128, 128), np.float32)
    cst["pidx"] = np.arange(128, dtype=np.float32).reshape(128, 1)
    cst["thr"] = np.tile((np.arange(MOE_NBLK + 8) * float(MOE_BS))[None, :], (128, 1)).astype(np.float32)
    return cst


CONST_SHAPES = {
    "ident": [128, 128], "cb": [128, 512], "cbs": [128, 512], "wb": [128, 512],
    "negtri": [128, 128], "negones": [128, 128], "cmpbias": [128, SEQ], "overlap": [128, 32],
    "expand": [32, 16, 128], "selA": [128, 16, 32], "selB": [128, 16, 32],
    "trilt": [128, 128], "ones": [128, 128], "pidx": [128, 1], "thr": [128, MOE_NBLK + 8],
}

INPUT_SHAPES = {
    "x": [TOK, D], "c": [NBC, D],
    "ada_w": [2, D, 6 * D], "ada_b": [2, 6 * D], "ln_g": [2, 2, D], "ln_b": [2, 2, D],
    "mix_w_in": [1, D, MIXIN], "cmp_pos": [1, 2, 32, 64], "cmp_w1": [1, 2, 2048, 256],
    "cmp_w2": [1, 2, 256, 64], "mix_w_out": [1, D, D],
    "ffn_w1": [1, D, DFF], "ffn_w3": [1, D, DFF], "ffn_w2": [1, DFF, D],
    "conv_w_in": [1, D, 3 * D], "conv_taps": [1, 3, D], "conv_w_out": [1, D, D],
    "router_w": [1, D, NEXP], "router_b": [1, NEXP],
    "exp_w1": [1, NEXP, 2 * D, DFF // 2], "exp_w3": [1, NEXP, 2 * D, DFF // 2], "exp_w2": [1, NEXP, DFF, D],
}

NQK = 18
VT_W = 768


def build_program(stages=("s0", "s1", "s2a", "s2", "s3", "s4", "s5", "s6"), dbg=(), feed=()):
    nc = bass.Bass("TRN2", target_bir_lowering=False)
    k = K(nc)
    S = k.S
    I = {n: nc.dram_tensor(n, shp, F32, kind="ExternalInput").ap() for n, shp in INPUT_SHAPES.items()}
    C = {n: nc.dram_tensor("c_" + n, shp, F32, kind="ExternalInput").ap() for n, shp in CONST_SHAPES.items()}
    out_d = nc.dram_tensor("out", [TOK, D], F32, kind="ExternalOutput").ap()

    def scratch(name, shape, dt):
        kind = "ExternalOutput" if name in dbg else ("ExternalInput" if name in feed else "Internal")
        return nc.dram_tensor(name, shape, dt, kind=kind).ap()

    mod_d = scratch("mod_d", [2, NBC, 6 * D], F32)
    qkT_d = scratch("qkT_d", [NBC, NQK, 128, SEQ], BF16)
    vtok_d = scratch("vtok_d", [NBC, SEQ, VT_W], BF16)
    gates_d = scratch("gates_d", [NBC, SEQ, 24], F32)
    kcT_d = scratch("kcT_d", [NBC, 2, 128, 128], BF16)
    vc_d = scratch("vc_d", [NBC, 2, 128, 64], BF16)
    oT_d = scratch("oT_d", [NBC, 8, 128, SEQ], BF16)
    x1_d = scratch("x1_d", [TOK, D], F32)
    x2_d = scratch("x2_d", [TOK, D], F32)
    x3_d = scratch("x3_d", [TOK, D], F32)
    h2T_d = scratch("h2T_d", [8, 128, TOK], BF16)
    acc_d = scratch("acc_d", [TOK, D], F32)
    dt_ = {n: T(None, n) for n in ("mod", "qkT", "vtok", "gates", "kcT", "vc", "oT", "x1", "x2", "x3", "h2T", "acc", "out")}
    dummy_in = T(None, "in")

    ident = k.pers("ident", [128, 128], F32)
    identb = k.pers("identb", [128, 128], BF16)
    modT = [k.pers("modT%d" % l, [128, 48, NBC], F32) for l in range(2)]
    modP = [k.pers("modP%d" % l, [128, 48, NBC], F32) for l in range(2)]
    gw = k.pers("gw", [128, TOK // 128, NEXP], F32)
    k.dma("sp", ident[:, :], C["ident"], writes=[ident])
    k.dma("pool", identb[:, :], C["ident"], writes=[identb])

    PS = k.ps

    def load_bc_row(name, src_row_ap, q="sp", add_one=False):
        t = name if isinstance(name, T) else k.sb(name, [128, D], F32)
        k.dma(q, t[:, :], src_row_ap.partition_broadcast(128), writes=[t])
        if add_one:
            k.ts(t, t[:, :], t, t[:, :], 1.0, None, ALU.add)
        return t

    def transpose_mod(xts, hT, l, b, sh_kind, sc_kind, psel, hT32=None):
        for c in range(8):
            p = PS[psel[c % len(psel)]]
            for j in range(4):
                k.tr(p, p[:, j * 128:(j + 1) * 128], xts[j], xts[j][:, c * 128:(c + 1) * 128], ident, ident[:, :])
            if hT is not None:
                k.act(hT, hT[:, c, :], p, p[:, :], AF.Identity,
                      bias=modT[l][:, sh_kind * 8 + c, b:b + 1], scale=modP[l][:, sc_kind * 8 + c, b:b + 1],
                      extra_reads=[modT[l], modP[l]])
            if hT32 is not None:
                k.act(hT32, hT32[:, c, :], p, p[:, :], AF.Identity,
                      bias=modT[l][:, sh_kind * 8 + c, b:b + 1], scale=modP[l][:, sc_kind * 8 + c, b:b + 1],
                      extra_reads=[modT[l], modP[l]])

    def resid_ln(xt, yps, gate_bc, lng, lnb, tmp, r, outt, stats, mv):
        if isinstance(yps, T):
            k.tt(tmp, tmp[:, :], yps, yps[:, :], gate_bc, gate_bc[:, :], ALU.mult)
        else:
            for h in range(2):
                k.tt(tmp, tmp[:, h * 512:(h + 1) * 512], yps[h], yps[h][:, :], gate_bc, gate_bc[:, h * 512:(h + 1) * 512], ALU.mult)
        k.stt(r, r[:, :], xt, xt[:, :], ALPHA, tmp, tmp[:, :], ALU.mult, ALU.add)
        for h in range(2):
            S.op("dve", (lambda hh: (lambda e: e.bn_stats(out=stats[:, hh, :], in_=r[:, hh * 512:(hh + 1) * 512])))(h),
                 reads=[r], writes=[stats])
        S.op("dve", lambda e: e.bn_aggr(out=mv[:, 0:2], in_=stats[:, :, :].rearrange("p a b -> p (a b)")),
             reads=[stats], writes=[mv])
        k.ts(mv, mv[:, 2:3], mv, mv[:, 1:2], EPS, None, ALU.add)
        S.op("pool", lambda e: e.tensor_tensor(out=mv[:, 3:4], in0=mv[:, 2:3], in1=mv[:, 4:5], op=ALU.pow),
             reads=[mv], writes=[mv])
        k.ts(tmp, tmp[:, :], r, r[:, :], mv[:, 0:1], mv[:, 3:4], ALU.subtract, ALU.mult, extra_reads=[mv])
        k.tt(tmp, tmp[:, :], tmp, tmp[:, :], lng, lng[:, :], ALU.mult)
        k.tt(outt, outt[:, :], tmp, tmp[:, :], lnb, lnb[:, :], ALU.add)

    def new_mv(name):
        mv = k.sb(name, [128, 8], F32)
        k.memset(mv, mv[:, :], -0.5)
        return mv

    if "s0" in stages:
        c_s = k.sb("c_s", [NBC, D], F32)
        cond_s = k.sb("cond_s", [NBC, D], F32)
        condT = k.sb("condT", [128, 8, NBC], BF16)
        k.dma("sp", c_s[:, :], I["c"], writes=[c_s])
        k.act(cond_s, cond_s[:, :], c_s, c_s[:, :], AF.Silu)
        for c in range(8):
            k.tr(PS[0], PS[0][:, c * NBC:(c + 1) * NBC], cond_s, cond_s[:, c * 128:(c + 1) * 128], ident, ident[0:NBC, 0:NBC])
        k.cp(condT, condT[:, :, :].rearrange("p a b -> p (a b)"), PS[0], PS[0][:, 0:8 * NBC])
        wbuf = [k.sb("adaw%d" % i, [128, 8, 512], BF16) for i in range(2)]
        mod_s = k.sb("mod_s", [NBC, 6 * D], F32)
        adab = k.sb("adab", [NBC, 6 * D], F32)
        for l in range(2):
            k.dma("sp", adab[:, :], I["ada_b"][l, :].partition_broadcast(NBC), writes=[adab])
            for blk in range(12):
                wb = wbuf[blk % 2]
                src = I["ada_w"][l].rearrange("(kc p) n -> p kc n", p=128)[:, :, blk * 512:(blk + 1) * 512]
                k.dma("pool", wb[:, :, :], src, writes=[wb])
                p = PS[1 + blk % 2]
                for kc in range(8):
                    k.mm(p, p[0:NBC, :], condT, condT[:, kc, :], wb, wb[:, kc, :], start=(kc == 0), stop=(kc == 7))
                k.tt(mod_s, mod_s[:, blk * 512:(blk + 1) * 512], p, p[0:NBC, :], adab, adab[:, blk * 512:(blk + 1) * 512], ALU.add)
            k.dma("sp", mod_d[l], mod_s[:, :], reads=[mod_s])
            for ch in range(48):
                k.tr(PS[3], PS[3][:, ch * NBC:(ch + 1) * NBC], mod_s, mod_s[:, ch * 128:(ch + 1) * 128], ident, ident[0:NBC, 0:NBC])
            k.cp(modT[l], modT[l][:, :, :].rearrange("p a b -> p (a b)"), PS[3], PS[3][:, 0:48 * NBC])
            k.ts(modP[l], modP[l][:, :, :].rearrange("p a b -> p (a b)"), modT[l],
                 modT[l][:, :, :].rearrange("p a b -> p (a b)"), 1.0, None, ALU.add)
        k.end_stage()

    if "s1" in stages:
        win = k.sb("win", [128, 8, MIXIN], BF16)
        wdup = k.sb("wdup", [128, 8, 512], BF16)
        wsrc = I["mix_w_in"][0].rearrange("(kc p) n -> p kc n", p=128)
        for kc in range(8):
            k.dma("pool", win[:, kc, :], wsrc[:, kc, :], writes=[win])
        for i, col in enumerate((2304, 2368, 2560, 2624)):
            for rep in range(2):
                k.dma("pool", wdup[:, :, i * 128 + rep * 64: i * 128 + rep * 64 + 64], wsrc[:, :, col:col + 64], writes=[wdup])
        xts = [[k.sb("xt%d_%d" % (i, j), [128, D], F32) for j in range(4)] for i in range(2)]
        hTs = [k.sb("hT%d" % i, [128, 8, 512], BF16) for i in range(2)]
        fm = [k.sb("fm%d" % i, [128, 512], BF16) for i in range(4)]
        vst = [k.sb("vst%d" % i, [128, VT_W], BF16) for i in range(2)]
        gst = [k.sb("gst%d" % i, [128, 24], F32) for i in range(2)]
        fm_i = 0
        v_i = 0
        chunks = []
        for c in range(4):
            chunks.append((win, 0 + c * 128, 1.0))
        for c in range(4):
            chunks.append((win, 512 + c * 128, 0.125))
        for c in range(4):
            chunks.append((win, 1536 + c * 128, 1.0))
        chunks.append((win, 2048, 1.0))
        chunks.append((win, 2176, 1.0))
        for c in range(4):
            chunks.append((wdup, c * 128, 0.125))
        for g in range(DBG.get("s1_groups", TOK // 512)):
            b = g // 4
            t0 = (g % 4) * 512
            xt = xts[g % 2]
            hT = hTs[g % 2]
            for j in range(4):
                k.dma("sp", xt[j][:, :], I["x"][g * 512 + j * 128: g * 512 + (j + 1) * 128, :], writes=[xt[j]])
            transpose_mod(xt, hT, 0, b, 0, 1, (0, 1))
            for ci, (wt, col0, scl) in enumerate(chunks if DBG.get("s1_fm", True) else []):
                p = PS[2 + ci % 3]
                for kc in range(8):
                    k.mm(p, p[:, :], wt, wt[:, kc, col0:col0 + 128], hT, hT[:, kc, :], start=(kc == 0), stop=(kc == 7))
                f = fm[fm_i % 4]
                fm_i += 1
                if ci % 2 == 0:
                    k.act(f, f[:, :], p, p[:, :], AF.Identity, scale=scl)
                else:
                    k.ts(f, f[:, :], p, p[:, :], scl, None, ALU.mult)
                k.dma("sp", qkT_d[b, ci, :, t0:t0 + 512], f[:, :], reads=[f])
            for j in range(4 if DBG.get("s1_tm", True) else 0):
                pv, pg = PS[5 + (j % 2)], PS[7]
                for kc in range(8):
                    k.mm(pv, pv[:, :], hT, hT[:, kc, j * 128:(j + 1) * 128], win, win[:, kc, 1024:1536], start=(kc == 0), stop=(kc == 7))
                if DBG.get("pg1", True):
                    for kc in range(8):
                        k.mm(pg, pg[:, 0:128], hT, hT[:, kc, j * 128:(j + 1) * 128], win, win[:, kc, 2432:2560], start=(kc == 0), stop=(kc == 7))
                if DBG.get("pg2", True):
                    for kc in range(8):
                        k.mm(pg, pg[:, 128:256], hT, hT[:, kc, j * 128:(j + 1) * 128], win, win[:, kc, 2688:2816], start=(kc == 0), stop=(kc == 7))
                    for kc in range(8):
                        k.mm(pg, pg[:, 256:280], hT, hT[:, kc, j * 128:(j + 1) * 128], win, win[:, kc, 2816:2840], start=(kc == 0), stop=(kc == 7))
                vs = vst[v_i % 2]
                gs = gst[v_i % 2]
                v_i += 1
                k.cp(vs, vs[:, 0:512], pv, pv[:, :], eng="dve")
                if DBG.get("pgc", True):
                    k.cp(vs, vs[:, 512:768], pg, pg[:, 0:256], eng=DBG.get("pgc_eng", "act"))
                if DBG.get("sig", True):
                    k.act(gs, gs[:, :], pg, pg[:, 256:280], AF.Sigmoid)
                tok = t0 + j * 128
                k.dma("sp", vtok_d[b, tok:tok + 128, :], vs[:, :], reads=[vs])
                if DBG.get("gdma", True):
                    k.dma("sp", gates_d[b, tok:tok + 128, :], gs[:, :], reads=[gs])
        k.end_stage()

    if "s2a" in stages:
        w1 = [k.sb("cw1_%d" % i, [128, 32, 256], BF16) for i in range(2)]
        w2k = k.sb("cw2k", [128, 2, 128], BF16)
        w2v = k.sb("cw2v", [128, 2, 64], BF16)
        posT = [k.sb("posT%d" % i, [128, 32], F32) for i in range(2)]
        pos_s = [k.sb("pos_s%d" % i, [32, 128], F32) for i in range(2)]
        for kv in range(2):
            src = I["cmp_w1"][0, kv].rearrange("(l d) h -> d l h", d=64)
            for half in range(2):
                k.dma("pool", w1[kv][half * 64:(half + 1) * 64, :, :], src, writes=[w1[kv]])
            for half in range(2):
                k.dma("sp", pos_s[kv][:, half * 64:(half + 1) * 64], I["cmp_pos"][0, kv], writes=[pos_s[kv]])
            k.tr(PS[0], PS[0][:, kv * 32:(kv + 1) * 32], pos_s[kv], pos_s[kv][:, :], ident, ident[0:32, 0:32])
            k.cp(posT[kv], posT[kv][:, :], PS[0], PS[0][:, kv * 32:(kv + 1) * 32])
        w2ksrc = I["cmp_w2"][0, 0].rearrange("(hc p) d -> p hc d", p=128)
        for rep in range(2):
            k.dma("pool", w2k[:, :, rep * 64:(rep + 1) * 64], w2ksrc, writes=[w2k])
        k.dma("pool", w2v[:, :, :], I["cmp_w2"][0, 1].rearrange("(hc p) d -> p hc d", p=128), writes=[w2v])
        src_t = [k.sb("cmpsrc%d" % i, [128, SEQ], BF16) for i in range(2)]
        kp = [k.sb("kp%d" % i, [128, 32, 128], BF16) for i in range(2)]
        HsT = [k.sb("HsT%d" % i, [128, 2, 128], BF16) for i in range(2)]
        kc_s = [k.sb("kc_s%d" % i, [128, 128], BF16) for i in range(2)]
        vc_s = [k.sb("vc_s%d" % i, [128, 64], BF16) for i in range(2)]
        it = 0
        for b in range(NBC):
            for kv in range(2):
                st = src_t[it % 2]
                kpt = kp[it % 2]
                it += 1
                k.dma("sp", st[:, :], qkT_d[b, 12 + kv, :, :], writes=[st])
                for l in range(32):
                    k.act(kpt, kpt[:, l, 0:127], st, st[:, l:l + 16 * 126 + 1:16], AF.Identity,
                          bias=posT[kv][:, l:l + 1], extra_reads=[posT[kv]])
                for g in range(2):
                    hs = HsT[g]
                    for hc in range(2):
                        p = PS[1 + hc]
                        for l in range(32):
                            k.mm(p, p[:, 0:127], w1[kv], w1[kv][g * 64:(g + 1) * 64, l, hc * 128:(hc + 1) * 128],
                                 kpt, kpt[g * 64:(g + 1) * 64, l, 0:127], start=(l == 0), stop=(l == 31))
                        k.act(hs, hs[:, hc, 0:127], p, p[:, 0:127], AF.Silu)
                    if kv == 0:
                        p = PS[3]
                        for hc in range(2):
                            k.mm(p, p[:, 0:127], w2k, w2k[:, hc, :], hs, hs[:, hc, 0:127], start=(hc == 0), stop=(hc == 1))
                        kcs = kc_s[g]
                        k.memset(kcs, kcs[:, 96:128], 0.0)
                        k.act(kcs, kcs[:, 0:127], p, p[:, 0:127], AF.Identity, scale=0.125)
                        k.dma("sp", kcT_d[b, g], kcs[:, :], reads=[kcs])
                    else:
                        p = PS[4]
                        for hc in range(2):
                            k.mm(p, p[0:127, 0:64], hs, hs[:, hc, 0:127], w2v, w2v[:, hc, :], start=(hc == 0), stop=(hc == 1))
                        vcs = vc_s[g]
                        k.memset(vcs, vcs[:, :], 0.0)
                        k.cp(vcs, vcs[0:127, :], p, p[0:127, 0:64])
                        k.dma("sp", vc_d[b, g], vcs[:, :], reads=[vcs])
        k.end_stage()

    if "s2" in stages:
        stage_attention(k, nc, I, C, dt_, ident, identb, qkT_d, vtok_d, gates_d, kcT_d, vc_d, oT_d)
        k.end_stage()

    if "s3" in stages:
        wout = k.sb("wout", [128, 8, D], BF16)
        k.dma("pool", wout[:, :, :], I["mix_w_out"][0].rearrange("(kc p) n -> p kc n", p=128), writes=[wout])
        lng = load_bc_row("lng", I["ln_g"][0, 0, :])
        lnb = load_bc_row("lnb", I["ln_b"][0, 0, :])
        gbc = [load_bc_row("gbc%d" % b, mod_d[0, b, 2 * D:3 * D], add_one=True) for b in range(NBC)]
        oTs = [k.sb("oTs%d" % i, [128, 8, 512], BF16) for i in range(2)]
        xts = [k.sb("xt%d" % i, [128, D], F32) for i in range(3)]
        outs = [k.sb("xo%d" % i, [128, D], F32) for i in range(2)]
        tmp = k.sb("tmp", [128, D], F32)
        r = k.sb("r", [128, D], F32)
        stats = k.sb("stats", [128, 2, 6], F32)
        mv = new_mv("mv")
        ti = 0
        for g in range(TOK // 512):
            b = g // 4
            t0 = (g % 4) * 512
            oT = oTs[g % 2]
            k.dma("sp", oT[:, :, :], oT_d[b, :, :, t0:t0 + 512].rearrange("c p t -> p c t"), writes=[oT])
            for j in range(4):
                xt = xts[ti % 3]
                ot = outs[ti % 2]
                tok = g * 512 + j * 128
                k.dma("sp", xt[:, :], I["x"][tok:tok + 128, :], writes=[xt])
                yps = (PS[(ti % 2) * 2], PS[(ti % 2) * 2 + 1])
                for h in range(2):
                    for kc in range(8):
                        k.mm(yps[h], yps[h][:, :], oT, oT[:, kc, j * 128:(j + 1) * 128], wout, wout[:, kc, h * 512:(h + 1) * 512],
                             start=(kc == 0), stop=(kc == 7))
                resid_ln(xt, yps, gbc[b], lng, lnb, tmp, r, ot, stats, mv)
                k.dma("sp", x1_d[tok:tok + 128, :], ot[:, :], reads=[ot])
                ti += 1
        k.end_stage()

    def ffn_weights(w1src, w3src, w2src):
        w1 = k.sb("fw1", [128, 8, DFF], BF16)
        w3 = k.sb("fw3", [128, 8, DFF], BF16)
        w2 = k.sb("fw2", [128, NF, D], BF16)
        return w1, w3, w2

    def ffn_load(w1, w3, w2, w1src, w3src, w2src):
        s1 = w1src.rearrange("(kc p) n -> p kc n", p=128)
        s3 = w3src.rearrange("(kc p) n -> p kc n", p=128)
        s2 = w2src.rearrange("(f p) n -> p f n", p=128)
        for kc in range(8):
            k.dma("pool", w1[:, kc, :], s1[:, kc, :], writes=[w1])
            k.dma("pool", w3[:, kc, :], s3[:, kc, :], writes=[w3])
        for f0 in range(0, NF, 4):
            f1 = min(NF, f0 + 4)
            k.dma("pool", w2[:, f0:f1, :], s2[:, f0:f1, :], writes=[w2])

    def ffn_up(hT, w1, w3, gT, sa_bufs, cnt):
        for f in range(NF):
            pa = PS[(cnt[0] % 2) * 2]
            pb = PS[(cnt[0] % 2) * 2 + 1]
            sa = sa_bufs[cnt[0] % 2]
            cnt[0] += 1
            for kc in range(8):
                k.mm(pa, pa[:, :], w1, w1[:, kc, f * 128:(f + 1) * 128], hT, hT[:, kc, :], start=(kc == 0), stop=(kc == 7))
            for kc in range(8):
                k.mm(pb, pb[:, :], w3, w3[:, kc, f * 128:(f + 1) * 128], hT, hT[:, kc, :], start=(kc == 0), stop=(kc == 7))
            k.act(sa, sa[:, :], pa, pa[:, :], AF.Silu)
            k.tt(gT, gT[:, f, :], sa, sa[:, :], pb, pb[:, :], ALU.mult)

    def ffn_down(gT, w2, j, yps):
        for h in range(2):
            for f in range(NF):
                k.mm(yps[h], yps[h][:, :], gT, gT[:, f, j * 128:(j + 1) * 128], w2, w2[:, f, h * 512:(h + 1) * 512],
                     start=(f == 0), stop=(f == NF - 1))

    if "s4" in stages:
        w1, w3, w2 = ffn_weights(None, None, None)
        ffn_load(w1, w3, w2, I["ffn_w1"][0], I["ffn_w3"][0], I["ffn_w2"][0])
        lng = load_bc_row("lng", I["ln_g"][0, 1, :])
        lnb = load_bc_row("lnb", I["ln_b"][0, 1, :])
        gbc1 = k.sb("gbc", [128, D], F32)
        xts = [k.sb("xt%d" % j, [128, D], F32) for j in range(4)]
        hT = k.sb("hT", [128, 8, 512], BF16)
        gT = k.sb("gT", [128, NF, 512], BF16)
        sa_bufs = [k.sb("sa%d" % i, [128, 512], F32) for i in range(2)]
        tmp = k.sb("tmp", [128, D], F32)
        r = k.sb("r", [128, D], F32)
        ot = r
        stats = k.sb("stats", [128, 2, 6], F32)
        mv = new_mv("mv")
        cnt = [0]
        for g in range(TOK // 512):
            b = g // 4
            if g % 4 == 0:
                load_bc_row(gbc1, mod_d[0, b, 5 * D:6 * D], add_one=True)
            gbc = [gbc1, gbc1]
            for j in range(4):
                tok = g * 512 + j * 128
                k.dma("sp", xts[j][:, :], x1_d[tok:tok + 128, :], writes=[xts[j]])
            transpose_mod(xts, hT, 0, b, 3, 4, (4, 5))
            ffn_up(hT, w1, w3, gT, sa_bufs, cnt)
            for j in range(4):
                tok = g * 512 + j * 128
                yps = (PS[4 + (j % 2) * 2], PS[5 + (j % 2) * 2])
                ffn_down(gT, w2, j, yps)
                resid_ln(xts[j], yps, gbc[b], lng, lnb, tmp, r, ot, stats, mv)
                k.dma("sp", x2_d[tok:tok + 128, :], ot[:, :], reads=[ot])
        k.end_stage()

    if "s5" in stages:
        cwin = k.sb("cwin", [128, 8, 3 * D], BF16)
        cwout = k.sb("cwout", [128, 8, D], BF16)
        s_in = I["conv_w_in"][0].rearrange("(kc p) n -> p kc n", p=128)
        for kc in range(8):
            k.dma("pool", cwin[:, kc, :], s_in[:, kc, :], writes=[cwin])
        k.dma("pool", cwout[:, :, :], I["conv_w_out"][0].rearrange("(kc p) n -> p kc n", p=128), writes=[cwout])
        taps_s = k.sb("taps_s", [3, D], F32)
        tapsT = k.sb("tapsT", [128, 8, 3], F32)
        k.dma("sp", taps_s[:, :], I["conv_taps"][0], writes=[taps_s])
        for c in range(8):
            k.tr(PS[0], PS[0][:, c * 3:(c + 1) * 3], taps_s, taps_s[:, c * 128:(c + 1) * 128], ident, ident[0:3, 0:3])
        k.cp(tapsT, tapsT[:, :, :].rearrange("p a b -> p (a b)"), PS[0], PS[0][:, 0:24])
        lng = load_bc_row("lng", I["ln_g"][1, 0, :])
        lnb = load_bc_row("lnb", I["ln_b"][1, 0, :])
        gbc = [load_bc_row("gbc%d" % b, mod_d[1, b, 2 * D:3 * D], add_one=True) for b in range(NBC)]
        xts = [k.sb("xt%d" % j, [128, D], F32) for j in range(4)]
        hT = k.sb("hT", [128, 8, 512], BF16)
        zT = [k.sb("zT%d" % i, [128, 8, 516], F32) for i in range(2)]
        cs = [k.sb("cs%d" % i, [128, 512], F32) for i in range(2)]
        bs = [k.sb("bs%d" % i, [128, 512], F32) for i in range(2)]
        zc = [k.sb("zc%d" % i, [128, 512], F32) for i in range(2)]
        vT = k.sb("vT", [128, 8, 512], BF16)
        tmp = k.sb("tmp", [128, D], F32)
        r = k.sb("r", [128, D], F32)
        ot = k.sb("xo", [128, D], F32)
        stats = k.sb("stats", [128, 2, 6], F32)
        mv = new_mv("mv")
        for g in range(TOK // 512):
            b = g // 4
            z = zT[g % 2]
            zprev = zT[(g + 1) % 2]
            for j in range(4):
                tok = g * 512 + j * 128
                k.dma("sp", xts[j][:, :], x2_d[tok:tok + 128, :], writes=[xts[j]])
            transpose_mod(xts, hT, 1, b, 0, 1, (0, 1))
            if g % 4 == 0:
                k.memset(z, z[:, :, 0:2], 0.0)
            else:
                k.cp(z, z[:, :, 0:2], zprev, zprev[:, :, 512:514], eng="dve")
            for c in range(8):
                pb_, pc_, pu_ = PS[2 + (c % 2) * 3], PS[3 + (c % 2) * 3], PS[4 + (c % 2) * 3]
                for which, p in ((0, pb_), (1, pc_), (2, pu_)):
                    col = which * D + c * 128
                    for kc in range(8):
                        k.mm(p, p[:, :], cwin, cwin[:, kc, col:col + 128], hT, hT[:, kc, :], start=(kc == 0), stop=(kc == 7))
                cst_ = cs[c % 2]
                bst = bs[c % 2]
                zct = zc[c % 2]
                k.cp(cst_, cst_[:, :], pc_, pc_[:, :], eng="act")
                k.cp(bst, bst[:, :], pb_, pb_[:, :], eng="act")
                k.tt(z, z[:, c, 2:514], cst_, cst_[:, :], pu_, pu_[:, :], ALU.mult)
                k.ts(zct, zct[:, :], z, z[:, c, 0:512], tapsT[:, c, 0:1], None, ALU.mult, extra_reads=[tapsT])
                k.stt(zct, zct[:, :], z, z[:, c, 1:513], tapsT[:, c, 1:2], zct, zct[:, :], ALU.mult, ALU.add, extra_reads=[tapsT])
                k.stt(zct, zct[:, :], z, z[:, c, 2:514], tapsT[:, c, 2:3], zct, zct[:, :], ALU.mult, ALU.add, extra_reads=[tapsT])
                k.tt(vT, vT[:, c, :], zct, zct[:, :], bst, bst[:, :], ALU.mult)
            for j in range(4):
                tok = g * 512 + j * 128
                yps = (PS[(j % 2) * 2], PS[(j % 2) * 2 + 1])
                for h in range(2):
                    for kc in range(8):
                        k.mm(yps[h], yps[h][:, :], vT, vT[:, kc, j * 128:(j + 1) * 128], cwout, cwout[:, kc, h * 512:(h + 1) * 512],
                             start=(kc == 0), stop=(kc == 7))
                resid_ln(xts[j], yps, gbc[b], lng, lnb, tmp, r, ot, stats, mv)
                k.dma("sp", x3_d[tok:tok + 128, :], ot[:, :], reads=[ot])
        k.end_stage()

    if "s6" in stages:
        stage_moe_sparse(k, nc, I, C, ident, identb, modT, modP, mod_d, x3_d, out_d, transpose_mod, resid_ln, load_bc_row, new_mv)

    if "s6dense" in stages:
        rw = k.sb("rw", [128, 8, NEXP], F32)
        rb = k.sb("rb", [128, NEXP], F32)
        k.dma("sp", rw[:, :, :], I["router_w"][0].rearrange("(kc p) e -> p kc e", p=128), writes=[rw])
        k.dma("sp", rb[:, :], I["router_b"][0, :].partition_broadcast(128), writes=[rb])
        xts = [k.sb("xt%d" % j, [128, D], F32) for j in range(4)]
        hTs = [k.sb("hT%d" % i, [128, 8, 512], BF16) for i in range(2)]
        hT32 = k.sb("hT32", [128, 8, 512], F32)
        lg = k.sb("lg", [128, NEXP], F32)
        lg2 = k.sb("lg2", [128, NEXP], F32)
        m1 = k.sb("m1", [128, NEXP], F32)
        m2 = k.sb("m2", [128, NEXP], F32)
        sc = k.sb("sc", [128, 8], F32)
        for g in range(TOK // 512):
            b = g // 4
            hT = hTs[g % 2]
            for j in range(4):
                tok = g * 512 + j * 128
                k.dma("sp", xts[j][:, :], x3_d[tok:tok + 128, :], writes=[xts[j]])
            transpose_mod(xts, hT, 1, b, 3, 4, (0, 1), hT32=hT32)
            k.dma("sp", h2T_d[:, :, g * 512:(g + 1) * 512].rearrange("c p t -> p c t"), hT[:, :, :], reads=[hT])
            for j in range(4):
                ti = g * 4 + j
                p = PS[2 + j % 2]
                for kc in range(8):
                    k.mm(p, p[:, 0:NEXP], hT32, hT32[:, kc, j * 128:(j + 1) * 128], rw, rw[:, kc, :], start=(kc == 0), stop=(kc == 7))
                k.tt(lg, lg[:, :], p, p[:, 0:NEXP], rb, rb[:, :], ALU.add)
                S.op("dve", lambda e: e.reduce_max(out=sc[:, 0:1], in_=lg[:, :], axis=AX.X), reads=[lg], writes=[sc])
                k.ts(m1, m1[:, :], lg, lg[:, :], sc[:, 0:1], None, ALU.is_equal, extra_reads=[sc])
                k.stt(lg2, lg2[:, :], m1, m1[:, :], -1e30, lg, lg[:, :], ALU.mult, ALU.add)
                S.op("dve", lambda e: e.reduce_max(out=sc[:, 1:2], in_=lg2[:, :], axis=AX.X), reads=[lg2], writes=[sc])
                k.ts(m2, m2[:, :], lg2, lg2[:, :], sc[:, 1:2], None, ALU.is_equal, extra_reads=[sc])
                k.tt(sc, sc[:, 2:3], sc, sc[:, 0:1], sc, sc[:, 1:2], ALU.subtract)
                k.act(sc, sc[:, 3:4], sc, sc[:, 2:3], AF.Sigmoid)
                k.ts(sc, sc[:, 4:5], sc, sc[:, 3:4], -1.0, 1.0, ALU.mult, ALU.add)
                k.ts(m1, m1[:, :], m1, m1[:, :], sc[:, 3:4], None, ALU.mult, extra_reads=[sc])
                k.stt(gw, gw[:, ti, :], m2, m2[:, :], sc[:, 4:5], m1, m1[:, :], ALU.mult, ALU.add, extra_reads=[sc])
        k.end_stage()
        w1, w3, w2 = ffn_weights(None, None, None)
        hT = [k.sb("hTe%d" % i, [128, 8, 512], BF16) for i in range(2)]
        gT = k.sb("gT", [128, NF, 512], BF16)
        sa_bufs = [k.sb("sa%d" % i, [128, 512], F32) for i in range(2)]
        accs = [k.sb("acc%d" % i, [128, D], F32) for i in range(2)]
        cnt = [0]
        ai = 0
        acc_tr = [T(None, "acc%d" % i) for i in range(TOK // 128)]
        for ex in range(NEXP):
            ffn_load(w1, w3, w2, I["exp_w1"][0, ex], I["exp_w3"][0, ex], I["exp_w2"][0, ex])
            for g in range(TOK // 512):
                h = hT[g % 2]
                k.dma("sp", h[:, :, :], h2T_d[:, :, g * 512:(g + 1) * 512].rearrange("c p t -> p c t"), writes=[h])
                ffn_up(h, w1, w3, gT, sa_bufs, cnt)
                for j in range(4):
                    tok = g * 512 + j * 128
                    ti = g * 4 + j
                    yps = (PS[4 + (j % 2) * 2], PS[5 + (j % 2) * 2])
                    ffn_down(gT, w2, j, yps)
                    acc = accs[ai % 2]
                    ai += 1
                    if ex == 0:
                        for hh in range(2):
                            k.ts(acc, acc[:, hh * 512:(hh + 1) * 512], yps[hh], yps[hh][:, :], gw[:, ti, ex:ex + 1], None, ALU.mult, extra_reads=[gw])
                    else:
                        k.dma("sp", acc[:, :], acc_d[tok:tok + 128, :], reads=[acc_tr[ti]], writes=[acc])
                        for hh in range(2):
                            k.stt(acc, acc[:, hh * 512:(hh + 1) * 512], yps[hh], yps[hh][:, :], gw[:, ti, ex:ex + 1],
                                  acc, acc[:, hh * 512:(hh + 1) * 512], ALU.mult, ALU.add, extra_reads=[gw])
                    k.dma("sp", acc_d[tok:tok + 128, :], acc[:, :], reads=[acc], writes=[acc_tr[ti]])
        k.end_stage()
        lng = load_bc_row("lng", I["ln_g"][1, 1, :])
        lnb = load_bc_row("lnb", I["ln_b"][1, 1, :])
        gbc = [load_bc_row("gbc%d" % b, mod_d[1, b, 5 * D:6 * D], add_one=True) for b in range(NBC)]
        xts = [k.sb("xt%d" % j, [128, D], F32) for j in range(2)]
        ys = [k.sb("ya%d" % j, [128, D], F32) for j in range(2)]
        outs = [k.sb("xo%d" % j, [128, D], F32) for j in range(2)]
        tmp = k.sb("tmp", [128, D], F32)
        r = k.sb("r", [128, D], F32)
        stats = k.sb("stats", [128, 2, 6], F32)
        mv = new_mv("mv")
        for ti in range(TOK // 128):
            b = ti // 16
            tok = ti * 128
            xt, ya, ot = xts[ti % 2], ys[ti % 2], outs[ti % 2]
            k.dma("sp", xt[:, :], x3_d[tok:tok + 128, :], writes=[xt])
            k.dma("sp", ya[:, :], acc_d[tok:tok + 128, :], writes=[ya])
            resid_ln(xt, ya, gbc[b], lng, lnb, tmp, r, ot, stats, mv)
            k.dma("sp", out_d[tok:tok + 128, :], ot[:, :], reads=[ot])
        k.end_stage()

    S.barrier()
    with ExitStack() as st:
        S.emit(st)
    return nc, k


def skewed(items):
    n = len(items)
    if n and len(items[0]) == 3:
        for t in range(-2, n):
            if 0 <= t + 2 < n:
                items[t + 2][0]()
            if 0 <= t + 1 < n:
                items[t + 1][1]()
            if 0 <= t < n:
                items[t][2]()
        return
    depth = 2
    for i in range(min(depth, n)):
        items[i][0]()
    for i in range(n):
        if i + depth < n:
            items[i + depth][0]()
        items[i][1]()


def stage_attention(k, nc, I, C, dt_, ident, identb, qkT_d, vtok_d, gates_d, kcT_d, vc_d, oT_d):
    S = k.S
    PS = k.ps
    TINY = 1e-30
    cb = k.sb("cb", [128, 512], BF16)
    cbs = k.sb("cbs", [128, 512], BF16)
    wbm = k.sb("wbm", [128, 512], BF16)
    negtri = k.sb("negtri", [128, 128], BF16)
    negones = k.sb("negones", [128, 128], BF16)
    cmpbias = k.sb("cmpbias", [128, SEQ], BF16)
    expand = k.sb("expand", [32, 16, 128], BF16)
    selA = k.sb("selA", [128, 16, 32], F32)
    selB = k.sb("selB", [128, 16, 32], F32)
    for t, n in ((cb, "cb"), (cbs, "cbs"), (wbm, "wb"), (negtri, "negtri"), (negones, "negones"), (cmpbias, "cmpbias")):
        k.dma("pool", t[:, :], C[n], writes=[t])
    k.dma("pool", expand[:, :, :], C["expand"], writes=[expand])
    k.dma("sp", selA[:, :, :], C["selA"], writes=[selA])
    k.dma("sp", selB[:, :, :], C["selB"], writes=[selB])
    QK_IDX = [0, 1, 2, 3, 4, 5, 6, 7, 8, 9, 10, 11, 14, 15, 16, 17]
    qk = {ci: k.sb("qk%d" % ci, [128, SEQ], BF16) for ci in QK_IDX}
    sbv = k.sb("sbv", [128, 16, 512], BF16)
    vslc = k.sb("vslc", [128, 16, 2, 66], BF16)
    vwin = k.sb("vwin", [128, 16, 2, 66], BF16)
    gates = k.sb("gates", [128, 16, 24], F32)
    kcT = k.sb("kcT", [128, 2, 128], BF16)
    vcaug = k.sb("vcaug", [128, 2, 98], BF16)
    selT = k.sb("selT", [32, 2, SEQ], BF16)
    imp = k.sb("imp", [128, 16, 2, 32], F32)
    imp2 = k.sb("imp2", [128, 16, 32], F32)
    cmp3 = k.sb("cmp3", [128, 32, 32], BF16)
    rank = k.sb("rank", [128, 32], F32)
    selbias = k.sb("selbias", [128, 16, 32], BF16)
    rd = [k.sb("rd%d" % i, [128, 8], F32) for i in range(4)]
    tmpi = k.sb("tmpi", [128, 4, 32], F32)
    tmpo = [k.sb("tmpo%d" % i, [128, 4, 64], F32) for i in range(2)]
    Ecmp = [k.sb("Ecmp%d" % i, [128, 512], BF16) for i in range(2)]
    Pb = [k.sb("Pb%d" % i, [128, 512], BF16) for i in range(4)]
    e32 = [k.sb("e32_%d" % i, [128, 512], F32) for i in range(2)]
    spb = [k.sb("spb%d" % i, [128, 512], BF16) for i in range(4)]
    Wb = [k.sb("Wb%d" % i, [128, 512], BF16) for i in range(3)]
    accb = k.sb("accb", [128, 512], BF16)
    onsa = [k.sb("onsa%d" % i, [128, 4, 128], F32) for i in range(2)]
    onsab = [k.sb("onsab%d" % i, [128, 4, 128], BF16) for i in range(2)]
    ostage = [k.sb("ostage%d" % i, [128, 512], BF16) for i in range(3)]
    k.memset(vslc, vslc[:, :, :, 64:66], 1.0)
    k.memset(vwin, vwin[:, :, :, 64:66], 1.0)
    k.memset(vcaug, vcaug[:, :, 64:65], 1.0)
    for g in range(2):
        k.dma("pool", vcaug[:, g, 65:97], C["overlap"], writes=[vcaug])
    ctr = {}

    def nxt(name, n):
        v = ctr.get(name, 0)
        ctr[name] = v + 1
        return v % n

    for b in range(NBC):
        for ci in QK_IDX:
            k.dma("sp", qk[ci][:, :], qkT_d[b, ci, :, :], writes=[qk[ci]])
        k.dma("sp", sbv[:, :, :], vtok_d[b, :, 0:512].rearrange("(t p) c -> p t c", p=128), writes=[sbv])
        for g in range(2):
            k.dma("sp", vslc[:, :, g, 0:64], vtok_d[b, :, 512 + g * 64:512 + (g + 1) * 64].rearrange("(t p) c -> p t c", p=128), writes=[vslc])
            k.dma("sp", vwin[:, :, g, 0:64], vtok_d[b, :, 640 + g * 64:640 + (g + 1) * 64].rearrange("(t p) c -> p t c", p=128), writes=[vwin])
            k.dma("sp", kcT[:, g, :], kcT_d[b, g], writes=[kcT])
            k.dma("sp", vcaug[:, g, 0:64], vc_d[b, g], writes=[vcaug])
        k.dma("sp", gates[:, :, :], gates_d[b].rearrange("(t p) c -> p t c", p=128), writes=[gates])

        def cmp_scores(g, hq, qg, ncols_rhs):
            chunk = 8 + hq // 2
            po = 64 * (hq % 2)
            t0 = qg * 512
            p = PS[nxt("cs", 2)]
            k.mm(p, p[:, :], kcT, kcT[po:po + 64, g, :], qk[chunk], qk[chunk][po:po + 64, t0:t0 + 512], start=True, stop=False)
            k.mm(p, p[:, :], identb, identb[:, :], cmpbias, cmpbias[:, t0:t0 + 512], start=False, stop=True)
            E = Ecmp[nxt("ec", 2)]
            k.act(E, E[:, :], p, p[:, :], AF.Exp)
            R = PS[2 + nxt("cr", 2)]
            for sub in range(4):
                k.mm(R, R[:, sub * 128:sub * 128 + ncols_rhs], E, E[:, sub * 128:(sub + 1) * 128], vcaug, vcaug[:, g, 0:ncols_rhs],
                     start=True, stop=True)
            return R

        def recip_den(R, col):
            rdt = rd[nxt("rd", 4)]
            Rv = R[:, :].rearrange("p (s c) -> p s c", s=4)
            k.ts(rdt, rdt[:, 0:4], R, Rv[:, :, col], TINY, None, ALU.max)
            S.op("dve", lambda e: e.reciprocal(out=rdt[:, 0:4], in_=rdt[:, 0:4]), reads=[rdt], writes=[rdt])
            return rdt, Rv

        for g in range(2):
            for r in range(4):
                hq = 4 * g + r
                for qg in range(4):
                    R = cmp_scores(g, hq, qg, 97)
                    rdt, Rv = recip_den(R, 64)
                    bc = rdt[:, 0:4].unsqueeze(2).broadcast_to([128, 4, 32])
                    if r == 0:
                        k.tt(imp, imp[:, 4 * qg:4 * qg + 4, g, :], R, Rv[:, :, 65:97], rdt, bc, ALU.mult)
                    else:
                        k.tt(tmpi, tmpi[:, :, :], R, Rv[:, :, 65:97], rdt, bc, ALU.mult)
                        k.tt(imp, imp[:, 4 * qg:4 * qg + 4, g, :], imp, imp[:, 4 * qg:4 * qg + 4, g, :], tmpi, tmpi[:, :, :], ALU.add)
        for g in range(2):
            k.tt(imp2, imp2[:, :, :], imp, imp[:, :, g, :], selA, selA[:, :, :], ALU.mult)
            k.tt(imp2, imp2[:, :, :], imp2, imp2[:, :, :], selB, selB[:, :, :], ALU.add)
            for tl in range(16):
                a = imp2[:, tl, :]
                in0 = a.unsqueeze(1).broadcast_to([128, 32, 32])
                in1 = a.unsqueeze(2).broadcast_to([128, 32, 32])
                k.tt(cmp3, cmp3[:, :, :], imp2, in0, imp2, in1, ALU.is_gt)
                S.op("dve", lambda e: e.reduce_sum(out=rank[:, :], in_=cmp3[:, :, :], axis=AX.X), reads=[cmp3], writes=[rank])
                k.ts(selbias, selbias[:, tl, :], rank, rank[:, :], 15.5, NEGB, ALU.is_gt, ALU.mult)
            for half in range(2):
                p = PS[4 + half]
                pv = p[:, :].bitcast(BF16)
                for t8 in range(8):
                    tl = half * 8 + t8
                    k.tr(p, pv[0:32, t8 * 128:(t8 + 1) * 128], selbias, selbias[:, tl, :], identb, identb[:, :])
                k.cp(selT, selT[0:32, g, half * 1024:(half + 1) * 1024], p, pv[0:32, 0:1024], eng="act")

        all_items = []
        for g in range(2):
            for pair in range(2):
                for qg in range(4):
                    t0 = qg * 512
                    on = onsa[nxt("on", 2)]
                    for r2 in range(2):
                        hq = 4 * g + 2 * pair + r2
                        chunk = 8 + hq // 2
                        po = 64 * r2
                        qT = qk[chunk]

                        def pre(g=g, hq=hq, qg=qg, on=on, po=po):
                            R = cmp_scores(g, hq, qg, 65)
                            rdt, Rv = recip_den(R, 64)
                            k.tt(rdt, rdt[:, 4:8], rdt, rdt[:, 0:4], gates, gates[:, 4 * qg:4 * qg + 4, hq * 3 + 0], ALU.mult)
                            k.tt(on, on[:, :, po:po + 64], R, Rv[:, :, 0:64], rdt, rdt[:, 4:8].unsqueeze(2).broadcast_to([128, 4, 64]), ALU.mult)

                        for br in range(2):
                            kT = qk[14 + g] if br == 0 else qk[16 + g]
                            vaug = vslc if br == 0 else vwin
                            ACC = PS[4 + br]
                            if br == 0:
                                kbs = list(range(0, 4 * qg + 4))
                            else:
                                kbs = list(range(max(0, 4 * qg - 4), 4 * qg + 4))
                            started = [False] * 4
                            items = []
                            for kb in kbs:
                                diag = kb >= 4 * qg
                                if diag:
                                    o = (kb - 4 * qg) * 128
                                    c0, c1 = o, 512
                                    bias_t, bias_ap = cb, cb[:, 0:512 - o]
                                    subs = list(range(o // 128, 4))
                                elif br == 1:
                                    m = kb - (4 * qg - 4)
                                    c0, c1 = 0, 128 * (m + 1)
                                    bias_t, bias_ap = wbm, wbm[:, 384 - 128 * m:512]
                                    subs = list(range(0, m + 1))
                                else:
                                    c0, c1 = 0, 512
                                    bias_t, bias_ap = None, None
                                    subs = [0, 1, 2, 3]
                                nco = c1 - c0
                                st = {}

                                def s1(kb=kb, c0=c0, c1=c1, nco=nco, bias_t=bias_t, bias_ap=bias_ap, st=st, kT=kT, br=br,
                                       po=po, qT=qT, t0=t0, g=g):
                                    Pt = Pb[nxt("pt", 4)]
                                    sp_ = PS[6 + nxt("ss", 2)]
                                    st["Pt"] = Pt
                                    k.mm(sp_, sp_[:, 0:nco], kT, kT[po:po + 64, kb * 128:(kb + 1) * 128], qT, qT[po:po + 64, t0 + c0:t0 + c1],
                                         start=True, stop=(br == 1 and bias_t is None))
                                    if br == 0:
                                        k.mm(sp_, sp_[:, 0:nco], expand, expand[0:32, kb, :], selT, selT[0:32, g, t0 + c0:t0 + c1],
                                             start=False, stop=(bias_t is None))
                                    if bias_t is not None:
                                        k.mm(sp_, sp_[:, 0:nco], identb, identb[:, :], bias_t, bias_ap, start=False, stop=True)
                                    k.act(Pt, Pt[:, 0:nco], sp_, sp_[:, 0:nco], AF.Exp)

                                def s2(kb=kb, c0=c0, subs=subs, st=st, vaug=vaug, ACC=ACC, started=started, qg=qg, g=g):
                                    Pt = st["Pt"]
                                    for sub in subs:
                                        lo = sub * 128 - c0
                                        is_last = (kb == 4 * qg + sub)
                                        k.mm(ACC, ACC[:, sub * 128:sub * 128 + 65], Pt, Pt[:, lo:lo + 128], vaug, vaug[:, kb, g, 0:65],
                                             start=(not any(started)), stop=is_last, skip=True)
                                        started[sub] = True
                                items.append([s1, s2])

                            def post(ACC=ACC, qg=qg, hq=hq, br=br, on=on, po=po):
                                rdt, Av = recip_den(ACC, 64)
                                k.tt(rdt, rdt[:, 4:8], rdt, rdt[:, 0:4], gates, gates[:, 4 * qg:4 * qg + 4, hq * 3 + 1 + br], ALU.mult)
                                to = tmpo[nxt("to", 2)]
                                k.tt(to, to[:, :, :], ACC, Av[:, :, 0:64], rdt, rdt[:, 4:8].unsqueeze(2).broadcast_to([128, 4, 64]), ALU.mult)
                                k.tt(on, on[:, :, po:po + 64], on, on[:, :, po:po + 64], to, to[:, :, :], ALU.add)

                            def fin(on=on, g=g, pair=pair, t0=t0):
                                onb = onsab[nxt("onb", 2)]
                                k.cp(onb, onb[:, :, :], on, on[:, :, :], eng="dve")
                                p = PS[2 + nxt("cr", 2)]
                                pv = p[:, :].bitcast(BF16)
                                for sub in range(4):
                                    k.tr(p, pv[:, sub * 128:(sub + 1) * 128], onb, onb[:, sub, :], identb, identb[:, :])
                                ost = ostage[nxt("os", 3)]
                                k.cp(ost, ost[:, :], p, pv[:, 0:512], eng="act")
                                k.dma("sp", oT_d[b, 4 + 2 * g + pair, :, t0:t0 + 512], ost[:, :], reads=[ost])

                            def chain(*fns):
                                def f():
                                    for fn in fns:
                                        fn()
                                return f
                            if br == 0:
                                items[0][0] = chain(pre, items[0][0])
                            if br == 1 and r2 == 1:
                                items[-1][1] = chain(items[-1][1], post, fin)
                            else:
                                items[-1][1] = chain(items[-1][1], post)
                            all_items.extend(items)
        skewed([tuple(it) for it in all_items])

        items = []
        for h in range(8):
            c = h // 2
            po = 64 * (h % 2)
            qT = qk[c]
            kT = qk[4 + c]
            for qg in range(4):
                t0 = qg * 512
                OT = PS[4 + ((h * 4 + qg) % 2)]
                kbs = list(range(4 * qg + 3, -1, -1))
                for bi, kb in enumerate(kbs):
                    diag = kb >= 4 * qg
                    o = (kb - 4 * qg) * 128 if diag else 0
                    nco = 512 - o
                    first = (bi == 0)

                    def s1(kb=kb, o=o, nco=nco, diag=diag, qT=qT, kT=kT, po=po, t0=t0, st={}):
                        p1 = PS[nxt("d1", 2)]
                        k.mm(p1, p1[:, 0:nco], kT, kT[po:po + 64, kb * 128:(kb + 1) * 128], qT, qT[po:po + 64, t0 + o:t0 + 512],
                             start=True, stop=(not diag))
                        if diag:
                            k.mm(p1, p1[:, 0:nco], identb, identb[:, :], cbs, cbs[:, 0:nco], start=False, stop=True)
                        e = e32[nxt("e", 2)]
                        k.act(e, e[:, 0:nco], p1, p1[:, 0:nco], AF.Exp)
                        spt = spb[nxt("sp", 4)]
                        k.act(spt, spt[:, 0:nco], e, e[:, 0:nco], AF.Ln, bias=1.0)
                        st["sp"] = spt

                    items.append([s1, None, dict(kb=kb, o=o, nco=nco, diag=diag, first=first, qT=qT, kT=kT, po=po, t0=t0, OT=OT, c=c, h=h, qg=qg)])
        def make_s2(s1, d):
            st = s1.__defaults__[-1]

            def s2():
                kb, o, nco, diag, first = d["kb"], d["o"], d["nco"], d["diag"], d["first"]
                qT, kT, po, t0, OT, c, h, qg = d["qT"], d["kT"], d["po"], d["t0"], d["OT"], d["c"], d["h"], d["qg"]
                spt = st["sp"]
                p2 = PS[2 + nxt("d2", 2)]
                k.mm(p2, p2[:, 0:nco], kT, kT[po:po + 64, kb * 128:(kb + 1) * 128], qT, qT[po:po + 64, t0 + o:t0 + 512], start=True, stop=False)
                if diag:
                    k.mm(p2, p2[:, 0:nco], identb, identb[:, :], cbs, cbs[:, 0:nco], start=False, stop=False)
                has_acc = not first
                k.mm(p2, p2[:, 0:nco], negtri, negtri[:, :], spt, spt[:, 0:nco], start=False, stop=(not has_acc))
                if has_acc:
                    if diag:
                        k.mm(p2, p2[:, 128:nco], negones, negones[:, :], accb, accb[:, o + 128:512], start=False, stop=True)
                    else:
                        k.mm(p2, p2[:, 0:512], negones, negones[:, :], accb, accb[:, 0:512], start=False, stop=True)
                W = Wb[nxt("w", 3)]
                st["W"] = W
                k.act(W, W[:, 0:nco], p2, p2[:, 0:nco], AF.Exp)
                if diag:
                    k.cp(accb, accb[:, o:o + 128], spt, spt[:, 0:128], eng="dve")
                    if nco > 128:
                        k.tt(accb, accb[:, o + 128:512], accb, accb[:, o + 128:512], spt, spt[:, 128:nco], ALU.add)
                else:
                    k.tt(accb, accb[:, :], accb, accb[:, :], spt, spt[:, :], ALU.add)

            def s3():
                kb, o, nco, diag, first = d["kb"], d["o"], d["nco"], d["diag"], d["first"]
                po, t0, OT, c = d["po"], d["t0"], d["OT"], d["c"]
                W = st["W"]
                last = (kb == 0)
                vl = sbv[:, kb, c * 128:(c + 1) * 128]
                if diag:
                    k.mm(OT, OT[:, o:o + 128], sbv, vl, W, W[:, 0:128], start=first, stop=last, skip=True)
                    if nco > 128:
                        k.mm(OT, OT[:, o + 128:512], sbv, vl, W, W[:, 128:nco], start=False, stop=last, skip=True)
                else:
                    k.mm(OT, OT[:, 0:512], sbv, vl, W, W[:, 0:512], start=False, stop=last, skip=True)
                if last:
                    ost = ostage[nxt("os", 3)]
                    k.cp(ost, ost[po:po + 64, :], OT, OT[po:po + 64, :], eng="dve")
                    k.dma("sp", oT_d[b, c, po:po + 64, t0:t0 + 512], ost[po:po + 64, :], reads=[ost])
            return s2, s3
        its = [(it[0],) + make_s2(it[0], it[2]) for it in items]
        skewed(its)


def stage_moe_sparse(k, nc, I, C, ident, identb, modT, modP, mod_d, x3_d, out_d, transpose_mod, resid_ln, load_bc_row, new_mv):
    S = k.S
    PS = k.ps
    NT = TOK // 128
    NB = MOE_NBLK
    BIG = 100000.0
    dk = "ExternalOutput" if DBG.get("moe_dbg") else "Internal"
    xs_d = nc.dram_tensor("xs_d", [NSLOT, D], BF16, kind=dk).ap()
    yo_d = nc.dram_tensor("yo_d", [NSLOT, D], F32, kind=dk).ap()
    gwt = k.pers("gwt", [128, NT, 2], F32)
    sloti = k.pers("sloti", [128, NT, 2], I32)
    widx1 = k.pers("widx1", [128, NB, 16], I32)
    widx2 = k.pers("widx2", [128, NB, NF], I32)

    rw = k.sb("rw", [128, 8, NEXP], F32)
    rb = k.sb("rb", [128, NEXP], F32)
    trilt = k.sb("trilt", [128, 128], BF16)
    onesb = k.sb("onesb", [128, 128], BF16)
    pidx = k.sb("pidx", [128, 1], F32)
    thr = k.sb("thr", [128, NB + 8], F32)
    k.dma("sp", rw[:, :, :], I["router_w"][0].rearrange("(kc p) e -> p kc e", p=128), writes=[rw])
    k.dma("sp", rb[:, :], I["router_b"][0, :].partition_broadcast(128), writes=[rb])
    k.dma("pool", trilt[:, :], C["trilt"], writes=[trilt])
    k.dma("pool", onesb[:, :], C["ones"], writes=[onesb])
    k.dma("sp", pidx[:, :], C["pidx"], writes=[pidx])
    k.dma("sp", thr[:, :], C["thr"], writes=[thr])
    zt = k.sb("zt", [128, 16384], BF16)
    k.memset(zt, zt[:, :], 0.0)
    xs_tr = T(None, "xs_dram")
    xs_flat = xs_d.rearrange("(p a) c -> p (a c)", p=128)
    for i in range(NSLOT * D // 128 // 16384):
        k.dma("sp", xs_flat[:, i * 16384:(i + 1) * 16384], zt[:, :], reads=[zt], writes=[xs_tr])
    scb = [load_bc_row("scb%d" % b, mod_d[1, b, 4 * D:5 * D], add_one=True) for b in range(NBC)]
    shb = [load_bc_row("shb%d" % b, mod_d[1, b, 3 * D:4 * D]) for b in range(NBC)]
    xts = [k.sb("xt%d" % j, [128, D], F32) for j in range(4)]
    hT32 = k.sb("hT32", [128, 8, 512], F32)
    h2tok = k.sb("h2tok", [128, NT, D], BF16)
    tmp32 = k.sb("tmp32", [128, D], F32)
    posall = k.sb("posall", [128, NT, NEXP], F32)
    m1s = k.sb("m1s", [128, NT, NEXP], F32)
    m2s = k.sb("m2s", [128, NT, NEXP], F32)
    Macc = k.sb("Macc", [128, NEXP], F32)
    Maccb = k.sb("Maccb", [128, NEXP], BF16)
    Mbs = [k.sb("Mb%d" % i, [128, NEXP], BF16) for i in range(2)]
    lg = k.sb("lg", [128, NEXP], F32)
    lg2 = k.sb("lg2", [128, NEXP], F32)
    sc = k.sb("sc", [128, 8], F32)
    k.memset(Macc, Macc[:, :], 0.0)
    k.memset(Maccb, Maccb[:, :], 0.0)
    for g in range(TOK // 512):
        b = g // 4
        for j in range(4):
            tok = g * 512 + j * 128
            k.dma("sp", xts[j][:, :], x3_d[tok:tok + 128, :], writes=[xts[j]])
        transpose_mod(xts, None, 1, b, 3, 4, (0, 1), hT32=hT32)
        for j in range(4):
            ti = g * 4 + j
            k.tt(tmp32, tmp32[:, :], xts[j], xts[j][:, :], scb[b], scb[b][:, :], ALU.mult)
            k.tt(h2tok, h2tok[:, ti, :], tmp32, tmp32[:, :], shb[b], shb[b][:, :], ALU.add)
            p = PS[2 + j % 2]
            for kc in range(8):
                k.mm(p, p[:, 0:NEXP], hT32, hT32[:, kc, j * 128:(j + 1) * 128], rw, rw[:, kc, :], start=(kc == 0), stop=(kc == 7))
            k.tt(lg, lg[:, :], p, p[:, 0:NEXP], rb, rb[:, :], ALU.add)
            S.op("dve", lambda e: e.reduce_max(out=sc[:, 0:1], in_=lg[:, :], axis=AX.X), reads=[lg], writes=[sc])
            k.ts(m1s, m1s[:, ti, :], lg, lg[:, :], sc[:, 0:1], None, ALU.is_equal, extra_reads=[sc])
            k.stt(lg2, lg2[:, :], m1s, m1s[:, ti, :], -1e30, lg, lg[:, :], ALU.mult, ALU.add)
            S.op("dve", lambda e: e.reduce_max(out=sc[:, 1:2], in_=lg2[:, :], axis=AX.X), reads=[lg2], writes=[sc])
            k.ts(m2s, m2s[:, ti, :], lg2, lg2[:, :], sc[:, 1:2], None, ALU.is_equal, extra_reads=[sc])
            k.tt(sc, sc[:, 2:3], sc, sc[:, 0:1], sc, sc[:, 1:2], ALU.subtract)
            k.act(gwt, gwt[:, ti, 0:1], sc, sc[:, 2:3], AF.Sigmoid)
            k.ts(gwt, gwt[:, ti, 1:2], gwt, gwt[:, ti, 0:1], -1.0, 1.0, ALU.mult, ALU.add)
            Mb = Mbs[ti % 2]
            k.tt(Mb, Mb[:, :], m1s, m1s[:, ti, :], m2s, m2s[:, ti, :], ALU.add)
            pp = PS[4 + ti % 2]
            k.mm(pp, pp[:, 0:NEXP], trilt, trilt[:, :], Mb, Mb[:, :], start=True, stop=(ti == 0))
            if ti > 0:
                k.mm(pp, pp[:, 0:NEXP], onesb, onesb[:, :], Maccb, Maccb[:, :], start=False, stop=True)
            k.cp(posall, posall[:, ti, :], pp, pp[:, 0:NEXP])
            k.tt(Macc, Macc[:, :], Macc, Macc[:, :], Mb, Mb[:, :], ALU.add)
            k.cp(Maccb, Maccb[:, :], Macc, Macc[:, :])
    cnt = k.sb("cnt", [128, NEXP], F32)
    cmpA = k.sb("cmpA", [128, NEXP, 8], F32)
    nblk = k.sb("nblk", [128, NEXP], F32)
    pst = k.sb("pst", [128, NEXP + 1], F32)
    cmpB = k.sb("cmpB", [128, NB, NEXP], F32)
    blk = k.sb("blk", [128, NB], F32)
    b1 = k.sb("b1", [128, NB], F32)
    b2 = k.sb("b2", [128, NB], F32)
    pidx2 = k.sb("pidx2", [128, 1], F32)
    w1f = k.sb("w1f", [128, NB, 16], F32)
    w2f = k.sb("w2f", [128, NB, NF], F32)
    posp = k.sb("posp", [128, NT, NEXP], F32)
    slotf = k.sb("slotf", [128, NT, 2], F32)
    pc = PS[6]
    k.mm(pc, pc[:, 0:NEXP], onesb, onesb[:, :], Maccb, Maccb[:, :], start=True, stop=True)
    k.cp(cnt, cnt[:, :], pc, pc[:, 0:NEXP])
    k.tt(cmpA, cmpA[:, :, :], cnt, cnt[:, :].unsqueeze(2).broadcast_to([128, NEXP, 8]),
         thr, thr[:, 0:8].unsqueeze(1).broadcast_to([128, NEXP, 8]), ALU.is_gt)
    S.op("dve", lambda e: e.reduce_sum(out=nblk[:, :], in_=cmpA[:, :, :], axis=AX.X), reads=[cmpA], writes=[nblk])
    k.memset(pst, pst[:, :], 0.0)
    for ex in range(NEXP):
        k.stt(pst, pst[:, ex + 1:ex + 2], nblk, nblk[:, ex:ex + 1], float(MOE_BS), pst, pst[:, ex:ex + 1], ALU.mult, ALU.add)
    k.tt(cmpB, cmpB[:, :, :], pst, pst[:, 1:NEXP + 1].unsqueeze(1).broadcast_to([128, NB, NEXP]),
         thr, thr[:, 0:NB].unsqueeze(2).broadcast_to([128, NB, NEXP]), ALU.is_le)
    S.op("dve", lambda e: e.reduce_sum(out=blk[:, :], in_=cmpB[:, :, :], axis=AX.X), reads=[cmpB], writes=[blk])
    k.ts(blk, blk[:, :], blk, blk[:, :], float(NEXP - 1), None, ALU.min)
    k.ts(pidx2, pidx2[:, :], pidx, pidx[:, :], 2.0, None, ALU.mult)
    k.ts(b1, b1[:, :], blk, blk[:, :], 2048.0, pidx2[:, 0:1], ALU.mult, ALU.add, extra_reads=[pidx2])
    k.ts(b2, b2[:, :], blk, blk[:, :], float(DFF), pidx[:, 0:1], ALU.mult, ALU.add, extra_reads=[pidx])
    for c in range(16):
        kc, h = c // 2, c % 2
        k.ts(w1f, w1f[:, :, c], b1, b1[:, :], float(kc * 256 + h), None, ALU.add)
    for f in range(NF):
        k.ts(w2f, w2f[:, :, f], b2, b2[:, :], float(f * 128), None, ALU.add)
    k.cp(widx1, widx1[:, :, :], w1f, w1f[:, :, :])
    k.cp(widx2, widx2[:, :, :], w2f, w2f[:, :, :])
    k.tt(posp, posp[:, :, :], posall, posall[:, :, :], pst, pst[:, 0:NEXP].unsqueeze(1).broadcast_to([128, NT, NEXP]), ALU.add)
    for kk, ms in ((0, m1s), (1, m2s)):
        k.tt(ms, ms[:, :, :], ms, ms[:, :, :], posp, posp[:, :, :], ALU.mult)
        S.op("dve", (lambda kk_, ms_: (lambda e: e.reduce_sum(out=slotf[:, :, kk_], in_=ms_[:, :, :], axis=AX.X)))(kk, ms),
             reads=[ms], writes=[slotf])
    k.cp(sloti, sloti[:, :, :], slotf, slotf[:, :, :])
    if DBG.get("moe_dbg"):
        md = nc.dram_tensor("moe_dbg", [128, 512], F32, kind="ExternalOutput").ap()
        k.dma("sp", md[:, 0:64], slotf[:, :, :].rearrange("p a b -> p (a b)"), reads=[slotf])
        k.dma("sp", md[:, 64:128], gwt[:, :, :].rearrange("p a b -> p (a b)"), reads=[gwt])
        k.dma("sp", md[:, 128:128 + NB], blk[:, :], reads=[blk])
        k.dma("sp", md[:, 160:169], pst[:, :], reads=[pst])
        k.dma("sp", md[:, 176:184], cnt[:, :], reads=[cnt])
        k.dma("sp", md[:, 256:512], posall[:, :, :].rearrange("p a b -> p (a b)"), reads=[posall])
    for ti in range(NT):
        for kk in range(2):
            k.scatter(xs_d, h2tok[:, ti, :], sloti[:, ti, kk:kk + 1], reads=[h2tok, sloti, xs_tr])
    k.end_stage()
    if DBG.get("moe_stop") == "a":
        return

    w1h = k.sb("ew1", [128, 8, DFF], BF16)
    w3h = k.sb("ew3", [128, 8, DFF], BF16)
    w2h = k.sb("ew2", [128, NF, D], BF16)
    w1T = [[k.view(w1h, "ew1_%d_%d" % (i, h)) for h in range(2)] for i in range(8)]
    w3T = [[k.view(w3h, "ew3_%d_%d" % (i, h)) for h in range(2)] for i in range(8)]
    w2T = [k.view(w2h, "ew2_%d" % i) for i in range(NF)]
    xt = [k.sb("xs%d" % j, [128, D], BF16) for j in range(4)]
    hTs = [k.sb("hTe%d" % i, [128, 8, 512], BF16) for i in range(2)]
    gT = k.sb("gT", [128, NF, 512], BF16)
    sab = [k.sb("sa%d" % i, [128, 512], F32) for i in range(2)]
    yo = [k.sb("yo%d" % i, [128, D], F32) for i in range(2)]
    HALF = DFF // 2
    w1src = I["exp_w1"][0].rearrange("e r c -> (e r) c")
    w3src = I["exp_w3"][0].rearrange("e r c -> (e r) c")
    w2src = I["exp_w2"][0].rearrange("e f n -> (e f) n")
    cntr = [0, 0]
    for kb in range(DBG.get("moe_nb", NB)):
        for h in range(2):
            for kc in range(8):
                k.gather(w1h[:, kc, h * HALF:(h + 1) * HALF], w1src, widx1[:, kb, kc * 2 + h:kc * 2 + h + 1],
                         reads=[widx1], writes=[w1T[kc][h]])
                k.gather(w3h[:, kc, h * HALF:(h + 1) * HALF], w3src, widx1[:, kb, kc * 2 + h:kc * 2 + h + 1],
                         reads=[widx1], writes=[w3T[kc][h]])
        for f in range(NF):
            k.gather(w2h[:, f, :], w2src, widx2[:, kb, f:f + 1], reads=[widx2], writes=[w2T[f]])
        for j in range(4):
            r0 = kb * MOE_BS + j * 128
            k.dma("sp", xt[j][:, :], xs_d[r0:r0 + 128, :], writes=[xt[j]])
        hT = hTs[kb % 2]
        for c in range(8):
            pb = PS[4 + c % 2]
            pv = pb[:, :].bitcast(BF16)
            for j in range(4):
                k.tr(pb, pv[:, j * 128:(j + 1) * 128], xt[j], xt[j][:, c * 128:(c + 1) * 128], identb, identb[:, :])
            k.cp(hT, hT[:, c, :], pb, pv[:, 0:512], eng="act")
        for f in range(NF):
            pa = PS[(cntr[0] % 2) * 2]
            pb = PS[(cntr[0] % 2) * 2 + 1]
            sa = sab[cntr[0] % 2]
            cntr[0] += 1
            fh = 0 if f < NF // 2 else 1
            for kc in range(8):
                k.mm(pa, pa[:, :], w1T[kc][fh], w1h[:, kc, f * 128:(f + 1) * 128], hT, hT[:, kc, :], start=(kc == 0), stop=(kc == 7))
            for kc in range(8):
                k.mm(pb, pb[:, :], w3T[kc][fh], w3h[:, kc, f * 128:(f + 1) * 128], hT, hT[:, kc, :], start=(kc == 0), stop=(kc == 7))
            k.act(sa, sa[:, :], pa, pa[:, :], AF.Silu)
            k.tt(gT, gT[:, f, :], sa, sa[:, :], pb, pb[:, :], ALU.mult)
        for j in range(4):
            yps = (PS[4 + (j % 2) * 2], PS[5 + (j % 2) * 2])
            for hh in range(2):
                for f in range(NF):
                    k.mm(yps[hh], yps[hh][:, :], gT, gT[:, f, j * 128:(j + 1) * 128], w2T[f], w2h[:, f, hh * 512:(hh + 1) * 512],
                         start=(f == 0), stop=(f == NF - 1))
            y = yo[cntr[1] % 2]
            cntr[1] += 1
            k.cp(y, y[:, 0:512], yps[0], yps[0][:, :], eng="act")
            k.cp(y, y[:, 512:1024], yps[1], yps[1][:, :], eng="dve")
            r0 = kb * MOE_BS + j * 128
            k.dma("sp", yo_d[r0:r0 + 128, :], y[:, :], reads=[y])
    k.end_stage()
    if DBG.get("moe_stop") == "b":
        return

    lng = load_bc_row("lng", I["ln_g"][1, 1, :])
    lnb = load_bc_row("lnb", I["ln_b"][1, 1, :])
    gbc = [load_bc_row("gbc%d" % b, mod_d[1, b, 5 * D:6 * D], add_one=True) for b in range(NBC)]
    xts = [k.sb("xt%d" % j, [128, D], F32) for j in range(2)]
    r1s = [k.sb("r1_%d" % j, [128, D], F32) for j in range(2)]
    r2s = [k.sb("r2_%d" % j, [128, D], F32) for j in range(2)]
    outs = [k.sb("xo%d" % j, [128, D], F32) for j in range(2)]
    tmp = k.sb("tmp", [128, D], F32)
    r = k.sb("r", [128, D], F32)
    stats = k.sb("stats", [128, 2, 6], F32)
    mv = new_mv("mv")
    for ti in range(NT):
        b = ti // (SEQ // 128)
        tok = ti * 128
        xt_, r1, r2, ot = xts[ti % 2], r1s[ti % 2], r2s[ti % 2], outs[ti % 2]
        k.dma("sp", xt_[:, :], x3_d[tok:tok + 128, :], writes=[xt_])
        k.gather(r1[:, :], yo_d, sloti[:, ti, 0:1], reads=[sloti], writes=[r1])
        k.gather(r2[:, :], yo_d, sloti[:, ti, 1:2], reads=[sloti], writes=[r2])
        k.ts(r1, r1[:, :], r1, r1[:, :], gwt[:, ti, 0:1], None, ALU.mult, extra_reads=[gwt])
        k.stt(r1, r1[:, :], r2, r2[:, :], gwt[:, ti, 1:2], r1, r1[:, :], ALU.mult, ALU.add, extra_reads=[gwt])
        resid_ln(xt_, r1, gbc[b], lng, lnb, tmp, r, ot, stats, mv)
        k.dma("sp", out_d[tok:tok + 128, :], ot[:, :], reads=[ot])
    k.end_stage()


_CACHE = {}


def kernel(**inputs):
    if "nc" not in _CACHE:
        _CACHE["nc"] = build_program()[0]
        _CACHE["cst"] = host_consts()
    nc = _CACHE["nc"]
    cst = _CACHE["cst"]
    x = np.ascontiguousarray(inputs["x"], dtype=np.float32)
    c = np.ascontiguousarray(inputs["c"], dtype=np.float32)
    in_maps = []
    for core in range(NCORES):
        m = {}
        for n in INPUT_SHAPES:
            if n == "x":
                m[n] = x[core * NBC:(core + 1) * NBC].reshape(TOK, D)
            elif n == "c":
                m[n] = c[core * NBC:(core + 1) * NBC]
            else:
                m[n] = np.ascontiguousarray(inputs[n], dtype=np.float32).reshape(INPUT_SHAPES[n])
        for n, v in cst.items():
            m["c_" + n] = v
        in_maps.append(m)
    res = run_bass_kernel_spmd(nc, in_maps, core_ids=list(range(NCORES)))
    outs = [np.asarray(r["out"]).reshape(NBC, SEQ, D) for r in res.results]
    return np.concatenate(outs, axis=0).astype(np.float32)
```
